# Optimizing a Trainium2 kernel written in Bass

```python
import jax, jax.numpy as jnp
from jax import lax
import numpy as np

D_MODEL = 1024
BATCH = 2
SEQ = 8192
DEPTH = 1

D_PLE = 256
EPS = 1e-6

GLA_HEADS = 4
GLA_DK = 64
GLA_DV = 128
GLA_LOWRANK = 16
GLA_TAU = 16.0
GLA_CHUNK = 64
GLA_QK = GLA_HEADS * GLA_DK
GLA_V = GLA_HEADS * GLA_DV

CONV_WIDTH = D_MODEL // 2
CONV_K = 3

D_MIX = GLA_V + CONV_WIDTH

IN_SPLIT_SIZES = (GLA_QK, GLA_QK, GLA_V, GLA_V, GLA_LOWRANK, CONV_WIDTH, CONV_WIDTH, CONV_WIDTH)
D_IN = sum(IN_SPLIT_SIZES)
IN_SPLIT_POINTS = tuple(int(c) for c in np.cumsum(IN_SPLIT_SIZES)[:-1])

N_GROUPS = 4
EXPERTS_PER_GROUP = 4
N_EXPERTS = N_GROUPS * EXPERTS_PER_GROUP
TOP_K = 2
D_EXPERT = D_MODEL // 2

kernel_name = "hymba_gla_shortconv_hiermoe_ple"


def rms_norm(x, g):
    xf = x.astype(jnp.float32)
    y = xf * lax.rsqrt(jnp.mean(xf * xf, axis=-1, keepdims=True) + EPS)
    return (y * g.astype(jnp.float32)).astype(x.dtype)


def gla_chunked(q, k, v, log_a):
    q, k, v, log_a = (t.astype(jnp.float32) for t in (q, k, v, log_a))
    b, h, s, dk = q.shape
    dv = v.shape[-1]
    n = s // GLA_CHUNK

    def to_chunks(t):
        return jnp.moveaxis(t.reshape(b, h, n, GLA_CHUNK, t.shape[-1]), 2, 0)

    causal = jnp.tril(jnp.ones((GLA_CHUNK, GLA_CHUNK), dtype=bool))[None, None, :, :, None]

    def step(state, inp):
        qc, kc, vc, ac = inp
        cum = jnp.cumsum(ac, axis=2)
        o_inter = jnp.einsum('bhld,bhde->bhle', qc * jnp.exp(cum), state)
        diff = cum[:, :, :, None, :] - cum[:, :, None, :, :]
        decay = jnp.exp(jnp.where(causal, diff, -jnp.inf))
        scores = jnp.einsum('bhtd,bhsd,bhtsd->bhts', qc, kc, decay)
        o_intra = jnp.einsum('bhts,bhse->bhte', scores, vc)
        last = cum[:, :, -1:, :]
        k_dec = kc * jnp.exp(last - cum)
        new_state = jnp.exp(last[:, :, 0, :])[..., None] * state + jnp.einsum('bhld,bhle->bhde', k_dec, vc)
        return new_state, o_inter + o_intra

    state0 = jnp.zeros((b, h, dk, dv), jnp.float32)
    _, out = lax.scan(step, state0, (to_chunks(q), to_chunks(k), to_chunks(v), to_chunks(log_a)))
    return jnp.moveaxis(out, 0, 2).reshape(b, h, s, dv)


def causal_depthwise_conv(u, w):
    return lax.conv_general_dilated(
        u, w[:, None, :].astype(u.dtype), window_strides=(1,),
        padding=[(CONV_K - 1, 0)], dimension_numbers=('NWC', 'WIO', 'NWC'),
        feature_group_count=u.shape[-1])


def hybrid_mixer(h, w_in, w_gla_gate, b_gla_gate, g_gla_out, w_conv, w_out):
    b, s, _ = h.shape
    proj = h @ w_in
    q, k, v, g, a_low, conv_b, conv_c, conv_u = jnp.split(proj, IN_SPLIT_POINTS, axis=-1)

    def heads(t, d):
        return t.reshape(b, s, GLA_HEADS, d).transpose(0, 2, 1, 3)

    log_a = jax.nn.log_sigmoid((a_low @ w_gla_gate + b_gla_gate).astype(jnp.float32)) / GLA_TAU
    o = gla_chunked(heads(q, GLA_DK) * (GLA_DK ** -0.5), heads(k, GLA_DK),
                    heads(v, GLA_DV), heads(log_a, GLA_DK))
    o = rms_norm(o, g_gla_out).astype(h.dtype)
    y_gla = o.transpose(0, 2, 1, 3).reshape(b, s, GLA_V) * jax.nn.silu(g)

    y_conv = conv_b * causal_depthwise_conv(conv_c * conv_u, w_conv)

    return jnp.concatenate([y_gla, y_conv], axis=-1) @ w_out


def hier_moe(h, w_group, b_group, w_router, b_router, w_gate, w_up, w_down):
    b, s, d = h.shape
    t = h.reshape(b * s, d)
    n_tok = t.shape[0]
    group_logits = (t @ w_group + b_group).astype(jnp.float32)
    group_prob = jax.nn.softmax(group_logits, axis=-1)
    p_grp, g_sel = lax.top_k(group_prob, 1)
    exp_logits = (t @ w_router + b_router).astype(jnp.float32).reshape(n_tok, N_GROUPS, EXPERTS_PER_GROUP)
    in_group = jnp.take_along_axis(exp_logits, g_sel[:, :, None], axis=1)[:, 0]
    top_p, top_i = lax.top_k(jax.nn.softmax(in_group, axis=-1), TOP_K)
    weights = p_grp * top_p / jnp.sum(top_p, axis=-1, keepdims=True)
    expert_id = g_sel * EXPERTS_PER_GROUP + top_i
    combine = jnp.einsum('tke,tk->te', jax.nn.one_hot(expert_id, N_EXPERTS, dtype=jnp.float32), weights).astype(t.dtype)
    y = jnp.zeros_like(t)
    for e in range(N_EXPERTS):
        hid = jax.nn.silu(t @ w_gate[e]) * (t @ w_up[e])
        y = y + combine[:, e:e + 1] * (hid @ w_down[e])
    return y.reshape(b, s, d)


def setup_inputs(seed: int = 0) -> dict:
    key = jax.random.key(seed)
    ks = jax.random.split(key, 24)
    f32 = jnp.float32

    def nrm(k, shape, scale):
        return jax.random.normal(k, shape, f32) * scale

    def gain(k, shape):
        return 1.0 + 0.02 * jax.random.normal(k, shape, f32)

    L = DEPTH
    return {
        "x": nrm(ks[0], (BATCH, SEQ, D_MODEL), 1.0),
        "p": nrm(ks[1], (DEPTH, BATCH, SEQ, D_PLE), 1.0),
        "g_mix": gain(ks[2], (L, D_MODEL)),
        "w_in": nrm(ks[3], (L, D_MODEL, D_IN), D_MODEL ** -0.5),
        "w_gla_gate": nrm(ks[4], (L, GLA_LOWRANK, GLA_QK), GLA_LOWRANK ** -0.5),
        "b_gla_gate": nrm(ks[5], (L, GLA_QK), 0.1),
        "g_gla_out": gain(ks[6], (L, GLA_DV)),
        "w_conv": nrm(ks[7], (L, CONV_K, CONV_WIDTH), CONV_K ** -0.5),
        "w_out": nrm(ks[8], (L, D_MIX, D_MODEL), D_MIX ** -0.5),
        "g_moe": gain(ks[9], (L, D_MODEL)),
        "w_group": nrm(ks[10], (L, D_MODEL, N_GROUPS), D_MODEL ** -0.5),
        "b_group": nrm(ks[11], (L, N_GROUPS), 0.01),
        "w_router": nrm(ks[12], (L, D_MODEL, N_EXPERTS), D_MODEL ** -0.5),
        "b_router": nrm(ks[13], (L, N_EXPERTS), 0.01),
        "w_exp_gate": nrm(ks[14], (L, N_EXPERTS, D_MODEL, D_EXPERT), D_MODEL ** -0.5),
        "w_exp_up": nrm(ks[15], (L, N_EXPERTS, D_MODEL, D_EXPERT), D_MODEL ** -0.5),
        "w_exp_down": nrm(ks[16], (L, N_EXPERTS, D_EXPERT, D_MODEL), D_EXPERT ** -0.5),
        "g_ple": gain(ks[17], (L, D_MODEL)),
        "w_ple_gate": nrm(ks[18], (L, D_MODEL, D_MODEL), D_MODEL ** -0.5),
        "w_ple_proj": nrm(ks[19], (L, D_PLE, D_MODEL), D_PLE ** -0.5),
        "g_final": gain(ks[20], (D_MODEL,)),
    }


def reference(x, p, g_mix, w_in, w_gla_gate, b_gla_gate, g_gla_out, w_conv, w_out,
              g_moe, w_group, b_group, w_router, b_router, w_exp_gate, w_exp_up, w_exp_down,
              g_ple, w_ple_gate, w_ple_proj, g_final):
    for i in range(DEPTH):
        h = rms_norm(x, g_mix[i])
        x = x + hybrid_mixer(h, w_in[i], w_gla_gate[i], b_gla_gate[i], g_gla_out[i], w_conv[i], w_out[i])
        h = rms_norm(x, g_moe[i])
        x = x + hier_moe(h, w_group[i], b_group[i], w_router[i], b_router[i],
                         w_exp_gate[i], w_exp_up[i], w_exp_down[i])
        gate = jax.nn.sigmoid(rms_norm(x, g_ple[i]) @ w_ple_gate[i])
        x = x + gate * (p[i] @ w_ple_proj[i])
    return rms_norm(x, g_final)
```

```python
import numpy as np
from contextlib import ExitStack
import concourse.bass as bass
import concourse.mybir as mybir
from concourse.bass_utils import run_bass_kernel_spmd

F32 = mybir.dt.float32
BF16 = mybir.dt.bfloat16
AF = mybir.ActivationFunctionType
ALU = mybir.AluOpType
AX = mybir.AxisListType

EPS = 1e-6
NT_ALL = 64
NT_OWN = 16
NT_PRE = NT_ALL - NT_OWN
D = 1024
DIN = 3088
NE = 16


class Buf:
    __slots__ = ("name", "w", "r")

    def __init__(self, name):
        self.name = name
        self.w = None
        self.r = []


class DSem:
    def __init__(self, h):
        self.h = h
        self.count = 0


class Prog:
    def __init__(self, nc, es):
        self.nc = nc
        self.es = es
        self.names = ["pe", "act", "dve", "pool", "sp"]
        self.streams = {k: [] for k in self.names}
        self.sem = {k: es.enter_context(nc.semaphore("c_" + k)) for k in ["pe", "act", "dve"]}
        self.cnt = {k: 0 for k in self.sem}
        self.known = {k: {} for k in self.names}
        self.handles = {}
        self.dsems = []
        self.nds = 0

    def dsem(self):
        self.nds += 1
        s = DSem(self.es.enter_context(self.nc.semaphore("d%d" % self.nds)))
        self.dsems.append(s)
        return s

    def _filter(self, e, toks):
        need = {}
        for (s, v) in toks:
            if e == "pe" and s is self.sem["pe"]:
                continue
            k = id(s)
            self.handles[k] = s
            if need.get(k, 0) < v:
                need[k] = v
        out = []
        kn = self.known[e]
        for k, v in need.items():
            if kn.get(k, 0) < v:
                kn[k] = v
                out.append((self.handles[k], v))
        return out

    def op(self, e, fn, reads=(), writes=(), dsem=None):
        toks = []
        for b in reads:
            if b.w is not None:
                toks.append(b.w)
        for b in writes:
            toks.extend(b.r)
            if b.w is not None:
                toks.append(b.w)
        waits = self._filter(e, toks)
        if dsem is None:
            self.cnt[e] += 1
            tok = (self.sem[e], self.cnt[e])
            inc = 1
        else:
            dsem.count += 16
            tok = (dsem.h, dsem.count)
            inc = 16
        self.streams[e].append((waits, fn, tok[0], inc))
        for b in writes:
            b.w = tok
            b.r = []
        for b in reads:
            b.r.append(tok)
        return tok

    def wait_only(self, e, toks):
        waits = self._filter(e, toks)
        if waits:
            self.streams[e].append((waits, None, None, 0))

    def barrier(self):
        toks = [(self.sem[k], self.cnt[k]) for k in self.sem if self.cnt[k] > 0]
        toks += [(d.h, d.count) for d in self.dsems if d.count > 0]
        for e in self.names:
            self.wait_only(e, toks)

    def emit(self):
        nc = self.nc
        with nc.Block() as block:
            decos = {"pe": block.tensor, "act": block.scalar, "dve": block.vector,
                     "pool": block.gpsimd, "sp": block.sync}
            for k in self.names:
                stream = self.streams[k]

                def body(engine, stream=stream):
                    for waits, fn, sem, inc in stream:
                        for (s, v) in waits:
                            engine.wait_ge(s, v)
                        if fn is not None:
                            ins = fn(engine)
                            ins.then_inc(sem, inc)

                decos[k](body)


def build_program(stop_after=None):
    nc = bass.Bass("TRN2", target_bir_lowering=False)
    es = ExitStack()

    def din(name, shape):
        return nc.dram_tensor(name, list(shape), F32, kind="ExternalInput").ap()

    xs_d = din("xs", [NT_ALL * 128, D])
    p_d = din("p_own", [NT_OWN * 128, 256])
    w_in_d = din("w_in", [D, DIN])
    w_out_d = din("w_out", [D, D])
    wg_d = din("w_exp_gate", [NE * D, 512])
    wu_d = din("w_exp_up", [NE * D, 512])
    wd_d = din("w_exp_down", [NE * 512, D])
    wpg_d = din("w_ple_gate", [D, D])
    wpp_d = din("w_ple_proj", [256, D])
    gmix_d = din("g_mix", [1, D])
    gmoe_d = din("g_moe", [1, D])
    gple_d = din("g_ple", [1, D])
    gfin_d = din("g_final", [1, D])
    wr_d = din("w_rt", [D, 20])
    br_d = din("b_rt", [1, 20])
    wgate_d = din("wg_aug", [32, 256])
    wconv_d = din("wconv_t", [128, 12])
    ggo_d = din("ggo", [128, 1])
    cid_d = din("c_ident", [128, 128])
    ctl_d = din("c_tril", [128, 128])
    ctu_d = din("c_triu", [128, 128])
    cmask_d = din("c_mask", [128, 512])
    cones_d = din("c_ones", [128, 128])
    cneg_d = din("c_neg", [128, 1])
    y_d = nc.dram_tensor("y", [NT_OWN * 128, D], F32, kind="ExternalOutput").ap()

    def sb(name, shape, dt):
        return es.enter_context(nc.sbuf_tensor("s_" + name, list(shape), dt))

    xres = sb("xres", [128, NT_OWN * D], F32)
    hT_all = sb("hT_all", [128, 8 * 2048], BF16)
    y1_all = sb("y1_all", [128, 4 * 2048], BF16)
    wbig = sb("wbig", [128, 24704], BF16)
    scr2 = sb("scr2", [128, 7200], F32)
    carry_t = sb("carry", [128, 8], F32)
    identb = sb("identb", [128, 128], BF16)
    identf = sb("identf", [128, 128], F32)
    tril = sb("tril", [128, 128], F32)
    triu = sb("triu", [128, 128], F32)
    maskb = sb("maskb", [128, 512], BF16)
    onesb = sb("onesb", [128, 128], BF16)
    negcol = sb("negcol", [128, 1], F32)
    gA = sb("gA", [128, D], F32)
    gB = sb("gB", [128, D], F32)
    wr = sb("wr", [128, 8 * 20], F32)
    brb = sb("brb", [128, 20], F32)
    wgate = sb("wgate", [32, 256], F32)
    wconv = sb("wconv", [128, 12], F32)
    ggo = sb("ggo", [128, 1], F32)
    Lg = sb("Lg", [128, NT_OWN * 20], F32)
    comb = sb("comb", [128, NT_OWN * 16], F32)
    small = sb("small", [128, 16], F32)

    banks = [es.enter_context(nc.psum_tensor("bank%d" % i, [128, 512], F32)) for i in range(8)]

    P = Prog(nc, es)

    class Carver:
        def __init__(self, base, nwords):
            self.base = base
            self.off = 0
            self.n = nwords

        def f32(self, n):
            a = self.base[:, self.off:self.off + n]
            self.off += n
            assert self.off <= self.n, (self.off, self.n)
            return a

        def bf16(self, n):
            assert n % 2 == 0
            return self.f32(n // 2).bitcast(BF16)

    b_const = Buf("const")
    ds_const = P.dsem()

    def ld(e, out_ap, in_ap, buf, dsem):
        return P.op(e, lambda g: g.dma_start(out=out_ap, in_=in_ap), writes=[buf], dsem=dsem)

    ld("sp", identf[:], cid_d, b_const, ds_const)
    ld("sp", tril[:], ctl_d, b_const, ds_const)
    ld("sp", triu[:], ctu_d, b_const, ds_const)
    ld("sp", negcol[:], cneg_d, b_const, ds_const)
    ld("sp", gA[:], gmix_d.broadcast_to([128, D]), b_const, ds_const)
    ld("sp", wr[:].rearrange("p (k n) -> p k n", k=8), wr_d.rearrange("(k p) n -> p k n", p=128), b_const, ds_const)
    ld("sp", brb[:], br_d.broadcast_to([128, 20]), b_const, ds_const)
    ld("sp", wgate[:], wgate_d, b_const, ds_const)
    ld("sp", wconv[:], wconv_d, b_const, ds_const)
    ld("sp", ggo[:], ggo_d, b_const, ds_const)
    ds_const2 = P.dsem()
    ld("pool", identb[:], cid_d, b_const, ds_const2)
    ld("pool", maskb[:], cmask_d, b_const, ds_const2)
    ld("pool", onesb[:], cones_d, b_const, ds_const2)

    c1 = Carver(xres, NT_OWN * D)
    Wqkva = c1.bf16(8 * 1296).rearrange("p (k c) -> p k c", k=8)
    xs_t = [c1.f32(1024) for _ in range(2)]
    hb_t = [c1.bf16(1024) for _ in range(2)]
    hts_off = c1.off
    hTs = [c1.bf16(1024).rearrange("p (k t) -> p k t", k=8) for _ in range(4)]
    aT_t = [c1.f32(128) for _ in range(2)]
    e1 = c1.f32(256)
    sp_t = [c1.f32(256) for _ in range(2)]
    Er = c1.f32(256)
    El_t = [c1.f32(8) for _ in range(2)]
    kd_t = [c1.bf16(256) for _ in range(2)]
    vb_t = [c1.bf16(512) for _ in range(2)]
    Eq = c1.f32(256)
    Ek = c1.f32(256)
    qt_t = [c1.bf16(256) for _ in range(2)]
    kt_t = [c1.bf16(256) for _ in range(2)]
    qkT0 = c1.bf16(1024)
    Pm = c1.bf16(512)
    S = c1.f32(512)
    Sb0 = c1.bf16(512)
    sq0 = c1.bf16(512)
    rstd = c1.f32(512)
    c1o = Carver(xres, hts_off + 1536)
    c1o.off = hts_off
    vb4 = [vb_t[0], vb_t[1], c1o.bf16(512), c1o.bf16(512)]
    qkT_t = [qkT0, c1o.bf16(1024)]
    Sb_t = [Sb0, c1o.bf16(512)]
    sq_t = [sq0, c1o.bf16(512)]

    w_in_v = w_in_d.rearrange("(k p) c -> p k c", p=128)
    b_wqkva = Buf("wqkva")
    ds_w1 = P.dsem()
    ld("pool", Wqkva[:, :, 0:1024], w_in_v[:, :, 0:1024], b_wqkva, ds_w1)
    ld("pool", Wqkva[:, :, 1024:1040], w_in_v[:, :, 1536:1552], b_wqkva, ds_w1)

    Wgbcu = wbig[:, 0:8 * 2048].rearrange("p (k c) -> p k c", k=8)
    Wout = wbig[:, 8 * 2048:8 * 3072].rearrange("p (k c) -> p k c", k=8)
    b_w2 = Buf("w2")
    ds_w2 = P.dsem()
    ld("pool", Wgbcu[:, :, 0:512], w_in_v[:, :, 1024:1536], b_w2, ds_w2)
    ld("pool", Wgbcu[:, :, 512:2048], w_in_v[:, :, 1552:3088], b_w2, ds_w2)
    ld("pool", Wout, w_out_d.rearrange("(k p) c -> p k c", p=128), b_w2, ds_w2)

    if stop_after == "consts":
        P.barrier(); P.emit(); return nc, es
    import os
    b_xs = [Buf("xs0"), Buf("xs1")]
    ds_xs = [P.dsem(), P.dsem()]
    b_hb = [Buf("hb0"), Buf("hb1")]
    b_hTs = [Buf("hTs%d" % i) for i in range(4)]
    b_hT = [Buf("hT%d" % i) for i in range(NT_OWN)]
    b_y1 = [Buf("y1_%d" % i) for i in range(NT_OWN)]
    b_bank = [Buf("bank%d" % i) for i in range(8)]
    b_aTp = Buf("aTp")
    b_lastp = Buf("lastp")
    b_zp = Buf("zp")
    b_aT = [Buf("aT0"), Buf("aT1")]
    b_e1 = Buf("e1")
    b_sp = [Buf("sp0"), Buf("sp1")]
    b_Eq, b_Ek, b_Er = Buf("Eq"), Buf("Ek"), Buf("Er")
    b_El = [Buf("El0"), Buf("El1")]
    b_kd = [Buf("kd0"), Buf("kd1")]
    b_vb = [Buf("vb0"), Buf("vb1")]
    b_qt, b_kt = [Buf("qt0"), Buf("qt1")], [Buf("kt0"), Buf("kt1")]
    b_Pm, b_S, b_rstd = Buf("Pm"), Buf("S"), Buf("rstd")
    b_qkT, b_Sb, b_sq = [Buf("qkT0"), Buf("qkT1")], [Buf("Sb0"), Buf("Sb1")], [Buf("sq0"), Buf("sq1")]
    b_vb4 = [b_vb[0], b_vb[1], Buf("vb2"), Buf("vb3")]

    def vb_of(i):
        if i >= NT_PRE:
            return vb4[i % 4], b_vb4[i % 4]
        return vb_t[i % 2], b_vb[i % 2]
    b_ss = [Buf("ss0"), Buf("ss1")]
    b_rs = [Buf("rs0"), Buf("rs1")]

    bank0b = banks[0][:, 0:512].bitcast(BF16)
    bank7b = banks[7][:, 0:512].bitcast(BF16)

    hT_all_v = hT_all[:].rearrange("p (k t) -> p k t", k=8)
    y1_all_v = y1_all[:].rearrange("p (h t) -> p h t", h=4)
    S_v = S.rearrange("p (h e) -> p h e", h=4)

    P.op("dve", lambda g: g.memset(S[0:64, :], 0.0), writes=[b_S])
    for a_ in range(2):
        P.op("dve", lambda g, a_=a_: g.memset(aT_t[a_][0:32, :], 1.0), writes=[b_aT[a_]])

    def rms_stats(src_ap, b_src, ss_col, rs_col, junk, b_junk, bss=None, brs=None):
        bss = bss or b_ss[0]
        brs = brs or b_rs[0]
        P.op("act", lambda g: g.activation(out=junk, in_=src_ap, func=AF.Square, accum_out=small[:, ss_col:ss_col + 1]),
             reads=[b_src], writes=[b_junk, bss])
        P.op("act", lambda g: g.activation(out=small[:, rs_col:rs_col + 1], in_=small[:, ss_col:ss_col + 1], func=AF.Ln,
                                           scale=1.0 / D, bias=EPS),
             reads=[bss], writes=[brs])
        P.op("act", lambda g: g.activation(out=small[:, rs_col:rs_col + 1], in_=small[:, rs_col:rs_col + 1], func=AF.Exp, scale=-0.5),
             reads=[brs], writes=[brs])

    def hT_of(i):
        if i >= NT_PRE:
            oi = i - NT_PRE
            return hT_all_v[:, :, oi * 128:(oi + 1) * 128], b_hT[oi]
        return hTs[i % 4], b_hTs[i % 4]

    def stageA(i):
        sl = i % 2
        xt = xs_t[sl]
        ld("sp", xt, xs_d[i * 128:(i + 1) * 128, :], b_xs[sl], ds_xs[sl])
        hb = hb_t[sl]
        rms_stats(xt, b_xs[sl], 8 * sl, 8 * sl + 1, hb, b_hb[sl], b_ss[sl], b_rs[sl])
        P.op("dve", lambda g: g.scalar_tensor_tensor(out=hb, in0=xt, scalar=small[:, 8 * sl + 1:8 * sl + 2], in1=gA[:],
                                                     op0=ALU.mult, op1=ALU.mult),
             reads=[b_xs[sl], b_rs[sl], b_const], writes=[b_hb[sl]])

    def stageB(i):
        sl = i % 2
        hb = hb_t[sl]
        hT, bh = hT_of(i)
        for k in range(8):
            P.op("pe", lambda g, k=k: g.transpose(out=bank0b[:, k * 128:(k + 1) * 128], in_=hb[:, k * 128:(k + 1) * 128],
                                                 identity=identb[:]),
                 reads=[b_hb[sl], b_const], writes=[b_bank[0]])
        P.op("act", lambda g: g.copy(out=hT, in_=bank0b.rearrange("p (k t) -> p k t", k=8)),
             reads=[b_bank[0]], writes=[bh])

    def stageC(i):
        sl = i % 2
        hT, bh = hT_of(i)
        for k in range(8):
            P.op("pe", lambda g, k=k: g.matmul(out=banks[1][0:16, 0:128], lhsT=Wqkva[:, k, 1024:1040], rhs=hT[:, k, :],
                                              start=(k == 0), stop=(k == 7)),
                 reads=[bh, b_wqkva], writes=[b_bank[1]])
        P.op("act", lambda g: g.copy(out=aT_t[sl][0:16, :], in_=banks[1][0:16, 0:128]), reads=[b_bank[1]], writes=[b_aT[sl]])

    def stageD(i):
        sl = i % 2
        zb = 1 if i >= NT_PRE else 2
        P.op("pe", lambda g: g.matmul(out=banks[zb][:, 256:512], lhsT=aT_t[sl][0:17, :], rhs=wgate[0:17, :], start=True, stop=True),
             reads=[b_aT[sl], b_const], writes=[b_bank[zb]])
        P.op("act", lambda g: g.activation(out=e1, in_=banks[zb][:, 256:512], func=AF.Exp, scale=-1.0), reads=[b_bank[zb]], writes=[b_e1])
        P.op("act", lambda g: g.activation(out=sp_t[sl], in_=e1, func=AF.Ln, bias=1.0), reads=[b_e1], writes=[b_sp[sl]])

    def stageE(i):
        own = i >= NT_PRE
        sl = i % 2
        hT, bh = hT_of(i)
        spb = sp_t[sl]
        if own:
            P.op("pe", lambda g: g.matmul(out=banks[3][:, 0:256], lhsT=tril[:], rhs=spb, start=True, stop=True),
                 reads=[b_sp[sl], b_const], writes=[b_bank[3]])
        P.op("pe", lambda g: g.matmul(out=banks[3][:, 256:512], lhsT=triu[:], rhs=spb, start=True, stop=True),
             reads=[b_sp[sl], b_const], writes=[b_bank[3]])
        lb = 1 if own else 7
        for h in range(4):
            P.op("pe", lambda g, h=h: g.matmul(out=banks[lb][0:64, 128 + h:129 + h], lhsT=spb[:, h * 64:(h + 1) * 64], rhs=negcol[:],
                                              start=True, stop=True),
                 reads=[b_sp[sl], b_const], writes=[b_bank[lb]])
        if own:
            ncol, c0 = 512, 0
        else:
            ncol, c0 = 256, 256
        for k in range(8):
            P.op("pe", lambda g, k=k: g.matmul(out=banks[4][:, 0:ncol], lhsT=hT[:, k, :], rhs=Wqkva[:, k, c0:c0 + ncol],
                                              start=(k == 0), stop=(k == 7)),
                 reads=[bh, b_wqkva], writes=[b_bank[4]])
        for k in range(8):
            P.op("pe", lambda g, k=k: g.matmul(out=banks[5][:, 0:512], lhsT=hT[:, k, :], rhs=Wqkva[:, k, 512:1024],
                                              start=(k == 0), stop=(k == 7)),
                 reads=[bh, b_wqkva], writes=[b_bank[5]])
        P.op("act", lambda g: g.activation(out=Er, in_=banks[3][:, 256:512], func=AF.Exp), reads=[b_bank[3]], writes=[b_Er])
        if own:
            P.op("act", lambda g: g.activation(out=Eq, in_=banks[3][:, 0:256], func=AF.Exp), reads=[b_bank[3]], writes=[b_Eq])
            P.op("act", lambda g: g.activation(out=Ek, in_=banks[3][:, 0:256], func=AF.Exp, scale=-1.0), reads=[b_bank[3]], writes=[b_Ek])
        P.op("act", lambda g: g.activation(out=El_t[sl][0:64, 0:4], in_=banks[lb][0:64, 128:132], func=AF.Exp),
             reads=[b_bank[lb]], writes=[b_El[sl]])
        kcol = 256 if own else 0
        P.op("dve", lambda g: g.tensor_tensor(out=kd_t[sl], in0=banks[4][:, kcol:kcol + 256], in1=Er, op=ALU.mult),
             reads=[b_bank[4], b_Er], writes=[b_kd[sl]])
        if own:
            P.op("dve", lambda g: g.scalar_tensor_tensor(out=qt_t[sl], in0=banks[4][:, 0:256], scalar=0.125, in1=Eq, op0=ALU.mult, op1=ALU.mult),
                 reads=[b_bank[4], b_Eq], writes=[b_qt[sl]])
            P.op("dve", lambda g: g.tensor_tensor(out=kt_t[sl], in0=banks[4][:, 256:512], in1=Ek, op=ALU.mult),
                 reads=[b_bank[4], b_Ek], writes=[b_kt[sl]])
        vb, bvb = vb_of(i)
        P.op("dve", lambda g: g.tensor_copy(out=vb, in_=banks[5][:, 0:512]), reads=[b_bank[5]], writes=[bvb])

    def stageF(i):
        sl = i % 2
        kd = kd_t[sl]
        vb, bvb = vb_of(i)
        for h in range(4):
            P.op("pe", lambda g, h=h: g.matmul(out=banks[6][0:64, h * 128:(h + 1) * 128], lhsT=kd[:, h * 64:(h + 1) * 64],
                                              rhs=vb[:, h * 128:(h + 1) * 128], start=True, stop=True),
                 reads=[b_kd[sl], bvb], writes=[b_bank[6]])

    def stageS(i):
        sl = i % 2
        P.op("dve", lambda g: g.tensor_tensor(out=S_v[0:64, :, :], in0=S_v[0:64, :, :],
                                              in1=El_t[sl][0:64, 0:4].unsqueeze(2).broadcast_to([64, 4, 128]), op=ALU.mult),
             reads=[b_S, b_El[sl]], writes=[b_S])
        P.op("dve", lambda g: g.tensor_tensor(out=S[0:64, :], in0=S[0:64, :], in1=banks[6][0:64, 0:512], op=ALU.add),
             reads=[b_S, b_bank[6]], writes=[b_S])

    def stageFo(i):
        sl = i % 2
        stageF(i)
        P.op("act", lambda g: g.copy(out=Sb_t[sl][0:64, :], in_=S[0:64, :]), reads=[b_S], writes=[b_Sb[sl]])
        stageS(i)
        for j in range(8):
            src = qt_t[sl] if j < 4 else kt_t[sl]
            a = j % 4
            P.op("pe", lambda g, j=j, src=src, a=a: g.transpose(out=bank7b[0:64, j * 128:(j + 1) * 128], in_=src[:, a * 64:(a + 1) * 64],
                                                               identity=identb[:]),
                 reads=[b_qt[sl], b_kt[sl], b_const], writes=[b_bank[7]])
        P.op("act", lambda g: g.copy(out=qkT_t[sl][0:64, :], in_=bank7b[0:64, :]), reads=[b_bank[7]], writes=[b_qkT[sl]])

    def stageHI(i):
        sl = i % 2
        vb, bvb = vb_of(i)
        qkT = qkT_t[sl]
        Sb_v = Sb_t[sl].rearrange("p (h e) -> p h e", h=4)
        for h in range(4):
            P.op("pe", lambda g, h=h: g.matmul(out=banks[2][:, h * 128:(h + 1) * 128], lhsT=qkT[0:64, (4 + h) * 128:(5 + h) * 128],
                                              rhs=qkT[0:64, h * 128:(h + 1) * 128], start=True, stop=True),
                 reads=[b_qkT[sl]], writes=[b_bank[2]])
        P.op("dve", lambda g: g.tensor_tensor(out=Pm, in0=banks[2][:, 0:512], in1=maskb[:], op=ALU.mult),
             reads=[b_bank[2], b_const], writes=[b_Pm])
        for h in range(4):
            P.op("pe", lambda g, h=h: g.matmul(out=banks[7][:, h * 128:(h + 1) * 128], lhsT=vb[:, h * 128:(h + 1) * 128],
                                              rhs=Pm[:, h * 128:(h + 1) * 128], start=True, stop=False),
                 reads=[bvb, b_Pm], writes=[b_bank[7]])
            P.op("pe", lambda g, h=h: g.matmul(out=banks[7][:, h * 128:(h + 1) * 128], lhsT=Sb_v[0:64, h, :],
                                              rhs=qkT[0:64, h * 128:(h + 1) * 128], start=False, stop=True),
                 reads=[b_Sb[sl], b_qkT[sl]], writes=[b_bank[7]])
        P.op("act", lambda g: g.activation(out=sq_t[sl], in_=banks[7][:, 0:512], func=AF.Square), reads=[b_bank[7]], writes=[b_sq[sl]])

    def stageJ(i):
        oi = i - NT_PRE
        sl = i % 2
        P.op("pe", lambda g: g.matmul(out=banks[2][:, 0:512], lhsT=onesb[:], rhs=sq_t[sl], start=True, stop=True),
             reads=[b_sq[sl], b_const], writes=[b_bank[2]])
        P.op("act", lambda g: g.activation(out=rstd, in_=banks[2][:, 0:512], func=AF.Ln, bias=EPS), reads=[b_bank[2]], writes=[b_rstd])
        P.op("act", lambda g: g.activation(out=rstd, in_=rstd, func=AF.Exp, scale=-0.5), reads=[b_rstd], writes=[b_rstd])
        P.op("dve", lambda g: g.tensor_tensor(out=y1_all_v[:, :, oi * 128:(oi + 1) * 128],
                                              in0=banks[7][:, 0:512].rearrange("p (h t) -> p h t", h=4),
                                              in1=rstd.rearrange("p (h t) -> p h t", h=4), op=ALU.mult),
             reads=[b_bank[7], b_rstd], writes=[b_y1[oi]])

    T0 = int(os.environ.get('K_T0', 0))
    T1 = int(os.environ.get('K_T1', NT_ALL))
    pre_stages = [stageA, stageB, stageC, stageD, stageE, lambda i: (stageF(i), stageS(i))]
    npre = min(T1, NT_PRE)
    for step in range(T0, npre + len(pre_stages) - 1):
        for k, st in enumerate(pre_stages):
            t = step - k
            if T0 <= t < npre:
                st(t)
    b_carry = Buf("carry")
    carry_v = carry_t[:].rearrange("p (c j) -> p c j", c=4)
    hT47, b_h47 = hT_of(NT_PRE - 1)
    for cc in range(4):
        for k in range(8):
            P.op("pe", lambda g, cc=cc, k=k: g.matmul(out=banks[2][:, 0:2], lhsT=Wgbcu[:, k, 1024 + cc * 128:1024 + (cc + 1) * 128],
                                                     rhs=hT47[:, k, 126:128], start=(k == 0), stop=(k == 7)),
                 reads=[b_h47, b_w2], writes=[b_bank[2]])
        for k in range(8):
            P.op("pe", lambda g, cc=cc, k=k: g.matmul(out=banks[3][:, 0:2], lhsT=Wgbcu[:, k, 1536 + cc * 128:1536 + (cc + 1) * 128],
                                                     rhs=hT47[:, k, 126:128], start=(k == 0), stop=(k == 7)),
                 reads=[b_h47, b_w2], writes=[b_bank[3]])
        P.op("act", lambda g: g.copy(out=small[:, 4:6], in_=banks[2][:, 0:2]), reads=[b_bank[2]], writes=[b_ss[0]])
        P.op("dve", lambda g, cc=cc: g.tensor_tensor(out=carry_v[:, cc, :], in0=banks[3][:, 0:2], in1=small[:, 4:6], op=ALU.mult),
             reads=[b_bank[3], b_ss[0]], writes=[b_carry])
    P.barrier()
    own_stages = [(stageJ, 7), (stageA, 0), (stageB, 1), (stageC, 2), (stageD, 3), (stageE, 4), (stageFo, 5), (stageHI, 6)]
    o0 = max(T0, NT_PRE)
    for step in range(o0, T1 + 7):
        for st, k in own_stages:
            t = step - k
            if o0 <= t < T1:
                st(t)

    if stop_after in ("p1", "p1s"):
        P.barrier(); P.emit(); return nc, es
    P.barrier()
    b_xr = [Buf("xr%d" % i) for i in range(NT_OWN)]
    for t in range(NT_OWN):
        ld("sp", xres[:, t * D:(t + 1) * D], xs_d[(NT_PRE + t) * 128:(NT_PRE + t + 1) * 128, :], b_xr[t], P.dsem())
    b_gB = Buf("gB")
    ds_gB = P.dsem()
    ld("sp", gB[:], gmoe_d.broadcast_to([128, D]), b_gB, ds_gB)

    c2 = Carver(scr2, 7200)
    gs = c2.f32(512)
    Ct = c2.f32(512)
    cu = c2.f32(520)
    acc = c2.f32(512)
    yg = c2.bf16(2048).rearrange("p (c t) -> p c t", c=4)
    ycv = c2.bf16(2048).rearrange("p (c t) -> p c t", c=4)
    h2_t = [c2.f32(1024) for _ in range(2)]
    h2Tf = c2.f32(1024).rearrange("p (k t) -> p k t", k=8)
    b_gs, b_Ct, b_cu, b_acc = Buf("gs"), Buf("Ct"), Buf("cu"), Buf("acc")
    b_yg, b_ycv, b_h2Tf, b_Lg = Buf("yg"), Buf("ycv"), Buf("h2Tf"), Buf("Lg")
    b_h2 = [Buf("h2a"), Buf("h2b")]

    Lg_v = Lg[:].rearrange("p (t n) -> p t n", t=NT_OWN)
    L2 = int(os.environ.get('K_L2', 99))
    b_s2 = [[Buf("s2_%d_%d" % (a, b)) for b in range(2)] for a in range(2)]
    for blk in range(4 if L2 >= 1 else 0):
        tcols = slice(blk * 512, (blk + 1) * 512)
        bts = [b_hT[blk * 4 + t] for t in range(4)]
        for cc in range(4):
            bb = 4 * (cc % 2)
            for k in range(8):
                P.op("pe", lambda g, cc=cc, k=k, bb=bb, tcols=tcols: g.matmul(out=banks[bb][:, 0:512], lhsT=Wgbcu[:, k, cc * 128:(cc + 1) * 128],
                                                                rhs=hT_all_v[:, k, tcols], start=(k == 0), stop=(k == 7)),
                     reads=bts + [b_w2], writes=[b_bank[bb]])
            P.op("act", lambda g, bb=bb: g.activation(out=gs, in_=banks[bb][:, 0:512], func=AF.Silu), reads=[b_bank[bb]], writes=[b_gs])
            P.op("dve", lambda g, cc=cc, tcols=tcols: g.scalar_tensor_tensor(out=yg[:, cc, :], in0=y1_all_v[:, cc, tcols], scalar=ggo[:, 0:1],
                                                                in1=gs, op0=ALU.mult, op1=ALU.mult),
                 reads=[b_gs, b_const] + [b_y1[blk * 4 + t] for t in range(4)], writes=[b_yg])
            for (bk, off) in ((bb + 1, 512), (bb + 2, 1024), (bb + 3, 1536)):
                for k in range(8):
                    P.op("pe", lambda g, cc=cc, k=k, bk=bk, off=off, tcols=tcols: g.matmul(
                        out=banks[bk][:, 0:512], lhsT=Wgbcu[:, k, off + cc * 128:off + (cc + 1) * 128],
                        rhs=hT_all_v[:, k, tcols], start=(k == 0), stop=(k == 7)),
                        reads=bts + [b_w2], writes=[b_bank[bk]])
            P.op("act", lambda g, bb=bb: g.copy(out=Ct, in_=banks[bb + 2][:, 0:512]), reads=[b_bank[bb + 2]], writes=[b_Ct])
            P.op("act", lambda g, cc=cc: g.copy(out=cu[:, 0:2], in_=carry_v[:, cc, :]), reads=[b_carry], writes=[b_cu])
            P.op("dve", lambda g, bb=bb: g.tensor_tensor(out=cu[:, 2:514], in0=banks[bb + 3][:, 0:512], in1=Ct, op=ALU.mult),
                 reads=[b_bank[bb + 3], b_Ct], writes=[b_cu])
            P.op("act", lambda g, cc=cc: g.copy(out=carry_v[:, cc, :], in_=cu[:, 512:514]), reads=[b_cu], writes=[b_carry])
            P.op("dve", lambda g, cc=cc: g.tensor_scalar(out=acc, in0=cu[:, 2:514], scalar1=wconv[:, cc * 3 + 2:cc * 3 + 3], scalar2=None,
                                                         op0=ALU.mult),
                 reads=[b_cu, b_const], writes=[b_acc])
            P.op("dve", lambda g, cc=cc: g.scalar_tensor_tensor(out=acc, in0=cu[:, 1:513], scalar=wconv[:, cc * 3 + 1:cc * 3 + 2], in1=acc,
                                                                op0=ALU.mult, op1=ALU.add),
                 reads=[b_cu, b_const], writes=[b_acc])
            P.op("dve", lambda g, cc=cc: g.scalar_tensor_tensor(out=acc, in0=cu[:, 0:512], scalar=wconv[:, cc * 3:cc * 3 + 1], in1=acc,
                                                                op0=ALU.mult, op1=ALU.add),
                 reads=[b_cu, b_const], writes=[b_acc])
            P.op("dve", lambda g, cc=cc, bb=bb: g.tensor_tensor(out=ycv[:, cc, :], in0=banks[bb + 1][:, 0:512], in1=acc, op=ALU.mult),
                 reads=[b_bank[bb + 1], b_acc], writes=[b_ycv])

        def st2P(t):
            ti = blk * 4 + t
            sl = t % 2
            xr = xres[:, ti * D:(ti + 1) * D]
            for half in range(2):
                for kc in range(8):
                    src = yg if kc < 4 else ycv
                    bsrc = b_yg if kc < 4 else b_ycv
                    P.op("pe", lambda g, half=half, kc=kc, src=src: g.matmul(
                        out=banks[half][:, 0:512], lhsT=src[:, kc % 4, t * 128:(t + 1) * 128],
                        rhs=Wout[:, kc, half * 512:(half + 1) * 512], start=(kc == 0), stop=(kc == 7)),
                        reads=[bsrc, b_w2], writes=[b_bank[half]])
                P.op("dve", lambda g, half=half: g.tensor_tensor(out=xr[:, half * 512:(half + 1) * 512], in0=banks[half][:, 0:512],
                                                                 in1=xr[:, half * 512:(half + 1) * 512], op=ALU.add),
                     reads=[b_bank[half]], writes=[b_xr[ti]])
            c = 2 + 8 * sl
            rms_stats(xr, b_xr[ti], c, c + 1, h2_t[sl].bitcast(BF16)[:, 0:1024], b_h2[sl], b_s2[sl][0], b_s2[sl][1])
            P.op("dve", lambda g: g.scalar_tensor_tensor(out=h2_t[sl], in0=xr, scalar=small[:, c + 1:c + 2], in1=gB[:], op0=ALU.mult, op1=ALU.mult),
                 reads=[b_xr[ti], b_s2[sl][1], b_gB], writes=[b_h2[sl]])

        def st2Q(t):
            ti = blk * 4 + t
            sl = t % 2
            h2 = h2_t[sl]
            for k in range(8):
                bk = 2 + k // 4
                P.op("pe", lambda g, k=k, bk=bk: g.matmul(out=banks[bk][:, (k % 4) * 128:(k % 4 + 1) * 128], lhsT=h2[:, k * 128:(k + 1) * 128],
                                                         rhs=identf[:], start=True, stop=True),
                     reads=[b_h2[sl], b_const], writes=[b_bank[bk]])
            for hh in range(2):
                P.op("dve", lambda g, hh=hh: g.tensor_copy(out=h2Tf[:, hh * 4:(hh + 1) * 4, :],
                                                           in_=banks[2 + hh][:, 0:512].rearrange("p (k t) -> p k t", k=4)),
                     reads=[b_bank[2 + hh]], writes=[b_h2Tf])
            P.op("act", lambda g: g.copy(out=hT_all_v[:, :, ti * 128:(ti + 1) * 128], in_=h2Tf),
                 reads=[b_h2Tf], writes=[b_hT[ti]])

        def st2R(t):
            ti = blk * 4 + t
            for k in range(8):
                P.op("pe", lambda g, k=k: g.matmul(out=banks[4][:, 0:20], lhsT=h2Tf[:, k, :], rhs=wr[:, k * 20:(k + 1) * 20],
                                                  start=(k == 0), stop=(k == 7)),
                     reads=[b_h2Tf, b_const], writes=[b_bank[4]])
            P.op("dve", lambda g: g.tensor_tensor(out=Lg_v[:, ti, :], in0=banks[4][:, 0:20], in1=brb[:], op=ALU.add),
                 reads=[b_bank[4], b_const], writes=[b_Lg])

        for step in range(6):
            if step < 4:
                st2P(step)
            if 2 <= step:
                st2R(step - 2)
            if 1 <= step < 5:
                st2Q(step - 1)

    if stop_after == "p2":
        P.barrier(); P.emit(); return nc, es
    P.barrier()
    c3 = Carver(scr2, 7200)
    T16 = NT_OWN
    gmax = c3.f32(16)
    ohg = c3.f32(64)
    ge = c3.f32(64)
    gsum = c3.f32(16)
    pg = c3.f32(16)
    gw = c3.f32(64)
    m1 = c3.f32(64)
    eq1 = c3.f32(256)
    el2 = c3.f32(256)
    m2 = c3.f32(64)
    sel = c3.f32(256)
    dd = c3.f32(256)
    ex = c3.f32(256)
    den = c3.f32(64)
    rden = c3.f32(64)
    wq = c3.f32(256)
    b_r = Buf("route")
    gl = Lg_v[:, :, 0:4]
    el4 = Lg_v[:, :, 4:20].rearrange("p t (g j) -> p t g j", g=4)

    def v3(ap, n=4):
        return ap.rearrange("p (t g) -> p t g", g=n)

    def v4(ap):
        return ap.rearrange("p (t g j) -> p t g j", g=4, j=4)

    def bc3(ap16):
        return ap16.unsqueeze(2).broadcast_to([128, T16, 4])

    def bc4(ap64):
        return ap64.rearrange("p (t g) -> p t g", g=4).unsqueeze(3).broadcast_to([128, T16, 4, 4])

    R = dict(reads=[b_r, b_Lg], writes=[b_r])
    P.op("dve", lambda g: g.tensor_reduce(out=gmax, in_=gl, axis=AX.X, op=ALU.max), **R)
    P.op("dve", lambda g: g.tensor_tensor(out=v3(ohg), in0=gl, in1=bc3(gmax), op=ALU.is_equal), **R)
    P.op("dve", lambda g: g.tensor_tensor(out=v3(ge), in0=gl, in1=bc3(gmax), op=ALU.subtract), **R)
    P.op("act", lambda g: g.activation(out=ge, in_=ge, func=AF.Exp), **R)
    P.op("dve", lambda g: g.tensor_reduce(out=gsum, in_=v3(ge), axis=AX.X, op=ALU.add), **R)
    P.op("dve", lambda g: g.reciprocal(out=pg, in_=gsum), **R)
    P.op("dve", lambda g: g.tensor_tensor(out=v3(gw), in0=v3(ohg), in1=bc3(pg), op=ALU.mult), **R)
    P.op("dve", lambda g: g.tensor_reduce(out=v3(m1), in_=el4, axis=AX.X, op=ALU.max), **R)
    P.op("dve", lambda g: g.tensor_tensor(out=v4(eq1), in0=el4, in1=bc4(m1), op=ALU.is_equal), **R)
    P.op("dve", lambda g: g.scalar_tensor_tensor(out=v4(el2), in0=v4(eq1), scalar=-1e30, in1=el4, op0=ALU.mult, op1=ALU.add), **R)
    P.op("dve", lambda g: g.tensor_reduce(out=v3(m2), in_=v4(el2), axis=AX.X, op=ALU.max), **R)
    P.op("dve", lambda g: g.tensor_tensor(out=v4(sel), in0=el4, in1=bc4(m2), op=ALU.is_ge), **R)
    P.op("dve", lambda g: g.tensor_tensor(out=v4(dd), in0=el4, in1=bc4(m1), op=ALU.subtract), **R)
    P.op("act", lambda g: g.activation(out=ex, in_=dd, func=AF.Exp), **R)
    P.op("dve", lambda g: g.tensor_tensor(out=ex, in0=ex, in1=sel, op=ALU.mult), **R)
    P.op("dve", lambda g: g.tensor_reduce(out=v3(den), in_=v4(ex), axis=AX.X, op=ALU.add), **R)
    P.op("dve", lambda g: g.reciprocal(out=rden, in_=den), **R)
    P.op("dve", lambda g: g.tensor_tensor(out=v4(wq), in0=v4(ex), in1=bc4(rden), op=ALU.mult), **R)
    b_comb = Buf("comb")
    P.op("dve", lambda g: g.tensor_tensor(out=v4(comb[:]), in0=v4(wq), in1=bc4(gw), op=ALU.mult), reads=[b_r], writes=[b_comb])
    comb_v = comb[:].rearrange("p (t e) -> p t e", e=16)

    if stop_after == "p3":
        P.barrier(); P.emit(); return nc, es
    P.barrier()
    c4 = Carver(scr2, 7200)
    hid = [c4.bf16(2048).rearrange("p (c t) -> p c t", c=4) for _ in range(2)]
    sg = [c4.f32(512) for _ in range(2)]
    b_hid = [Buf("hid0"), Buf("hid1")]
    b_sg = [Buf("sg0"), Buf("sg1")]
    WE = []
    for s in range(2):
        base = s * 12288
        WE.append((wbig[:, base:base + 4096].rearrange("p (k c) -> p k c", k=8),
                   wbig[:, base + 4096:base + 8192].rearrange("p (k c) -> p k c", k=8),
                   wbig[:, base + 8192:base + 12288].rearrange("p (k c) -> p k c", k=4)))
    b_we = [Buf("we0"), Buf("we1")]
    ds_we = [P.dsem(), P.dsem()]
    cnt4 = {"it": 0, "dn": 0}

    def moe_gu(e, blk):
        s = e % 2
        Wg_e, Wu_e, Wd_e = WE[s]
        tcols = slice(blk * 512, (blk + 1) * 512)
        bts = [b_hT[blk * 4 + t] for t in range(4)]
        hs = (e * 4 + blk) % 2
        for hc in range(4):
            pb = (cnt4["it"] % 2) * 2
            cnt4["it"] += 1
            ss_ = cnt4["it"] % 2
            for k in range(8):
                P.op("pe", lambda g, k=k, hc=hc, pb=pb: g.matmul(
                    out=banks[pb][:, 0:512], lhsT=Wg_e[:, k, hc * 128:(hc + 1) * 128], rhs=hT_all_v[:, k, tcols],
                    start=(k == 0), stop=(k == 7)), reads=bts + [b_we[s]], writes=[b_bank[pb]])
            for k in range(8):
                P.op("pe", lambda g, k=k, hc=hc, pb=pb: g.matmul(
                    out=banks[pb + 1][:, 0:512], lhsT=Wu_e[:, k, hc * 128:(hc + 1) * 128], rhs=hT_all_v[:, k, tcols],
                    start=(k == 0), stop=(k == 7)), reads=bts + [b_we[s]], writes=[b_bank[pb + 1]])
            P.op("act", lambda g, pb=pb, ss_=ss_: g.activation(out=sg[ss_], in_=banks[pb][:, 0:512], func=AF.Silu),
                 reads=[b_bank[pb]], writes=[b_sg[ss_]])
            P.op("dve", lambda g, pb=pb, ss_=ss_, hc=hc: g.tensor_tensor(out=hid[hs][:, hc, :], in0=banks[pb + 1][:, 0:512],
                                                                         in1=sg[ss_], op=ALU.mult),
                 reads=[b_bank[pb + 1], b_sg[ss_]], writes=[b_hid[hs]])

    def moe_dn(e, blk):
        s = e % 2
        Wg_e, Wu_e, Wd_e = WE[s]
        hs = (e * 4 + blk) % 2
        for t in range(4):
            ti = blk * 4 + t
            xr = xres[:, ti * D:(ti + 1) * D]
            db = 4 + (cnt4["dn"] % 2) * 2
            cnt4["dn"] += 1
            for half in range(2):
                for hc in range(4):
                    P.op("pe", lambda g, t=t, half=half, hc=hc, db=db: g.matmul(
                        out=banks[db + half][:, 0:512], lhsT=hid[hs][:, hc, t * 128:(t + 1) * 128],
                        rhs=Wd_e[:, hc, half * 512:(half + 1) * 512], start=(hc == 0), stop=(hc == 3)),
                        reads=[b_hid[hs], b_we[s]], writes=[b_bank[db + half]])
                P.op("dve", lambda g, half=half, db=db, xr=xr, ti=ti: g.scalar_tensor_tensor(
                    out=xr[:, half * 512:(half + 1) * 512], in0=banks[db + half][:, 0:512], scalar=comb_v[:, ti, e:e + 1],
                    in1=xr[:, half * 512:(half + 1) * 512], op0=ALU.mult, op1=ALU.add),
                    reads=[b_bank[db + half], b_comb], writes=[b_xr[ti]])

    prev = None
    for e in range(NE):
        s = e % 2
        Wg_e, Wu_e, Wd_e = WE[s]
        ld("pool", Wg_e, wg_d[e * D:(e + 1) * D, :].rearrange("(k p) c -> p k c", p=128), b_we[s], ds_we[s])
        ld("pool", Wu_e, wu_d[e * D:(e + 1) * D, :].rearrange("(k p) c -> p k c", p=128), b_we[s], ds_we[s])
        ld("pool", Wd_e, wd_d[e * 512:(e + 1) * 512, :].rearrange("(k p) c -> p k c", p=128), b_we[s], ds_we[s])
        for blk in range(4):
            moe_gu(e, blk)
            if prev is not None:
                moe_dn(*prev)
            prev = (e, blk)
    moe_dn(*prev)

    if stop_after == "p4":
        P.barrier(); P.emit(); return nc, es
    P.barrier()
    Wpg = wbig[:, 0:8192].rearrange("p (k c) -> p k c", k=8)
    Wpp = wbig[:, 8192:8192 + 2048].rearrange("p (k c) -> p k c", k=2)
    b_w5 = Buf("w5")
    ds_w5 = P.dsem()
    ld("pool", Wpg, wpg_d.rearrange("(k p) c -> p k c", p=128), b_w5, ds_w5)
    ld("pool", Wpp, wpp_d.rearrange("(k p) c -> p k c", p=128), b_w5, ds_w5)
    b_g5 = Buf("g5")
    ds_g5 = P.dsem()
    ld("sp", gA[:], gple_d.broadcast_to([128, D]), b_g5, ds_g5)
    ld("sp", gB[:], gfin_d.broadcast_to([128, D]), b_g5, ds_g5)
    c5 = Carver(scr2, 7200)
    pt = [c5.f32(256) for _ in range(2)]
    ptb = [c5.bf16(256) for _ in range(2)]
    pT = [c5.bf16(256).rearrange("p (k t) -> p k t", k=2) for _ in range(2)]
    h3b = [c5.bf16(1024) for _ in range(2)]
    h3T = [c5.bf16(1024).rearrange("p (k t) -> p k t", k=8) for _ in range(2)]
    sig = c5.f32(1024)
    outst = [c5.f32(1024) for _ in range(2)]
    b_pt = [Buf("pt0"), Buf("pt1")]
    ds_pt = [P.dsem(), P.dsem()]
    b_ptb, b_pT = [Buf("ptb0"), Buf("ptb1")], [Buf("pT0"), Buf("pT1")]
    b_h3b, b_h3T = [Buf("h3b0"), Buf("h3b1")], [Buf("h3T0"), Buf("h3T1")]
    b_sig = Buf("sig")
    b_out = [Buf("o0"), Buf("o1")]
    ds_out = [P.dsem(), P.dsem()]
    b_s5 = [[Buf("s5_%d_%d" % (a, b)) for b in range(4)] for a in range(2)]
    bank1b = banks[1][:, 0:512].bitcast(BF16)
    last_store = []

    def st5X(ti):
        sl = ti % 2
        xr = xres[:, ti * D:(ti + 1) * D]
        ld("sp", pt[sl], p_d[ti * 128:(ti + 1) * 128, :], b_pt[sl], ds_pt[sl])
        c = 4 + 8 * sl
        rms_stats(xr, b_xr[ti], c, c + 1, h3b[sl], b_h3b[sl], b_s5[sl][0], b_s5[sl][1])
        P.op("dve", lambda g: g.scalar_tensor_tensor(out=h3b[sl], in0=xr, scalar=small[:, c + 1:c + 2], in1=gA[:], op0=ALU.mult, op1=ALU.mult),
             reads=[b_xr[ti], b_s5[sl][1], b_g5], writes=[b_h3b[sl]])
        P.op("dve", lambda g: g.tensor_copy(out=ptb[sl], in_=pt[sl]), reads=[b_pt[sl]], writes=[b_ptb[sl]])

    def st5Y(ti):
        sl = ti % 2
        for k in range(8):
            P.op("pe", lambda g, k=k: g.transpose(out=bank0b[:, k * 128:(k + 1) * 128], in_=h3b[sl][:, k * 128:(k + 1) * 128], identity=identb[:]),
                 reads=[b_h3b[sl], b_const], writes=[b_bank[0]])
        P.op("dve", lambda g: g.tensor_copy(out=h3T[sl], in_=bank0b.rearrange("p (k t) -> p k t", k=8)), reads=[b_bank[0]], writes=[b_h3T[sl]])
        for k in range(2):
            P.op("pe", lambda g, k=k: g.transpose(out=bank1b[:, k * 128:(k + 1) * 128], in_=ptb[sl][:, k * 128:(k + 1) * 128], identity=identb[:]),
                 reads=[b_ptb[sl], b_const], writes=[b_bank[1]])
        P.op("dve", lambda g: g.tensor_copy(out=pT[sl], in_=bank1b[:, 0:256].rearrange("p (k t) -> p k t", k=2)), reads=[b_bank[1]], writes=[b_pT[sl]])

    def st5Z(ti):
        sl = ti % 2
        xr = xres[:, ti * D:(ti + 1) * D]
        for half in range(2):
            for k in range(8):
                P.op("pe", lambda g, k=k, half=half: g.matmul(out=banks[2 + half][:, 0:512], lhsT=h3T[sl][:, k, :],
                                                             rhs=Wpg[:, k, half * 512:(half + 1) * 512], start=(k == 0), stop=(k == 7)),
                     reads=[b_h3T[sl], b_w5], writes=[b_bank[2 + half]])
            for k in range(2):
                P.op("pe", lambda g, k=k, half=half: g.matmul(out=banks[4 + half][:, 0:512], lhsT=pT[sl][:, k, :],
                                                             rhs=Wpp[:, k, half * 512:(half + 1) * 512], start=(k == 0), stop=(k == 1)),
                     reads=[b_pT[sl], b_w5], writes=[b_bank[4 + half]])
            P.op("act", lambda g, half=half: g.activation(out=sig[:, half * 512:(half + 1) * 512], in_=banks[2 + half][:, 0:512], func=AF.Sigmoid),
                 reads=[b_bank[2 + half]], writes=[b_sig])
            P.op("dve", lambda g, half=half: g.tensor_tensor(out=sig[:, half * 512:(half + 1) * 512], in0=banks[4 + half][:, 0:512],
                                                             in1=sig[:, half * 512:(half + 1) * 512], op=ALU.mult),
                 reads=[b_bank[4 + half], b_sig], writes=[b_sig])
        P.op("dve", lambda g: g.tensor_tensor(out=xr, in0=sig, in1=xr, op=ALU.add), reads=[b_sig], writes=[b_xr[ti]])

    def st5W(ti):
        sl = ti % 2
        xr = xres[:, ti * D:(ti + 1) * D]
        c = 6 + 8 * sl
        rms_stats(xr, b_xr[ti], c, c + 1, outst[sl].bitcast(BF16)[:, 0:1024], b_out[sl], b_s5[sl][2], b_s5[sl][3])
        P.op("dve", lambda g: g.scalar_tensor_tensor(out=outst[sl], in0=xr, scalar=small[:, c + 1:c + 2], in1=gB[:], op0=ALU.mult, op1=ALU.mult),
             reads=[b_xr[ti], b_s5[sl][3], b_g5], writes=[b_out[sl]])
        tok = P.op("pool", lambda g: g.dma_start(out=y_d[ti * 128:(ti + 1) * 128, :], in_=outst[sl]),
                   reads=[b_out[sl]], dsem=ds_out[sl])
        last_store.append(tok)

    st5 = [st5X, st5Y, st5Z, st5W]
    for step in range(NT_OWN + len(st5) - 1):
        for k, st in enumerate(st5):
            t = step - k
            if 0 <= t < NT_OWN:
                st(t)
    P.wait_only("pool", last_store[-2:])
    P.wait_only("sp", last_store[-2:])
    P.emit()
    return nc, es


_CACHE = {}


def _consts():
    s = np.arange(128)[:, None]
    t = np.arange(128)[None, :]
    tril = np.where(s <= t, -1.0 / 16.0, 0.0).astype(np.float32)
    triu = np.where(s > t, -1.0 / 16.0, 0.0).astype(np.float32)
    mask = np.tile((s <= t).astype(np.float32), (1, 4))
    return {
        "c_ident": np.eye(128, dtype=np.float32),
        "c_tril": tril,
        "c_triu": triu,
        "c_mask": np.ascontiguousarray(mask),
        "c_ones": np.full((128, 128), 1.0 / 128.0, np.float32),
        "c_neg": np.full((128, 1), -1.0 / 16.0, np.float32),
    }


def kernel(x, p, g_mix, w_in, w_gla_gate, b_gla_gate, g_gla_out, w_conv, w_out,
           g_moe, w_group, b_group, w_router, b_router, w_exp_gate, w_exp_up, w_exp_down,
           g_ple, w_ple_gate, w_ple_proj, g_final):
    f = lambda a: np.ascontiguousarray(np.asarray(a, dtype=np.float32))
    x = f(x); p = f(p)
    if "nc" not in _CACHE:
        _CACHE["nc"] = build_program()
    nc, _es = _CACHE["nc"]
    wg_aug = np.zeros((32, 256), np.float32)
    wg_aug[0:16] = f(w_gla_gate)[0]
    wg_aug[16] = f(b_gla_gate)[0]
    shared = {
        "w_in": f(w_in)[0], "w_out": f(w_out)[0],
        "w_exp_gate": f(w_exp_gate)[0].reshape(NE * D, 512),
        "w_exp_up": f(w_exp_up)[0].reshape(NE * D, 512),
        "w_exp_down": f(w_exp_down)[0].reshape(NE * 512, D),
        "w_ple_gate": f(w_ple_gate)[0], "w_ple_proj": f(w_ple_proj)[0],
        "g_mix": f(g_mix).reshape(1, D), "g_moe": f(g_moe).reshape(1, D), "g_ple": f(g_ple).reshape(1, D),
        "g_final": f(g_final).reshape(1, D),
        "w_rt": np.ascontiguousarray(np.concatenate([f(w_group)[0], f(w_router)[0]], axis=1)),
        "b_rt": np.ascontiguousarray(np.concatenate([f(b_group)[0], f(b_router)[0]], axis=0).reshape(1, 20)),
        "wg_aug": wg_aug,
        "wconv_t": np.ascontiguousarray(f(w_conv)[0].reshape(3, 4, 128).transpose(2, 1, 0).reshape(128, 12)),
        "ggo": np.ascontiguousarray(f(g_gla_out)[0].reshape(128, 1)),
    }
    shared.update(_consts())
    in_maps = []
    for c in range(8):
        b, j = c // 4, c % 4
        xs = np.zeros((NT_ALL * 128, D), np.float32)
        n = 2048 * (j + 1)
        xs[NT_ALL * 128 - n:] = x[b, 0:n]
        m = dict(shared)
        m["xs"] = xs
        m["p_own"] = np.ascontiguousarray(p[0, b, 2048 * j:2048 * (j + 1)])
        in_maps.append(m)
    res = run_bass_kernel_spmd(nc, in_maps, core_ids=list(range(8)))
    out = np.empty((2, 8192, D), np.float32)
    for c in range(8):
        b, j = c // 4, c % 4
        out[b, 2048 * j:2048 * (j + 1)] = res.results[c]["y"]
    return out
```

```python
import numpy as np
from contextlib import ExitStack
import concourse.bass as bass
import concourse.mybir as mybir
from concourse.bass_utils import run_bass_kernel_spmd

F32 = mybir.dt.float32
BF16 = mybir.dt.bfloat16
AF = mybir.ActivationFunctionType
ALU = mybir.AluOpType
AX = mybir.AxisListType

EPS = 1e-6
NT_ALL = 64
NT_OWN = 16
NT_PRE = NT_ALL - NT_OWN
D = 1024
DIN = 3088
NE = 16


class Buf:
    __slots__ = ("name", "w", "r")

    def __init__(self, name):
        self.name = name
        self.w = None
        self.r = []


class DSem:
    def __init__(self, h):
        self.h = h
        self.count = 0
        self.nobar = False


class Prog:
    def __init__(self, nc, es):
        self.nc = nc
        self.es = es
        self.names = ["pe", "act", "dve", "pool", "sp"]
        self.streams = {k: [] for k in self.names}
        self.sem = {k: es.enter_context(nc.semaphore("c_" + k)) for k in ["pe", "act", "dve"]}
        self.cnt = {k: 0 for k in self.sem}
        self.known = {k: {} for k in self.names}
        self.handles = {}
        self.dsems = []
        self.nds = 0

    def dsem(self):
        self.nds += 1
        s = DSem(self.es.enter_context(self.nc.semaphore("d%d" % self.nds)))
        self.dsems.append(s)
        return s

    def _filter(self, e, toks):
        need = {}
        for (s, v) in toks:
            if e == "pe" and s is self.sem["pe"]:
                continue
            k = id(s)
            self.handles[k] = s
            if need.get(k, 0) < v:
                need[k] = v
        out = []
        kn = self.known[e]
        for k, v in need.items():
            if kn.get(k, 0) < v:
                kn[k] = v
                out.append((self.handles[k], v))
        return out

    def op(self, e, fn, reads=(), writes=(), dsem=None):
        toks = []
        for b in reads:
            if b.w is not None:
                toks.append(b.w)
        for b in writes:
            toks.extend(b.r)
            if b.w is not None:
                toks.append(b.w)
        waits = self._filter(e, toks)
        if dsem is None:
            self.cnt[e] += 1
            tok = (self.sem[e], self.cnt[e])
            inc = 1
        else:
            dsem.count += 16
            tok = (dsem.h, dsem.count)
            inc = 16
        self.streams[e].append((waits, fn, tok[0], inc))
        for b in writes:
            b.w = tok
            b.r = []
        for b in reads:
            b.r.append(tok)
        return tok

    def wait_only(self, e, toks):
        waits = self._filter(e, toks)
        if waits:
            self.streams[e].append((waits, None, None, 0))

    def barrier(self):
        toks = [(self.sem[k], self.cnt[k]) for k in self.sem if self.cnt[k] > 0]
        toks += [(d.h, d.count) for d in self.dsems if d.count > 0 and not d.nobar]
        for e in self.names:
            self.wait_only(e, toks)

    def emit(self):
        nc = self.nc
        with nc.Block() as block:
            decos = {"pe": block.tensor, "act": block.scalar, "dve": block.vector,
                     "pool": block.gpsimd, "sp": block.sync}
            for k in self.names:
                stream = self.streams[k]

                def body(engine, stream=stream):
                    for waits, fn, sem, inc in stream:
                        for (s, v) in waits:
                            engine.wait_ge(s, v)
                        if fn is not None:
                            ins = fn(engine)
                            ins.then_inc(sem, inc)

                decos[k](body)


def build_program(stop_after=None):
    nc = bass.Bass("TRN2", target_bir_lowering=False)
    es = ExitStack()

    def din(name, shape):
        return nc.dram_tensor(name, list(shape), F32, kind="ExternalInput").ap()

    xs_d = din("xs", [NT_ALL * 128, D])
    p_d = din("p_own", [NT_OWN * 128, 256])
    w_in_d = din("w_in", [D, DIN])
    w_out_d = din("w_out", [D, D])
    wg_d = din("w_exp_gate", [NE * D, 512])
    wu_d = din("w_exp_up", [NE * D, 512])
    wd_d = din("w_exp_down", [NE * 512, D])
    wpg_d = din("w_ple_gate", [D, D])
    wpp_d = din("w_ple_proj", [256, D])
    gmix_d = din("g_mix", [1, D])
    gmoe_d = din("g_moe", [1, D])
    gple_d = din("g_ple", [1, D])
    gfin_d = din("g_final", [1, D])
    wr_d = din("w_rt", [D, 20])
    br_d = din("b_rt", [1, 20])
    wgate_d = din("wg_aug", [32, 256])
    wconv_d = din("wconv_t", [128, 12])
    ggo_d = din("ggo", [128, 1])
    cid_d = din("c_ident", [128, 128])
    ctl_d = din("c_tril", [128, 128])
    ctu_d = din("c_triu", [128, 128])
    cmask_d = din("c_mask", [128, 512])
    cones_d = din("c_ones", [128, 128])
    cneg_d = din("c_neg", [128, 1])
    y_d = nc.dram_tensor("y", [NT_OWN * 128, D], F32, kind="ExternalOutput").ap()

    def sb(name, shape, dt):
        return es.enter_context(nc.sbuf_tensor("s_" + name, list(shape), dt))

    xres = sb("xres", [128, NT_OWN * D], F32)
    hT_all = sb("hT_all", [128, 8 * 2048], BF16)
    y1_all = sb("y1_all", [128, 4 * 2048], BF16)
    wbig = sb("wbig", [128, 24704], BF16)
    scr2 = sb("scr2", [128, 7200], F32)
    carry_t = sb("carry", [128, 8], F32)
    identb = sb("identb", [128, 128], BF16)
    identf = sb("identf", [128, 128], F32)
    tril = sb("tril", [128, 128], F32)
    triu = sb("triu", [128, 128], F32)
    maskb = sb("maskb", [128, 512], BF16)
    onesb = sb("onesb", [128, 128], BF16)
    negcol = sb("negcol", [128, 1], F32)
    gA = sb("gA", [128, D], F32)
    gB = sb("gB", [128, D], F32)
    wr = sb("wr", [128, 8 * 20], F32)
    brb = sb("brb", [128, 20], F32)
    wgate = sb("wgate", [32, 256], F32)
    wconv = sb("wconv", [128, 12], F32)
    ggo = sb("ggo", [128, 1], F32)
    Lg = sb("Lg", [128, NT_OWN * 20], F32)
    comb = sb("comb", [128, NT_OWN * 16], F32)
    small = sb("small", [128, 16], F32)

    banks = [es.enter_context(nc.psum_tensor("bank%d" % i, [128, 512], F32)) for i in range(8)]

    P = Prog(nc, es)

    class Carver:
        def __init__(self, base, nwords):
            self.base = base
            self.off = 0
            self.n = nwords

        def f32(self, n):
            a = self.base[:, self.off:self.off + n]
            self.off += n
            assert self.off <= self.n, (self.off, self.n)
            return a

        def bf16(self, n):
            assert n % 2 == 0
            return self.f32(n // 2).bitcast(BF16)

    b_const = Buf("const")
    ds_const = P.dsem()

    def ld(e, out_ap, in_ap, buf, dsem):
        bufs = buf if isinstance(buf, (list, tuple)) else [buf]
        return P.op(e, lambda g: g.dma_start(out=out_ap, in_=in_ap), writes=list(bufs), dsem=dsem)

    ld("sp", identf[:], cid_d, b_const, ds_const)
    ld("sp", tril[:], ctl_d, b_const, ds_const)
    ld("sp", triu[:], ctu_d, b_const, ds_const)
    ld("sp", negcol[:], cneg_d, b_const, ds_const)
    ld("sp", gA[:], gmix_d.broadcast_to([128, D]), b_const, ds_const)
    ld("sp", wr[:].rearrange("p (k n) -> p k n", k=8), wr_d.rearrange("(k p) n -> p k n", p=128), b_const, ds_const)
    ld("sp", brb[:], br_d.broadcast_to([128, 20]), b_const, ds_const)
    ld("sp", wgate[:], wgate_d, b_const, ds_const)
    ld("sp", wconv[:], wconv_d, b_const, ds_const)
    ld("sp", ggo[:], ggo_d, b_const, ds_const)
    ds_const2 = P.dsem()
    ld("pool", identb[:], cid_d, b_const, ds_const2)
    ld("pool", maskb[:], cmask_d, b_const, ds_const2)
    ld("pool", onesb[:], cones_d, b_const, ds_const2)

    c1 = Carver(xres, NT_OWN * D)
    Wqkva = c1.bf16(8 * 1296).rearrange("p (k c) -> p k c", k=8)
    xs_t = [c1.f32(1024) for _ in range(2)]
    hb_t = [c1.bf16(1024) for _ in range(2)]
    hts_off = c1.off
    hTs = [c1.bf16(1024).rearrange("p (k t) -> p k t", k=8) for _ in range(4)]
    aT_t = [c1.f32(128) for _ in range(2)]
    e1 = c1.f32(256)
    sp_t = [c1.f32(256) for _ in range(2)]
    Er = c1.f32(256)
    El_t = [c1.f32(8) for _ in range(2)]
    kd_t = [c1.bf16(256) for _ in range(2)]
    vb_t = [c1.bf16(512) for _ in range(2)]
    Eq = c1.f32(256)
    Ek = c1.f32(256)
    qt_t = [c1.bf16(256) for _ in range(2)]
    kt_t = [c1.bf16(256) for _ in range(2)]
    qkT0 = c1.bf16(1024)
    Pm = c1.bf16(512)
    S = c1.f32(512)
    Sb0 = c1.bf16(512)
    sq0 = c1.bf16(512)
    rstd = c1.f32(512)
    c1o = Carver(xres, hts_off + 1536)
    c1o.off = hts_off
    vb4 = [vb_t[0], vb_t[1], c1o.bf16(512), c1o.bf16(512)]
    qkT_t = [qkT0, c1o.bf16(1024)]
    Sb_t = [Sb0, c1o.bf16(512)]
    sq_t = [sq0, c1o.bf16(512)]

    w_in_v = w_in_d.rearrange("(k p) c -> p k c", p=128)
    b_wqkva = Buf("wqkva")
    ds_w1 = P.dsem()
    ld("pool", Wqkva[:, :, 0:1024], w_in_v[:, :, 0:1024], b_wqkva, ds_w1)
    ld("pool", Wqkva[:, :, 1024:1040], w_in_v[:, :, 1536:1552], b_wqkva, ds_w1)

    Wgbcu = wbig[:, 0:8 * 2048].rearrange("p (k c) -> p k c", k=8)
    Wout = wbig[:, 8 * 2048:8 * 3072].rearrange("p (k c) -> p k c", k=8)
    b_wg = Buf("wgbcu")
    b_wo = Buf("wout")
    ds_w2 = P.dsem()
    ds_wo = P.dsem()
    ld("pool", Wgbcu[:, :, 0:512], w_in_v[:, :, 1024:1536], b_wg, ds_w2)
    ld("pool", Wgbcu[:, :, 512:2048], w_in_v[:, :, 1552:3088], b_wg, ds_w2)
    ld("pool", Wout, w_out_d.rearrange("(k p) c -> p k c", p=128), b_wo, ds_wo)
    WE = []
    for s_ in range(2):
        base = s_ * 12288
        WE.append((wbig[:, base:base + 4096].rearrange("p (k c) -> p k c", k=8),
                   wbig[:, base + 4096:base + 8192].rearrange("p (k c) -> p k c", k=8),
                   wbig[:, base + 8192:base + 12288].rearrange("p (k c) -> p k c", k=4)))
    b_we = [Buf("we0"), Buf("we1")]
    ds_we = [P.dsem(), P.dsem()]
    ds_we[0].nobar = True
    ds_we[1].nobar = True
    Wpg = wbig[:, 0:8192].rearrange("p (k c) -> p k c", k=8)
    Wpp = wbig[:, 8192:8192 + 2048].rearrange("p (k c) -> p k c", k=2)
    b_w5 = Buf("w5")
    ds_w5 = P.dsem()
    ds_w5.nobar = True

    def load_expert(e, extra=()):
        s_ = e % 2
        Wg_e, Wu_e, Wd_e = WE[s_]
        bufs = [b_we[s_]] + list(extra)
        ld("pool", Wg_e, wg_d[e * D:(e + 1) * D, :].rearrange("(k p) c -> p k c", p=128), bufs, ds_we[s_])
        ld("pool", Wu_e, wu_d[e * D:(e + 1) * D, :].rearrange("(k p) c -> p k c", p=128), bufs, ds_we[s_])
        ld("pool", Wd_e, wd_d[e * 512:(e + 1) * 512, :].rearrange("(k p) c -> p k c", p=128), bufs, ds_we[s_])

    if stop_after == "consts":
        P.barrier(); P.emit(); return nc, es
    import os
    b_xs = [Buf("xs0"), Buf("xs1")]
    ds_xs = [P.dsem(), P.dsem()]
    b_hb = [Buf("hb0"), Buf("hb1")]
    b_hTs = [Buf("hTs%d" % i) for i in range(4)]
    b_hT = [Buf("hT%d" % i) for i in range(NT_OWN)]
    b_y1 = [Buf("y1_%d" % i) for i in range(NT_OWN)]
    b_bank = [Buf("bank%d" % i) for i in range(8)]
    b_aTp = Buf("aTp")
    b_lastp = Buf("lastp")
    b_zp = Buf("zp")
    b_aT = [Buf("aT0"), Buf("aT1")]
    b_e1 = Buf("e1")
    b_sp = [Buf("sp0"), Buf("sp1")]
    b_Eq, b_Ek, b_Er = Buf("Eq"), Buf("Ek"), Buf("Er")
    b_El = [Buf("El0"), Buf("El1")]
    b_kd = [Buf("kd0"), Buf("kd1")]
    b_vb = [Buf("vb0"), Buf("vb1")]
    b_qt, b_kt = [Buf("qt0"), Buf("qt1")], [Buf("kt0"), Buf("kt1")]
    b_Pm, b_S, b_rstd = Buf("Pm"), Buf("S"), Buf("rstd")
    b_qkT, b_Sb, b_sq = [Buf("qkT0"), Buf("qkT1")], [Buf("Sb0"), Buf("Sb1")], [Buf("sq0"), Buf("sq1")]
    b_vb4 = [b_vb[0], b_vb[1], Buf("vb2"), Buf("vb3")]

    def vb_of(i):
        if i >= NT_PRE:
            return vb4[i % 4], b_vb4[i % 4]
        return vb_t[i % 2], b_vb[i % 2]
    b_ss = [Buf("ss0"), Buf("ss1")]
    b_rs = [Buf("rs0"), Buf("rs1")]

    bank0b = banks[0][:, 0:512].bitcast(BF16)
    bank7b = banks[7][:, 0:512].bitcast(BF16)

    hT_all_v = hT_all[:].rearrange("p (k t) -> p k t", k=8)
    y1_all_v = y1_all[:].rearrange("p (h t) -> p h t", h=4)
    S_v = S.rearrange("p (h e) -> p h e", h=4)

    P.op("dve", lambda g: g.memset(S[0:64, :], 0.0), writes=[b_S])
    for a_ in range(2):
        P.op("dve", lambda g, a_=a_: g.memset(aT_t[a_][0:32, :], 1.0), writes=[b_aT[a_]])

    def rms_stats(src_ap, b_src, ss_col, rs_col, junk, b_junk, bss=None, brs=None):
        bss = bss or b_ss[0]
        brs = brs or b_rs[0]
        P.op("act", lambda g: g.activation(out=junk, in_=src_ap, func=AF.Square, accum_out=small[:, ss_col:ss_col + 1]),
             reads=[b_src], writes=[b_junk, bss])
        P.op("act", lambda g: g.activation(out=small[:, rs_col:rs_col + 1], in_=small[:, ss_col:ss_col + 1], func=AF.Ln,
                                           scale=1.0 / D, bias=EPS),
             reads=[bss], writes=[brs])
        P.op("act", lambda g: g.activation(out=small[:, rs_col:rs_col + 1], in_=small[:, rs_col:rs_col + 1], func=AF.Exp, scale=-0.5),
             reads=[brs], writes=[brs])

    def hT_of(i):
        if i >= NT_PRE:
            oi = i - NT_PRE
            return hT_all_v[:, :, oi * 128:(oi + 1) * 128], b_hT[oi]
        return hTs[i % 4], b_hTs[i % 4]

    def stageA(i):
        sl = i % 2
        xt = xs_t[sl]
        ld("sp", xt, xs_d[i * 128:(i + 1) * 128, :], b_xs[sl], ds_xs[sl])
        hb = hb_t[sl]
        rms_stats(xt, b_xs[sl], 8 * sl, 8 * sl + 1, hb, b_hb[sl], b_ss[sl], b_rs[sl])
        P.op("dve", lambda g: g.scalar_tensor_tensor(out=hb, in0=xt, scalar=small[:, 8 * sl + 1:8 * sl + 2], in1=gA[:],
                                                     op0=ALU.mult, op1=ALU.mult),
             reads=[b_xs[sl], b_rs[sl], b_const], writes=[b_hb[sl]])

    def stageB(i):
        sl = i % 2
        hb = hb_t[sl]
        hT, bh = hT_of(i)
        for k in range(8):
            P.op("pe", lambda g, k=k: g.transpose(out=bank0b[:, k * 128:(k + 1) * 128], in_=hb[:, k * 128:(k + 1) * 128],
                                                 identity=identb[:]),
                 reads=[b_hb[sl], b_const], writes=[b_bank[0]])
        P.op("act", lambda g: g.copy(out=hT, in_=bank0b.rearrange("p (k t) -> p k t", k=8)),
             reads=[b_bank[0]], writes=[bh])

    def stageC(i):
        sl = i % 2
        hT, bh = hT_of(i)
        for k in range(8):
            P.op("pe", lambda g, k=k: g.matmul(out=banks[1][0:16, 0:128], lhsT=Wqkva[:, k, 1024:1040], rhs=hT[:, k, :],
                                              start=(k == 0), stop=(k == 7)),
                 reads=[bh, b_wqkva], writes=[b_bank[1]])
        P.op("act", lambda g: g.copy(out=aT_t[sl][0:16, :], in_=banks[1][0:16, 0:128]), reads=[b_bank[1]], writes=[b_aT[sl]])

    def stageD(i):
        sl = i % 2
        P.op("pe", lambda g: g.matmul(out=banks[2][:, 256:512], lhsT=aT_t[sl][0:17, :], rhs=wgate[0:17, :], start=True, stop=True),
             reads=[b_aT[sl], b_const], writes=[b_bank[2]])
        P.op("act", lambda g: g.activation(out=e1, in_=banks[2][:, 256:512], func=AF.Exp, scale=-1.0), reads=[b_bank[2]], writes=[b_e1])
        P.op("act", lambda g: g.activation(out=sp_t[sl], in_=e1, func=AF.Ln, bias=1.0), reads=[b_e1], writes=[b_sp[sl]])

    def stageE(i):
        own = i >= NT_PRE
        sl = i % 2
        hT, bh = hT_of(i)
        spb = sp_t[sl]
        if own:
            P.op("pe", lambda g: g.matmul(out=banks[3][:, 0:256], lhsT=tril[:], rhs=spb, start=True, stop=True),
                 reads=[b_sp[sl], b_const], writes=[b_bank[3]])
        P.op("pe", lambda g: g.matmul(out=banks[3][:, 256:512], lhsT=triu[:], rhs=spb, start=True, stop=True),
             reads=[b_sp[sl], b_const], writes=[b_bank[3]])
        for h in range(4):
            P.op("pe", lambda g, h=h: g.matmul(out=banks[7][0:64, 128 + h:129 + h], lhsT=spb[:, h * 64:(h + 1) * 64], rhs=negcol[:],
                                              start=True, stop=True),
                 reads=[b_sp[sl], b_const], writes=[b_bank[7]])
        if own:
            ncol, c0 = 512, 0
        else:
            ncol, c0 = 256, 256
        for k in range(8):
            P.op("pe", lambda g, k=k: g.matmul(out=banks[4][:, 0:ncol], lhsT=hT[:, k, :], rhs=Wqkva[:, k, c0:c0 + ncol],
                                              start=(k == 0), stop=(k == 7)),
                 reads=[bh, b_wqkva], writes=[b_bank[4]])
        for k in range(8):
            P.op("pe", lambda g, k=k: g.matmul(out=banks[5][:, 0:512], lhsT=hT[:, k, :], rhs=Wqkva[:, k, 512:1024],
                                              start=(k == 0), stop=(k == 7)),
                 reads=[bh, b_wqkva], writes=[b_bank[5]])
        P.op("act", lambda g: g.activation(out=Er, in_=banks[3][:, 256:512], func=AF.Exp), reads=[b_bank[3]], writes=[b_Er])
        if own:
            P.op("act", lambda g: g.activation(out=Eq, in_=banks[3][:, 0:256], func=AF.Exp), reads=[b_bank[3]], writes=[b_Eq])
            P.op("act", lambda g: g.activation(out=Ek, in_=banks[3][:, 0:256], func=AF.Exp, scale=-1.0), reads=[b_bank[3]], writes=[b_Ek])
        P.op("act", lambda g: g.activation(out=El_t[sl][0:64, 0:4], in_=banks[7][0:64, 128:132], func=AF.Exp),
             reads=[b_bank[7]], writes=[b_El[sl]])
        kcol = 256 if own else 0
        P.op("dve", lambda g: g.tensor_tensor(out=kd_t[sl], in0=banks[4][:, kcol:kcol + 256], in1=Er, op=ALU.mult),
             reads=[b_bank[4], b_Er], writes=[b_kd[sl]])
        if own:
            P.op("dve", lambda g: g.scalar_tensor_tensor(out=qt_t[sl], in0=banks[4][:, 0:256], scalar=0.125, in1=Eq, op0=ALU.mult, op1=ALU.mult),
                 reads=[b_bank[4], b_Eq], writes=[b_qt[sl]])
            P.op("dve", lambda g: g.tensor_tensor(out=kt_t[sl], in0=banks[4][:, 256:512], in1=Ek, op=ALU.mult),
                 reads=[b_bank[4], b_Ek], writes=[b_kt[sl]])
        vb, bvb = vb_of(i)
        P.op("dve", lambda g: g.tensor_copy(out=vb, in_=banks[5][:, 0:512]), reads=[b_bank[5]], writes=[bvb])

    def stageF(i):
        sl = i % 2
        kd = kd_t[sl]
        vb, bvb = vb_of(i)
        for h in range(4):
            P.op("pe", lambda g, h=h: g.matmul(out=banks[6][0:64, h * 128:(h + 1) * 128], lhsT=kd[:, h * 64:(h + 1) * 64],
                                              rhs=vb[:, h * 128:(h + 1) * 128], start=True, stop=True),
                 reads=[b_kd[sl], bvb], writes=[b_bank[6]])

    def stageS(i):
        sl = i % 2
        P.op("dve", lambda g: g.tensor_tensor(out=S_v[0:64, :, :], in0=S_v[0:64, :, :],
                                              in1=El_t[sl][0:64, 0:4].unsqueeze(2).broadcast_to([64, 4, 128]), op=ALU.mult),
             reads=[b_S, b_El[sl]], writes=[b_S])
        P.op("dve", lambda g: g.tensor_tensor(out=S[0:64, :], in0=S[0:64, :], in1=banks[6][0:64, 0:512], op=ALU.add),
             reads=[b_S, b_bank[6]], writes=[b_S])

    def stageFo(i):
        sl = i % 2
        stageF(i)
        P.op("act", lambda g: g.copy(out=Sb_t[sl][0:64, :], in_=S[0:64, :]), reads=[b_S], writes=[b_Sb[sl]])
        stageS(i)
        for j in range(8):
            src = qt_t[sl] if j < 4 else kt_t[sl]
            a = j % 4
            P.op("pe", lambda g, j=j, src=src, a=a: g.transpose(out=bank7b[0:64, j * 128:(j + 1) * 128], in_=src[:, a * 64:(a + 1) * 64],
                                                               identity=identb[:]),
                 reads=[b_qt[sl], b_kt[sl], b_const], writes=[b_bank[7]])
        P.op("act", lambda g: g.copy(out=qkT_t[sl][0:64, :], in_=bank7b[0:64, :]), reads=[b_bank[7]], writes=[b_qkT[sl]])

    def stageHI(i):
        sl = i % 2
        vb, bvb = vb_of(i)
        qkT = qkT_t[sl]
        Sb_v = Sb_t[sl].rearrange("p (h e) -> p h e", h=4)
        for h in range(4):
            P.op("pe", lambda g, h=h: g.matmul(out=banks[2][:, h * 128:(h + 1) * 128], lhsT=qkT[0:64, (4 + h) * 128:(5 + h) * 128],
                                              rhs=qkT[0:64, h * 128:(h + 1) * 128], start=True, stop=True),
                 reads=[b_qkT[sl]], writes=[b_bank[2]])
        P.op("dve", lambda g: g.tensor_tensor(out=Pm, in0=banks[2][:, 0:512], in1=maskb[:], op=ALU.mult),
             reads=[b_bank[2], b_const], writes=[b_Pm])
        for h in range(4):
            P.op("pe", lambda g, h=h: g.matmul(out=banks[7][:, h * 128:(h + 1) * 128], lhsT=vb[:, h * 128:(h + 1) * 128],
                                              rhs=Pm[:, h * 128:(h + 1) * 128], start=True, stop=False),
                 reads=[bvb, b_Pm], writes=[b_bank[7]])
            P.op("pe", lambda g, h=h: g.matmul(out=banks[7][:, h * 128:(h + 1) * 128], lhsT=Sb_v[0:64, h, :],
                                              rhs=qkT[0:64, h * 128:(h + 1) * 128], start=False, stop=True),
                 reads=[b_Sb[sl], b_qkT[sl]], writes=[b_bank[7]])
        P.op("act", lambda g: g.activation(out=sq_t[sl], in_=banks[7][:, 0:512], func=AF.Square), reads=[b_bank[7]], writes=[b_sq[sl]])

    def stageJ(i):
        oi = i - NT_PRE
        sl = i % 2
        P.op("pe", lambda g: g.matmul(out=banks[2][:, 0:512], lhsT=onesb[:], rhs=sq_t[sl], start=True, stop=True),
             reads=[b_sq[sl], b_const], writes=[b_bank[2]])
        P.op("act", lambda g: g.activation(out=rstd, in_=banks[2][:, 0:512], func=AF.Ln, bias=EPS), reads=[b_bank[2]], writes=[b_rstd])
        P.op("act", lambda g: g.activation(out=rstd, in_=rstd, func=AF.Exp, scale=-0.5), reads=[b_rstd], writes=[b_rstd])
        P.op("dve", lambda g: g.tensor_tensor(out=y1_all_v[:, :, oi * 128:(oi + 1) * 128],
                                              in0=banks[7][:, 0:512].rearrange("p (h t) -> p h t", h=4),
                                              in1=rstd.rearrange("p (h t) -> p h t", h=4), op=ALU.mult),
             reads=[b_bank[7], b_rstd], writes=[b_y1[oi]])

    T0 = int(os.environ.get('K_T0', 0))
    T1 = int(os.environ.get('K_T1', NT_ALL))
    pre_stages = [stageA, stageB, stageC, stageD, stageE, lambda i: (stageF(i), stageS(i))]
    npre = min(T1, NT_PRE)
    for step in range(T0, npre + len(pre_stages) - 1):
        for k, st in enumerate(pre_stages):
            t = step - k
            if T0 <= t < npre:
                st(t)
    b_carry = Buf("carry")
    carry_v = carry_t[:].rearrange("p (c j) -> p c j", c=4)
    hT47, b_h47 = hT_of(NT_PRE - 1)
    for cc in range(4):
        for k in range(8):
            P.op("pe", lambda g, cc=cc, k=k: g.matmul(out=banks[2][:, 0:2], lhsT=Wgbcu[:, k, 1024 + cc * 128:1024 + (cc + 1) * 128],
                                                     rhs=hT47[:, k, 126:128], start=(k == 0), stop=(k == 7)),
                 reads=[b_h47, b_wg], writes=[b_bank[2]])
        for k in range(8):
            P.op("pe", lambda g, cc=cc, k=k: g.matmul(out=banks[3][:, 0:2], lhsT=Wgbcu[:, k, 1536 + cc * 128:1536 + (cc + 1) * 128],
                                                     rhs=hT47[:, k, 126:128], start=(k == 0), stop=(k == 7)),
                 reads=[b_h47, b_wg], writes=[b_bank[3]])
        P.op("act", lambda g: g.copy(out=small[:, 4:6], in_=banks[2][:, 0:2]), reads=[b_bank[2]], writes=[b_ss[0]])
        P.op("dve", lambda g, cc=cc: g.tensor_tensor(out=carry_v[:, cc, :], in0=banks[3][:, 0:2], in1=small[:, 4:6], op=ALU.mult),
             reads=[b_bank[3], b_ss[0]], writes=[b_carry])
    P.barrier()
    own_stages = [(stageJ, 7), (stageA, 0), (stageB, 1), (stageC, 2), (stageD, 3), (stageE, 4), (stageFo, 5), (stageHI, 6)]
    o0 = max(T0, NT_PRE)
    for step in range(o0, T1 + 7):
        for st, k in own_stages:
            t = step - k
            if o0 <= t < T1:
                st(t)

    if stop_after in ("p1", "p1s"):
        P.barrier(); P.emit(); return nc, es
    P.barrier()
    b_xr = [Buf("xr%d" % i) for i in range(NT_OWN)]
    for t in range(NT_OWN):
        ld("sp", xres[:, t * D:(t + 1) * D], xs_d[(NT_PRE + t) * 128:(NT_PRE + t + 1) * 128, :], b_xr[t], P.dsem())
    b_gB = Buf("gB")
    ds_gB = P.dsem()
    ld("sp", gB[:], gmoe_d.broadcast_to([128, D]), b_gB, ds_gB)

    c2 = Carver(scr2, 7200)
    gs = c2.f32(512)
    Ct = c2.f32(512)
    cu = c2.f32(520)
    acc = c2.f32(512)
    yg = c2.bf16(2048).rearrange("p (c t) -> p c t", c=4)
    ycv = c2.bf16(2048).rearrange("p (c t) -> p c t", c=4)
    h2_t = [c2.f32(1024) for _ in range(2)]
    h2Tf = c2.f32(1024).rearrange("p (k t) -> p k t", k=8)
    b_gs, b_Ct, b_cu, b_acc = Buf("gs"), Buf("Ct"), Buf("cu"), Buf("acc")
    b_yg, b_ycv, b_h2Tf, b_Lg = Buf("yg"), Buf("ycv"), Buf("h2Tf"), Buf("Lg")
    b_h2 = [Buf("h2a"), Buf("h2b")]

    Lg_v = Lg[:].rearrange("p (t n) -> p t n", t=NT_OWN)
    L2 = int(os.environ.get('K_L2', 99))
    b_s2 = [[Buf("s2_%d_%d" % (a, b)) for b in range(2)] for a in range(2)]
    for blk in range(4 if L2 >= 1 else 0):
        tcols = slice(blk * 512, (blk + 1) * 512)
        bts = [b_hT[blk * 4 + t] for t in range(4)]
        for cc in range(4):
            bb = 4 * (cc % 2)
            for k in range(8):
                P.op("pe", lambda g, cc=cc, k=k, bb=bb, tcols=tcols: g.matmul(out=banks[bb][:, 0:512], lhsT=Wgbcu[:, k, cc * 128:(cc + 1) * 128],
                                                                rhs=hT_all_v[:, k, tcols], start=(k == 0), stop=(k == 7)),
                     reads=bts + [b_wg], writes=[b_bank[bb]])
            P.op("act", lambda g, bb=bb: g.activation(out=gs, in_=banks[bb][:, 0:512], func=AF.Silu), reads=[b_bank[bb]], writes=[b_gs])
            P.op("dve", lambda g, cc=cc, tcols=tcols: g.scalar_tensor_tensor(out=yg[:, cc, :], in0=y1_all_v[:, cc, tcols], scalar=ggo[:, 0:1],
                                                                in1=gs, op0=ALU.mult, op1=ALU.mult),
                 reads=[b_gs, b_const] + [b_y1[blk * 4 + t] for t in range(4)], writes=[b_yg])
            for (bk, off) in ((bb + 1, 512), (bb + 2, 1024), (bb + 3, 1536)):
                for k in range(8):
                    P.op("pe", lambda g, cc=cc, k=k, bk=bk, off=off, tcols=tcols: g.matmul(
                        out=banks[bk][:, 0:512], lhsT=Wgbcu[:, k, off + cc * 128:off + (cc + 1) * 128],
                        rhs=hT_all_v[:, k, tcols], start=(k == 0), stop=(k == 7)),
                        reads=bts + [b_wg], writes=[b_bank[bk]])
            P.op("act", lambda g, bb=bb: g.copy(out=Ct, in_=banks[bb + 2][:, 0:512]), reads=[b_bank[bb + 2]], writes=[b_Ct])
            P.op("act", lambda g, cc=cc: g.copy(out=cu[:, 0:2], in_=carry_v[:, cc, :]), reads=[b_carry], writes=[b_cu])
            P.op("dve", lambda g, bb=bb: g.tensor_tensor(out=cu[:, 2:514], in0=banks[bb + 3][:, 0:512], in1=Ct, op=ALU.mult),
                 reads=[b_bank[bb + 3], b_Ct], writes=[b_cu])
            P.op("act", lambda g, cc=cc: g.copy(out=carry_v[:, cc, :], in_=cu[:, 512:514]), reads=[b_cu], writes=[b_carry])
            P.op("dve", lambda g, cc=cc: g.tensor_scalar(out=acc, in0=cu[:, 2:514], scalar1=wconv[:, cc * 3 + 2:cc * 3 + 3], scalar2=None,
                                                         op0=ALU.mult),
                 reads=[b_cu, b_const], writes=[b_acc])
            P.op("dve", lambda g, cc=cc: g.scalar_tensor_tensor(out=acc, in0=cu[:, 1:513], scalar=wconv[:, cc * 3 + 1:cc * 3 + 2], in1=acc,
                                                                op0=ALU.mult, op1=ALU.add),
                 reads=[b_cu, b_const], writes=[b_acc])
            P.op("dve", lambda g, cc=cc: g.scalar_tensor_tensor(out=acc, in0=cu[:, 0:512], scalar=wconv[:, cc * 3:cc * 3 + 1], in1=acc,
                                                                op0=ALU.mult, op1=ALU.add),
                 reads=[b_cu, b_const], writes=[b_acc])
            P.op("dve", lambda g, cc=cc, bb=bb: g.tensor_tensor(out=ycv[:, cc, :], in0=banks[bb + 1][:, 0:512], in1=acc, op=ALU.mult),
                 reads=[b_bank[bb + 1], b_acc], writes=[b_ycv])

        if blk == 3:
            load_expert(0, extra=[b_wg])

        def st2P(t):
            ti = blk * 4 + t
            sl = t % 2
            xr = xres[:, ti * D:(ti + 1) * D]
            for half in range(2):
                for kc in range(8):
                    src = yg if kc < 4 else ycv
                    bsrc = b_yg if kc < 4 else b_ycv
                    P.op("pe", lambda g, half=half, kc=kc, src=src: g.matmul(
                        out=banks[half][:, 0:512], lhsT=src[:, kc % 4, t * 128:(t + 1) * 128],
                        rhs=Wout[:, kc, half * 512:(half + 1) * 512], start=(kc == 0), stop=(kc == 7)),
                        reads=[bsrc, b_wo], writes=[b_bank[half]])
                P.op("dve", lambda g, half=half: g.tensor_tensor(out=xr[:, half * 512:(half + 1) * 512], in0=banks[half][:, 0:512],
                                                                 in1=xr[:, half * 512:(half + 1) * 512], op=ALU.add),
                     reads=[b_bank[half]], writes=[b_xr[ti]])
            c = 2 + 8 * sl
            rms_stats(xr, b_xr[ti], c, c + 1, h2_t[sl].bitcast(BF16)[:, 0:1024], b_h2[sl], b_s2[sl][0], b_s2[sl][1])
            P.op("dve", lambda g: g.scalar_tensor_tensor(out=h2_t[sl], in0=xr, scalar=small[:, c + 1:c + 2], in1=gB[:], op0=ALU.mult, op1=ALU.mult),
                 reads=[b_xr[ti], b_s2[sl][1], b_gB], writes=[b_h2[sl]])

        def st2Q(t):
            ti = blk * 4 + t
            sl = t % 2
            h2 = h2_t[sl]
            for k in range(8):
                bk = 2 + k // 4
                P.op("pe", lambda g, k=k, bk=bk: g.matmul(out=banks[bk][:, (k % 4) * 128:(k % 4 + 1) * 128], lhsT=h2[:, k * 128:(k + 1) * 128],
                                                         rhs=identf[:], start=True, stop=True),
                     reads=[b_h2[sl], b_const], writes=[b_bank[bk]])
            for hh in range(2):
                P.op("dve", lambda g, hh=hh: g.tensor_copy(out=h2Tf[:, hh * 4:(hh + 1) * 4, :],
                                                           in_=banks[2 + hh][:, 0:512].rearrange("p (k t) -> p k t", k=4)),
                     reads=[b_bank[2 + hh]], writes=[b_h2Tf])
            P.op("act", lambda g: g.copy(out=hT_all_v[:, :, ti * 128:(ti + 1) * 128], in_=h2Tf),
                 reads=[b_h2Tf], writes=[b_hT[ti]])

        def st2R(t):
            ti = blk * 4 + t
            for k in range(8):
                P.op("pe", lambda g, k=k: g.matmul(out=banks[4][:, 0:20], lhsT=h2Tf[:, k, :], rhs=wr[:, k * 20:(k + 1) * 20],
                                                  start=(k == 0), stop=(k == 7)),
                     reads=[b_h2Tf, b_const], writes=[b_bank[4]])
            P.op("dve", lambda g: g.tensor_tensor(out=Lg_v[:, ti, :], in0=banks[4][:, 0:20], in1=brb[:], op=ALU.add),
                 reads=[b_bank[4], b_const], writes=[b_Lg])

        for step in range(6):
            if step < 4:
                st2P(step)
            if 2 <= step:
                st2R(step - 2)
            if 1 <= step < 5:
                st2Q(step - 1)

    if stop_after == "p2":
        P.barrier(); P.emit(); return nc, es
    P.barrier()
    b_g5 = Buf("g5")
    ds_g5 = P.dsem()
    ld("sp", gA[:], gple_d.broadcast_to([128, D]), b_g5, ds_g5)
    ld("sp", gB[:], gfin_d.broadcast_to([128, D]), b_g5, ds_g5)
    c3 = Carver(scr2, 7200)
    T16 = NT_OWN
    gmax = c3.f32(16)
    ohg = c3.f32(64)
    ge = c3.f32(64)
    gsum = c3.f32(16)
    pg = c3.f32(16)
    gw = c3.f32(64)
    m1 = c3.f32(64)
    eq1 = c3.f32(256)
    el2 = c3.f32(256)
    m2 = c3.f32(64)
    sel = c3.f32(256)
    dd = c3.f32(256)
    ex = c3.f32(256)
    den = c3.f32(64)
    rden = c3.f32(64)
    wq = c3.f32(256)
    b_r = Buf("route")
    gl = Lg_v[:, :, 0:4]
    el4 = Lg_v[:, :, 4:20].rearrange("p t (g j) -> p t g j", g=4)

    def v3(ap, n=4):
        return ap.rearrange("p (t g) -> p t g", g=n)

    def v4(ap):
        return ap.rearrange("p (t g j) -> p t g j", g=4, j=4)

    def bc3(ap16):
        return ap16.unsqueeze(2).broadcast_to([128, T16, 4])

    def bc4(ap64):
        return ap64.rearrange("p (t g) -> p t g", g=4).unsqueeze(3).broadcast_to([128, T16, 4, 4])

    R = dict(reads=[b_r, b_Lg], writes=[b_r])
    P.op("dve", lambda g: g.tensor_reduce(out=gmax, in_=gl, axis=AX.X, op=ALU.max), **R)
    P.op("dve", lambda g: g.tensor_tensor(out=v3(ohg), in0=gl, in1=bc3(gmax), op=ALU.is_equal), **R)
    P.op("dve", lambda g: g.tensor_tensor(out=v3(ge), in0=gl, in1=bc3(gmax), op=ALU.subtract), **R)
    P.op("act", lambda g: g.activation(out=ge, in_=ge, func=AF.Exp), **R)
    P.op("dve", lambda g: g.tensor_reduce(out=gsum, in_=v3(ge), axis=AX.X, op=ALU.add), **R)
    P.op("dve", lambda g: g.reciprocal(out=pg, in_=gsum), **R)
    P.op("dve", lambda g: g.tensor_tensor(out=v3(gw), in0=v3(ohg), in1=bc3(pg), op=ALU.mult), **R)
    P.op("dve", lambda g: g.tensor_reduce(out=v3(m1), in_=el4, axis=AX.X, op=ALU.max), **R)
    P.op("dve", lambda g: g.tensor_tensor(out=v4(eq1), in0=el4, in1=bc4(m1), op=ALU.is_equal), **R)
    P.op("dve", lambda g: g.scalar_tensor_tensor(out=v4(el2), in0=v4(eq1), scalar=-1e30, in1=el4, op0=ALU.mult, op1=ALU.add), **R)
    P.op("dve", lambda g: g.tensor_reduce(out=v3(m2), in_=v4(el2), axis=AX.X, op=ALU.max), **R)
    P.op("dve", lambda g: g.tensor_tensor(out=v4(sel), in0=el4, in1=bc4(m2), op=ALU.is_ge), **R)
    P.op("dve", lambda g: g.tensor_tensor(out=v4(dd), in0=el4, in1=bc4(m1), op=ALU.subtract), **R)
    P.op("act", lambda g: g.activation(out=ex, in_=dd, func=AF.Exp), **R)
    P.op("dve", lambda g: g.tensor_tensor(out=ex, in0=ex, in1=sel, op=ALU.mult), **R)
    P.op("dve", lambda g: g.tensor_reduce(out=v3(den), in_=v4(ex), axis=AX.X, op=ALU.add), **R)
    P.op("dve", lambda g: g.reciprocal(out=rden, in_=den), **R)
    P.op("dve", lambda g: g.tensor_tensor(out=v4(wq), in0=v4(ex), in1=bc4(rden), op=ALU.mult), **R)
    b_comb = Buf("comb")
    P.op("dve", lambda g: g.tensor_tensor(out=v4(comb[:]), in0=v4(wq), in1=bc4(gw), op=ALU.mult), reads=[b_r], writes=[b_comb])
    comb_v = comb[:].rearrange("p (t e) -> p t e", e=16)

    if stop_after == "p3":
        P.barrier(); P.emit(); return nc, es
    P.barrier()
    c4 = Carver(scr2, 7200)
    hid = [c4.bf16(2048).rearrange("p (c t) -> p c t", c=4) for _ in range(2)]
    sg = [c4.f32(512) for _ in range(2)]
    b_hid = [Buf("hid0"), Buf("hid1")]
    b_sg = [Buf("sg0"), Buf("sg1")]
    cnt4 = {"it": 0, "dn": 0}

    def moe_gu(e, blk):
        s = e % 2
        Wg_e, Wu_e, Wd_e = WE[s]
        tcols = slice(blk * 512, (blk + 1) * 512)
        bts = [b_hT[blk * 4 + t] for t in range(4)]
        hs = (e * 4 + blk) % 2
        for hc in range(4):
            pb = (cnt4["it"] % 2) * 2
            cnt4["it"] += 1
            ss_ = cnt4["it"] % 2
            for k in range(8):
                P.op("pe", lambda g, k=k, hc=hc, pb=pb: g.matmul(
                    out=banks[pb][:, 0:512], lhsT=Wg_e[:, k, hc * 128:(hc + 1) * 128], rhs=hT_all_v[:, k, tcols],
                    start=(k == 0), stop=(k == 7)), reads=bts + [b_we[s]], writes=[b_bank[pb]])
            for k in range(8):
                P.op("pe", lambda g, k=k, hc=hc, pb=pb: g.matmul(
                    out=banks[pb + 1][:, 0:512], lhsT=Wu_e[:, k, hc * 128:(hc + 1) * 128], rhs=hT_all_v[:, k, tcols],
                    start=(k == 0), stop=(k == 7)), reads=bts + [b_we[s]], writes=[b_bank[pb + 1]])
            P.op("act", lambda g, pb=pb, ss_=ss_: g.activation(out=sg[ss_], in_=banks[pb][:, 0:512], func=AF.Silu),
                 reads=[b_bank[pb]], writes=[b_sg[ss_]])
            P.op("dve", lambda g, pb=pb, ss_=ss_, hc=hc: g.tensor_tensor(out=hid[hs][:, hc, :], in0=banks[pb + 1][:, 0:512],
                                                                         in1=sg[ss_], op=ALU.mult),
                 reads=[b_bank[pb + 1], b_sg[ss_]], writes=[b_hid[hs]])

    def moe_dn(e, blk):
        s = e % 2
        Wg_e, Wu_e, Wd_e = WE[s]
        hs = (e * 4 + blk) % 2
        for t in range(4):
            ti = blk * 4 + t
            xr = xres[:, ti * D:(ti + 1) * D]
            db = 4 + (cnt4["dn"] % 2) * 2
            cnt4["dn"] += 1
            for half in range(2):
                for hc in range(4):
                    P.op("pe", lambda g, t=t, half=half, hc=hc, db=db: g.matmul(
                        out=banks[db + half][:, 0:512], lhsT=hid[hs][:, hc, t * 128:(t + 1) * 128],
                        rhs=Wd_e[:, hc, half * 512:(half + 1) * 512], start=(hc == 0), stop=(hc == 3)),
                        reads=[b_hid[hs], b_we[s]], writes=[b_bank[db + half]])
                P.op("dve", lambda g, half=half, db=db, xr=xr, ti=ti: g.scalar_tensor_tensor(
                    out=xr[:, half * 512:(half + 1) * 512], in0=banks[db + half][:, 0:512], scalar=comb_v[:, ti, e:e + 1],
                    in1=xr[:, half * 512:(half + 1) * 512], op0=ALU.mult, op1=ALU.add),
                    reads=[b_bank[db + half], b_comb], writes=[b_xr[ti]])

    prev = None
    for e in range(NE):
        if e > 0:
            load_expert(e)
        for blk in range(4):
            moe_gu(e, blk)
            if prev is not None:
                moe_dn(*prev)
            prev = (e, blk)
            if e == NE - 1 and blk == 0:
                ld("pool", Wpg, wpg_d.rearrange("(k p) c -> p k c", p=128), [b_w5, b_we[0]], ds_w5)
                ld("pool", Wpp, wpp_d.rearrange("(k p) c -> p k c", p=128), [b_w5, b_we[0]], ds_w5)
    moe_dn(*prev)

    if stop_after == "p4":
        P.barrier(); P.emit(); return nc, es
    P.barrier()
    c5 = Carver(scr2, 7200)
    pt = [c5.f32(256) for _ in range(2)]
    ptb = [c5.bf16(256) for _ in range(2)]
    pT = [c5.bf16(256).rearrange("p (k t) -> p k t", k=2) for _ in range(2)]
    h3b = [c5.bf16(1024) for _ in range(2)]
    h3T = [c5.bf16(1024).rearrange("p (k t) -> p k t", k=8) for _ in range(2)]
    sig = c5.f32(1024)
    outst = [c5.f32(1024) for _ in range(2)]
    b_pt = [Buf("pt0"), Buf("pt1")]
    ds_pt = [P.dsem(), P.dsem()]
    b_ptb, b_pT = [Buf("ptb0"), Buf("ptb1")], [Buf("pT0"), Buf("pT1")]
    b_h3b, b_h3T = [Buf("h3b0"), Buf("h3b1")], [Buf("h3T0"), Buf("h3T1")]
    b_sig = Buf("sig")
    b_out = [Buf("o0"), Buf("o1")]
    ds_out = [P.dsem(), P.dsem()]
    b_s5 = [[Buf("s5_%d_%d" % (a, b)) for b in range(4)] for a in range(2)]
    bank1b = banks[1][:, 0:512].bitcast(BF16)
    last_store = []

    def st5X(ti):
        sl = ti % 2
        xr = xres[:, ti * D:(ti + 1) * D]
        ld("sp", pt[sl], p_d[ti * 128:(ti + 1) * 128, :], b_pt[sl], ds_pt[sl])
        c = 4 + 8 * sl
        rms_stats(xr, b_xr[ti], c, c + 1, h3b[sl], b_h3b[sl], b_s5[sl][0], b_s5[sl][1])
        P.op("dve", lambda g: g.scalar_tensor_tensor(out=h3b[sl], in0=xr, scalar=small[:, c + 1:c + 2], in1=gA[:], op0=ALU.mult, op1=ALU.mult),
             reads=[b_xr[ti], b_s5[sl][1], b_g5], writes=[b_h3b[sl]])
        P.op("dve", lambda g: g.tensor_copy(out=ptb[sl], in_=pt[sl]), reads=[b_pt[sl]], writes=[b_ptb[sl]])

    def st5Y(ti):
        sl = ti % 2
        for k in range(8):
            P.op("pe", lambda g, k=k: g.transpose(out=bank0b[:, k * 128:(k + 1) * 128], in_=h3b[sl][:, k * 128:(k + 1) * 128], identity=identb[:]),
                 reads=[b_h3b[sl], b_const], writes=[b_bank[0]])
        P.op("dve", lambda g: g.tensor_copy(out=h3T[sl], in_=bank0b.rearrange("p (k t) -> p k t", k=8)), reads=[b_bank[0]], writes=[b_h3T[sl]])
        for k in range(2):
            P.op("pe", lambda g, k=k: g.transpose(out=bank1b[:, k * 128:(k + 1) * 128], in_=ptb[sl][:, k * 128:(k + 1) * 128], identity=identb[:]),
                 reads=[b_ptb[sl], b_const], writes=[b_bank[1]])
        P.op("dve", lambda g: g.tensor_copy(out=pT[sl], in_=bank1b[:, 0:256].rearrange("p (k t) -> p k t", k=2)), reads=[b_bank[1]], writes=[b_pT[sl]])

    def st5Z(ti):
        sl = ti % 2
        xr = xres[:, ti * D:(ti + 1) * D]
        for half in range(2):
            for k in range(8):
                P.op("pe", lambda g, k=k, half=half: g.matmul(out=banks[2 + half][:, 0:512], lhsT=h3T[sl][:, k, :],
                                                             rhs=Wpg[:, k, half * 512:(half + 1) * 512], start=(k == 0), stop=(k == 7)),
                     reads=[b_h3T[sl], b_w5], writes=[b_bank[2 + half]])
            for k in range(2):
                P.op("pe", lambda g, k=k, half=half: g.matmul(out=banks[4 + half][:, 0:512], lhsT=pT[sl][:, k, :],
                                                             rhs=Wpp[:, k, half * 512:(half + 1) * 512], start=(k == 0), stop=(k == 1)),
                     reads=[b_pT[sl], b_w5], writes=[b_bank[4 + half]])
            P.op("act", lambda g, half=half: g.activation(out=sig[:, half * 512:(half + 1) * 512], in_=banks[2 + half][:, 0:512], func=AF.Sigmoid),
                 reads=[b_bank[2 + half]], writes=[b_sig])
            P.op("dve", lambda g, half=half: g.tensor_tensor(out=sig[:, half * 512:(half + 1) * 512], in0=banks[4 + half][:, 0:512],
                                                             in1=sig[:, half * 512:(half + 1) * 512], op=ALU.mult),
                 reads=[b_bank[4 + half], b_sig], writes=[b_sig])
        P.op("dve", lambda g: g.tensor_tensor(out=xr, in0=sig, in1=xr, op=ALU.add), reads=[b_sig], writes=[b_xr[ti]])

    def st5W(ti):
        sl = ti % 2
        xr = xres[:, ti * D:(ti + 1) * D]
        c = 6 + 8 * sl
        rms_stats(xr, b_xr[ti], c, c + 1, outst[sl].bitcast(BF16)[:, 0:1024], b_out[sl], b_s5[sl][2], b_s5[sl][3])
        P.op("dve", lambda g: g.scalar_tensor_tensor(out=outst[sl], in0=xr, scalar=small[:, c + 1:c + 2], in1=gB[:], op0=ALU.mult, op1=ALU.mult),
             reads=[b_xr[ti], b_s5[sl][3], b_g5], writes=[b_out[sl]])
        tok = P.op("pool", lambda g: g.dma_start(out=y_d[ti * 128:(ti + 1) * 128, :], in_=outst[sl]),
                   reads=[b_out[sl]], dsem=ds_out[sl])
        last_store.append(tok)

    st5 = [st5X, st5Y, st5Z, st5W]
    for step in range(NT_OWN + len(st5) - 1):
        for k, st in enumerate(st5):
            t = step - k
            if 0 <= t < NT_OWN:
                st(t)
    P.wait_only("pool", last_store[-2:])
    P.wait_only("sp", last_store[-2:])
    P.emit()
    return nc, es


_CACHE = {}


def _consts():
    s = np.arange(128)[:, None]
    t = np.arange(128)[None, :]
    tril = np.where(s <= t, -1.0 / 16.0, 0.0).astype(np.float32)
    triu = np.where(s > t, -1.0 / 16.0, 0.0).astype(np.float32)
    mask = np.tile((s <= t).astype(np.float32), (1, 4))
    return {
        "c_ident": np.eye(128, dtype=np.float32),
        "c_tril": tril,
        "c_triu": triu,
        "c_mask": np.ascontiguousarray(mask),
        "c_ones": np.full((128, 128), 1.0 / 128.0, np.float32),
        "c_neg": np.full((128, 1), -1.0 / 16.0, np.float32),
    }


def kernel(x, p, g_mix, w_in, w_gla_gate, b_gla_gate, g_gla_out, w_conv, w_out,
           g_moe, w_group, b_group, w_router, b_router, w_exp_gate, w_exp_up, w_exp_down,
           g_ple, w_ple_gate, w_ple_proj, g_final):
    f = lambda a: np.ascontiguousarray(np.asarray(a, dtype=np.float32))
    x = f(x); p = f(p)
    if "nc" not in _CACHE:
        _CACHE["nc"] = build_program()
    nc, _es = _CACHE["nc"]
    wg_aug = np.zeros((32, 256), np.float32)
    wg_aug[0:16] = f(w_gla_gate)[0]
    wg_aug[16] = f(b_gla_gate)[0]
    shared = {
        "w_in": f(w_in)[0], "w_out": f(w_out)[0],
        "w_exp_gate": f(w_exp_gate)[0].reshape(NE * D, 512),
        "w_exp_up": f(w_exp_up)[0].reshape(NE * D, 512),
        "w_exp_down": f(w_exp_down)[0].reshape(NE * 512, D),
        "w_ple_gate": f(w_ple_gate)[0], "w_ple_proj": f(w_ple_proj)[0],
        "g_mix": f(g_mix).reshape(1, D), "g_moe": f(g_moe).reshape(1, D), "g_ple": f(g_ple).reshape(1, D),
        "g_final": f(g_final).reshape(1, D),
        "w_rt": np.ascontiguousarray(np.concatenate([f(w_group)[0], f(w_router)[0]], axis=1)),
        "b_rt": np.ascontiguousarray(np.concatenate([f(b_group)[0], f(b_router)[0]], axis=0).reshape(1, 20)),
        "wg_aug": wg_aug,
        "wconv_t": np.ascontiguousarray(f(w_conv)[0].reshape(3, 4, 128).transpose(2, 1, 0).reshape(128, 12)),
        "ggo": np.ascontiguousarray(f(g_gla_out)[0].reshape(128, 1)),
    }
    shared.update(_consts())
    in_maps = []
    for c in range(8):
        b, j = c // 4, c % 4
        xs = np.zeros((NT_ALL * 128, D), np.float32)
        n = 2048 * (j + 1)
        xs[NT_ALL * 128 - n:] = x[b, 0:n]
        m = dict(shared)
        m["xs"] = xs
        m["p_own"] = np.ascontiguousarray(p[0, b, 2048 * j:2048 * (j + 1)])
        in_maps.append(m)
    res = run_bass_kernel_spmd(nc, in_maps, core_ids=list(range(8)))
    out = np.empty((2, 8192, D), np.float32)
    for c in range(8):
        b, j = c // 4, c % 4
        out[b, 2048 * j:2048 * (j + 1)] = res.results[c]["y"]
    return out
```

```python
import numpy as np
from contextlib import ExitStack
import concourse.bass as bass
import concourse.mybir as mybir
from concourse.bass_utils import run_bass_kernel_spmd

F32 = mybir.dt.float32
BF16 = mybir.dt.bfloat16
AF = mybir.ActivationFunctionType
ALU = mybir.AluOpType
AX = mybir.AxisListType

EPS = 1e-6
NT_ALL = 64
NT_OWN = 16
NT_PRE = NT_ALL - NT_OWN
D = 1024
DIN = 3088
NE = 16


class Buf:
    __slots__ = ("name", "w", "r")

    def __init__(self, name):
        self.name = name
        self.w = None
        self.r = []


class DSem:
    def __init__(self, h):
        self.h = h
        self.count = 0
        self.nobar = False


class Prog:
    def __init__(self, nc, es):
        self.nc = nc
        self.es = es
        self.names = ["pe", "act", "dve", "pool", "sp"]
        self.streams = {k: [] for k in self.names}
        self.sem = {k: es.enter_context(nc.semaphore("c_" + k)) for k in ["pe", "act", "dve"]}
        self.cnt = {k: 0 for k in self.sem}
        self.known = {k: {} for k in self.names}
        self.handles = {}
        self.dsems = []
        self.nds = 0

    def dsem(self):
        self.nds += 1
        s = DSem(self.es.enter_context(self.nc.semaphore("d%d" % self.nds)))
        self.dsems.append(s)
        return s

    def _filter(self, e, toks):
        need = {}
        for (s, v) in toks:
            if e == "pe" and s is self.sem["pe"]:
                continue
            k = id(s)
            self.handles[k] = s
            if need.get(k, 0) < v:
                need[k] = v
        out = []
        kn = self.known[e]
        for k, v in need.items():
            if kn.get(k, 0) < v:
                kn[k] = v
                out.append((self.handles[k], v))
        return out

    def op(self, e, fn, reads=(), writes=(), dsem=None):
        toks = []
        for b in reads:
            if b.w is not None:
                toks.append(b.w)
        for b in writes:
            toks.extend(b.r)
            if b.w is not None:
                toks.append(b.w)
        waits = self._filter(e, toks)
        if dsem is None:
            self.cnt[e] += 1
            tok = (self.sem[e], self.cnt[e])
            inc = 1
        else:
            dsem.count += 16
            tok = (dsem.h, dsem.count)
            inc = 16
        self.streams[e].append((waits, fn, tok[0], inc))
        for b in writes:
            b.w = tok
            b.r = []
        for b in reads:
            b.r.append(tok)
        return tok

    def wait_only(self, e, toks):
        waits = self._filter(e, toks)
        if waits:
            self.streams[e].append((waits, None, None, 0))

    def barrier(self):
        toks = [(self.sem[k], self.cnt[k]) for k in self.sem if self.cnt[k] > 0]
        toks += [(d.h, d.count) for d in self.dsems if d.count > 0 and not d.nobar]
        for e in self.names:
            self.wait_only(e, toks)

    def emit(self):
        nc = self.nc
        with nc.Block() as block:
            decos = {"pe": block.tensor, "act": block.scalar, "dve": block.vector,
                     "pool": block.gpsimd, "sp": block.sync}
            for k in self.names:
                stream = self.streams[k]

                def body(engine, stream=stream):
                    for waits, fn, sem, inc in stream:
                        for (s, v) in waits:
                            engine.wait_ge(s, v)
                        if fn is not None:
                            ins = fn(engine)
                            ins.then_inc(sem, inc)

                decos[k](body)


def build_program(stop_after=None):
    nc = bass.Bass("TRN2", target_bir_lowering=False)
    es = ExitStack()

    def din(name, shape):
        return nc.dram_tensor(name, list(shape), F32, kind="ExternalInput").ap()

    xs_d = din("xs", [NT_ALL * 128, D])
    p_d = din("p_own", [NT_OWN * 128, 256])
    w_in_d = din("w_in", [D, DIN])
    w_out_d = din("w_out", [D, D])
    wg_d = din("w_exp_gate", [NE * D, 512])
    wu_d = din("w_exp_up", [NE * D, 512])
    wd_d = din("w_exp_down", [NE * 512, D])
    wpg_d = din("w_ple_gate", [D, D])
    wpp_d = din("w_ple_proj", [256, D])
    gmix_d = din("g_mix", [1, D])
    gmoe_d = din("g_moe", [1, D])
    gple_d = din("g_ple", [1, D])
    gfin_d = din("g_final", [1, D])
    wr_d = din("w_rt", [D, 20])
    br_d = din("b_rt", [1, 20])
    wgate_d = din("wg_aug", [32, 256])
    wconv_d = din("wconv_t", [128, 12])
    ggo_d = din("ggo", [128, 1])
    cid_d = din("c_ident", [128, 128])
    ctl_d = din("c_tril", [128, 128])
    ctu_d = din("c_triu", [128, 128])
    cmask_d = din("c_mask", [128, 512])
    cones_d = din("c_ones", [128, 128])
    cneg_d = din("c_neg", [128, 1])
    y_d = nc.dram_tensor("y", [NT_OWN * 128, D], F32, kind="ExternalOutput").ap()

    def sb(name, shape, dt):
        return es.enter_context(nc.sbuf_tensor("s_" + name, list(shape), dt))

    xres = sb("xres", [128, NT_OWN * D], F32)
    hT_all = sb("hT_all", [128, 8 * 2048], BF16)
    y1_all = sb("y1_all", [128, 4 * 2048], BF16)
    wbig = sb("wbig", [128, 24704], BF16)
    scr2 = sb("scr2", [128, 7200], F32)
    carry_t = sb("carry", [128, 8], F32)
    identb = sb("identb", [128, 128], BF16)
    identf = sb("identf", [128, 128], F32)
    tril = sb("tril", [128, 128], F32)
    triu = sb("triu", [128, 128], F32)
    maskb = sb("maskb", [128, 512], BF16)
    onesb = sb("onesb", [128, 128], BF16)
    negcol = sb("negcol", [128, 1], F32)
    gA = sb("gA", [128, D], F32)
    gB = sb("gB", [128, D], F32)
    wr = sb("wr", [128, 8 * 20], F32)
    brb = sb("brb", [128, 20], F32)
    wgate = sb("wgate", [32, 256], F32)
    wconv = sb("wconv", [128, 12], F32)
    ggo = sb("ggo", [128, 1], F32)
    Lg = sb("Lg", [128, NT_OWN * 20], F32)
    comb = sb("comb", [128, NT_OWN * 16], F32)
    small = sb("small", [128, 16], F32)

    banks = [es.enter_context(nc.psum_tensor("bank%d" % i, [128, 512], F32)) for i in range(8)]

    P = Prog(nc, es)

    class Carver:
        def __init__(self, base, nwords):
            self.base = base
            self.off = 0
            self.n = nwords

        def f32(self, n):
            a = self.base[:, self.off:self.off + n]
            self.off += n
            assert self.off <= self.n, (self.off, self.n)
            return a

        def bf16(self, n):
            assert n % 2 == 0
            return self.f32(n // 2).bitcast(BF16)

    b_const = Buf("const")
    ds_const = P.dsem()

    def ld(e, out_ap, in_ap, buf, dsem):
        bufs = buf if isinstance(buf, (list, tuple)) else [buf]
        return P.op(e, lambda g: g.dma_start(out=out_ap, in_=in_ap), writes=list(bufs), dsem=dsem)

    ld("sp", identf[:], cid_d, b_const, ds_const)
    ld("sp", tril[:], ctl_d, b_const, ds_const)
    ld("sp", triu[:], ctu_d, b_const, ds_const)
    ld("sp", negcol[:], cneg_d, b_const, ds_const)
    ld("sp", gA[:], gmix_d.broadcast_to([128, D]), b_const, ds_const)
    ld("sp", wr[:].rearrange("p (k n) -> p k n", k=8), wr_d.rearrange("(k p) n -> p k n", p=128), b_const, ds_const)
    ld("sp", brb[:], br_d.broadcast_to([128, 20]), b_const, ds_const)
    ld("sp", wgate[:], wgate_d, b_const, ds_const)
    ld("sp", wconv[:], wconv_d, b_const, ds_const)
    ld("sp", ggo[:], ggo_d, b_const, ds_const)
    ds_const2 = P.dsem()
    ld("pool", identb[:], cid_d, b_const, ds_const2)
    ld("pool", maskb[:], cmask_d, b_const, ds_const2)
    ld("pool", onesb[:], cones_d, b_const, ds_const2)

    c1 = Carver(xres, NT_OWN * D)
    Wqkva = c1.bf16(8 * 1296).rearrange("p (k c) -> p k c", k=8)
    xs_t = [c1.f32(1024) for _ in range(2)]
    hb_t = [c1.bf16(1024) for _ in range(2)]
    hts_off = c1.off
    hTs = [c1.bf16(1024).rearrange("p (k t) -> p k t", k=8) for _ in range(4)]
    aT_t = [c1.f32(128) for _ in range(2)]
    e1 = c1.f32(256)
    sp_t = [c1.f32(256) for _ in range(2)]
    Er = c1.f32(256)
    El_t = [c1.f32(8) for _ in range(2)]
    kd_t = [c1.bf16(256) for _ in range(2)]
    vb_t = [c1.bf16(512) for _ in range(2)]
    Eq = c1.f32(256)
    Ek = c1.f32(256)
    qt_t = [c1.bf16(256) for _ in range(2)]
    kt_t = [c1.bf16(256) for _ in range(2)]
    qkT0 = c1.bf16(1024)
    Pm = c1.bf16(512)
    S = c1.f32(512)
    Sb0 = c1.bf16(512)
    sq0 = c1.bf16(512)
    rstd = c1.f32(512)
    c1o = Carver(xres, hts_off + 1536)
    c1o.off = hts_off
    vb4 = [vb_t[0], vb_t[1], c1o.bf16(512), c1o.bf16(512)]
    qkT_t = [qkT0, c1o.bf16(1024)]
    Sb_t = [Sb0, c1o.bf16(512)]
    sq_t = [sq0, c1o.bf16(512)]

    w_in_v = w_in_d.rearrange("(k p) c -> p k c", p=128)
    b_wqkva = Buf("wqkva")
    ds_w1 = P.dsem()
    b_wa = Buf("wa")
    ds_wa = P.dsem()
    ld("pool", Wqkva[:, :, 1024:1040], w_in_v[:, :, 1536:1552], b_wa, ds_wa)
    ld("pool", Wqkva[:, :, 256:1024], w_in_v[:, :, 256:1024], b_wqkva, ds_w1)
    b_wq = Buf("wq")
    ds_wq = P.dsem()
    ld("pool", Wqkva[:, :, 0:256], w_in_v[:, :, 0:256], b_wq, ds_wq)

    Wgbcu = wbig[:, 0:8 * 2048].rearrange("p (k c) -> p k c", k=8)
    Wout = wbig[:, 8 * 2048:8 * 3072].rearrange("p (k c) -> p k c", k=8)
    b_wg = Buf("wgbcu")
    b_wo = Buf("wout")
    ds_w2 = P.dsem()
    ds_wo = P.dsem()
    ld("pool", Wgbcu[:, :, 0:512], w_in_v[:, :, 1024:1536], b_wg, ds_w2)
    ld("pool", Wgbcu[:, :, 512:2048], w_in_v[:, :, 1552:3088], b_wg, ds_w2)
    ld("pool", Wout, w_out_d.rearrange("(k p) c -> p k c", p=128), b_wo, ds_wo)
    WE = []
    for s_ in range(2):
        base = s_ * 12288
        WE.append((wbig[:, base:base + 4096].rearrange("p (k c) -> p k c", k=8),
                   wbig[:, base + 4096:base + 8192].rearrange("p (k c) -> p k c", k=8),
                   wbig[:, base + 8192:base + 12288].rearrange("p (k c) -> p k c", k=4)))
    b_we = [Buf("we0"), Buf("we1")]
    ds_we = [P.dsem(), P.dsem()]
    ds_we[0].nobar = True
    ds_we[1].nobar = True
    Wpg = wbig[:, 0:8192].rearrange("p (k c) -> p k c", k=8)
    Wpp = wbig[:, 8192:8192 + 2048].rearrange("p (k c) -> p k c", k=2)
    b_w5 = Buf("w5")
    ds_w5 = P.dsem()
    ds_w5.nobar = True

    def load_expert(e, extra=()):
        s_ = e % 2
        Wg_e, Wu_e, Wd_e = WE[s_]
        bufs = [b_we[s_]] + list(extra)
        ld("pool", Wg_e, wg_d[e * D:(e + 1) * D, :].rearrange("(k p) c -> p k c", p=128), bufs, ds_we[s_])
        ld("pool", Wu_e, wu_d[e * D:(e + 1) * D, :].rearrange("(k p) c -> p k c", p=128), bufs, ds_we[s_])
        ld("pool", Wd_e, wd_d[e * 512:(e + 1) * 512, :].rearrange("(k p) c -> p k c", p=128), bufs, ds_we[s_])

    if stop_after == "consts":
        P.barrier(); P.emit(); return nc, es
    import os
    b_xs = [Buf("xs0"), Buf("xs1")]
    ds_xs = [P.dsem(), P.dsem()]
    b_hb = [Buf("hb0"), Buf("hb1")]
    b_hTs = [Buf("hTs%d" % i) for i in range(4)]
    b_hT = [Buf("hT%d" % i) for i in range(NT_OWN)]
    b_y1 = [Buf("y1_%d" % i) for i in range(NT_OWN)]
    b_bank = [Buf("bank%d" % i) for i in range(8)]
    b_aTp = Buf("aTp")
    b_lastp = Buf("lastp")
    b_zp = Buf("zp")
    b_aT = [Buf("aT0"), Buf("aT1")]
    b_e1 = Buf("e1")
    b_sp = [Buf("sp0"), Buf("sp1")]
    b_Eq, b_Ek, b_Er = Buf("Eq"), Buf("Ek"), Buf("Er")
    b_El = [Buf("El0"), Buf("El1")]
    b_kd = [Buf("kd0"), Buf("kd1")]
    b_vb = [Buf("vb0"), Buf("vb1")]
    b_qt, b_kt = [Buf("qt0"), Buf("qt1")], [Buf("kt0"), Buf("kt1")]
    b_Pm, b_S, b_rstd = Buf("Pm"), Buf("S"), Buf("rstd")
    b_qkT, b_Sb, b_sq = [Buf("qkT0"), Buf("qkT1")], [Buf("Sb0"), Buf("Sb1")], [Buf("sq0"), Buf("sq1")]
    b_vb4 = [b_vb[0], b_vb[1], Buf("vb2"), Buf("vb3")]

    def vb_of(i):
        if i >= NT_PRE:
            return vb4[i % 4], b_vb4[i % 4]
        return vb_t[i % 2], b_vb[i % 2]
    b_ss = [Buf("ss0"), Buf("ss1")]
    b_rs = [Buf("rs0"), Buf("rs1")]

    bank0b = banks[0][:, 0:512].bitcast(BF16)
    bank7b = banks[7][:, 0:512].bitcast(BF16)

    hT_all_v = hT_all[:].rearrange("p (k t) -> p k t", k=8)
    y1_all_v = y1_all[:].rearrange("p (h t) -> p h t", h=4)
    S_v = S.rearrange("p (h e) -> p h e", h=4)

    P.op("dve", lambda g: g.memset(S[0:64, :], 0.0), writes=[b_S])
    for a_ in range(2):
        P.op("dve", lambda g, a_=a_: g.memset(aT_t[a_][0:32, :], 1.0), writes=[b_aT[a_]])

    def rms_stats(src_ap, b_src, ss_col, rs_col, junk, b_junk, bss=None, brs=None):
        bss = bss or b_ss[0]
        brs = brs or b_rs[0]
        P.op("act", lambda g: g.activation(out=junk, in_=src_ap, func=AF.Square, accum_out=small[:, ss_col:ss_col + 1]),
             reads=[b_src], writes=[b_junk, bss])
        P.op("act", lambda g: g.activation(out=small[:, rs_col:rs_col + 1], in_=small[:, ss_col:ss_col + 1], func=AF.Ln,
                                           scale=1.0 / D, bias=EPS),
             reads=[bss], writes=[brs])
        P.op("act", lambda g: g.activation(out=small[:, rs_col:rs_col + 1], in_=small[:, rs_col:rs_col + 1], func=AF.Exp, scale=-0.5),
             reads=[brs], writes=[brs])

    def hT_of(i):
        if i >= NT_PRE:
            oi = i - NT_PRE
            return hT_all_v[:, :, oi * 128:(oi + 1) * 128], b_hT[oi]
        return hTs[i % 4], b_hTs[i % 4]

    def stageA(i):
        sl = i % 2
        xt = xs_t[sl]
        ld("sp", xt, xs_d[i * 128:(i + 1) * 128, :], b_xs[sl], ds_xs[sl])
        hb = hb_t[sl]
        rms_stats(xt, b_xs[sl], 8 * sl, 8 * sl + 1, hb, b_hb[sl], b_ss[sl], b_rs[sl])
        P.op("dve", lambda g: g.scalar_tensor_tensor(out=hb, in0=xt, scalar=small[:, 8 * sl + 1:8 * sl + 2], in1=gA[:],
                                                     op0=ALU.mult, op1=ALU.mult),
             reads=[b_xs[sl], b_rs[sl], b_const], writes=[b_hb[sl]])

    def stageB(i):
        sl = i % 2
        hb = hb_t[sl]
        hT, bh = hT_of(i)
        for k in range(8):
            P.op("pe", lambda g, k=k: g.transpose(out=bank0b[:, k * 128:(k + 1) * 128], in_=hb[:, k * 128:(k + 1) * 128],
                                                 identity=identb[:]),
                 reads=[b_hb[sl], b_const], writes=[b_bank[0]])
        P.op("act", lambda g: g.copy(out=hT, in_=bank0b.rearrange("p (k t) -> p k t", k=8)),
             reads=[b_bank[0]], writes=[bh])

    def stageC(i):
        sl = i % 2
        hT, bh = hT_of(i)
        for k in range(8):
            P.op("pe", lambda g, k=k: g.matmul(out=banks[1][0:16, 0:128], lhsT=Wqkva[:, k, 1024:1040], rhs=hT[:, k, :],
                                              start=(k == 0), stop=(k == 7)),
                 reads=[bh, b_wa], writes=[b_bank[1]])
        P.op("act", lambda g: g.copy(out=aT_t[sl][0:16, :], in_=banks[1][0:16, 0:128]), reads=[b_bank[1]], writes=[b_aT[sl]])

    def stageD(i):
        sl = i % 2
        P.op("pe", lambda g: g.matmul(out=banks[2][:, 256:512], lhsT=aT_t[sl][0:17, :], rhs=wgate[0:17, :], start=True, stop=True),
             reads=[b_aT[sl], b_const], writes=[b_bank[2]])
        P.op("act", lambda g: g.activation(out=e1, in_=banks[2][:, 256:512], func=AF.Exp, scale=-1.0), reads=[b_bank[2]], writes=[b_e1])
        P.op("act", lambda g: g.activation(out=sp_t[sl], in_=e1, func=AF.Ln, bias=1.0), reads=[b_e1], writes=[b_sp[sl]])

    def stageE(i):
        own = i >= NT_PRE
        sl = i % 2
        hT, bh = hT_of(i)
        spb = sp_t[sl]
        if own:
            P.op("pe", lambda g: g.matmul(out=banks[3][:, 0:256], lhsT=tril[:], rhs=spb, start=True, stop=True),
                 reads=[b_sp[sl], b_const], writes=[b_bank[3]])
        P.op("pe", lambda g: g.matmul(out=banks[3][:, 256:512], lhsT=triu[:], rhs=spb, start=True, stop=True),
             reads=[b_sp[sl], b_const], writes=[b_bank[3]])
        for h in range(4):
            P.op("pe", lambda g, h=h: g.matmul(out=banks[7][0:64, 128 + h:129 + h], lhsT=spb[:, h * 64:(h + 1) * 64], rhs=negcol[:],
                                              start=True, stop=True),
                 reads=[b_sp[sl], b_const], writes=[b_bank[7]])
        if own:
            ncol, c0 = 512, 0
        else:
            ncol, c0 = 256, 256
        for k in range(8):
            P.op("pe", lambda g, k=k: g.matmul(out=banks[4][:, 0:ncol], lhsT=hT[:, k, :], rhs=Wqkva[:, k, c0:c0 + ncol],
                                              start=(k == 0), stop=(k == 7)),
                 reads=[bh, b_wqkva, b_wq], writes=[b_bank[4]])
        for k in range(8):
            P.op("pe", lambda g, k=k: g.matmul(out=banks[5][:, 0:512], lhsT=hT[:, k, :], rhs=Wqkva[:, k, 512:1024],
                                              start=(k == 0), stop=(k == 7)),
                 reads=[bh, b_wqkva], writes=[b_bank[5]])
        P.op("act", lambda g: g.activation(out=Er, in_=banks[3][:, 256:512], func=AF.Exp), reads=[b_bank[3]], writes=[b_Er])
        if own:
            P.op("act", lambda g: g.activation(out=Eq, in_=banks[3][:, 0:256], func=AF.Exp), reads=[b_bank[3]], writes=[b_Eq])
            P.op("act", lambda g: g.activation(out=Ek, in_=banks[3][:, 0:256], func=AF.Exp, scale=-1.0), reads=[b_bank[3]], writes=[b_Ek])
        P.op("act", lambda g: g.activation(out=El_t[sl][0:64, 0:4], in_=banks[7][0:64, 128:132], func=AF.Exp),
             reads=[b_bank[7]], writes=[b_El[sl]])
        kcol = 256 if own else 0
        P.op("dve", lambda g: g.tensor_tensor(out=kd_t[sl], in0=banks[4][:, kcol:kcol + 256], in1=Er, op=ALU.mult),
             reads=[b_bank[4], b_Er], writes=[b_kd[sl]])
        if own:
            P.op("dve", lambda g: g.scalar_tensor_tensor(out=qt_t[sl], in0=banks[4][:, 0:256], scalar=0.125, in1=Eq, op0=ALU.mult, op1=ALU.mult),
                 reads=[b_bank[4], b_Eq], writes=[b_qt[sl]])
            P.op("dve", lambda g: g.tensor_tensor(out=kt_t[sl], in0=banks[4][:, 256:512], in1=Ek, op=ALU.mult),
                 reads=[b_bank[4], b_Ek], writes=[b_kt[sl]])
        vb, bvb = vb_of(i)
        P.op("dve", lambda g: g.tensor_copy(out=vb, in_=banks[5][:, 0:512]), reads=[b_bank[5]], writes=[bvb])

    def stageF(i):
        sl = i % 2
        kd = kd_t[sl]
        vb, bvb = vb_of(i)
        for h in range(4):
            P.op("pe", lambda g, h=h: g.matmul(out=banks[6][0:64, h * 128:(h + 1) * 128], lhsT=kd[:, h * 64:(h + 1) * 64],
                                              rhs=vb[:, h * 128:(h + 1) * 128], start=True, stop=True),
                 reads=[b_kd[sl], bvb], writes=[b_bank[6]])

    def stageS(i):
        sl = i % 2
        P.op("dve", lambda g: g.tensor_tensor(out=S_v[0:64, :, :], in0=S_v[0:64, :, :],
                                              in1=El_t[sl][0:64, 0:4].unsqueeze(2).broadcast_to([64, 4, 128]), op=ALU.mult),
             reads=[b_S, b_El[sl]], writes=[b_S])
        P.op("dve", lambda g: g.tensor_tensor(out=S[0:64, :], in0=S[0:64, :], in1=banks[6][0:64, 0:512], op=ALU.add),
             reads=[b_S, b_bank[6]], writes=[b_S])

    def stageFo(i):
        sl = i % 2
        stageF(i)
        P.op("act", lambda g: g.copy(out=Sb_t[sl][0:64, :], in_=S[0:64, :]), reads=[b_S], writes=[b_Sb[sl]])
        stageS(i)
        for j in range(8):
            src = qt_t[sl] if j < 4 else kt_t[sl]
            a = j % 4
            P.op("pe", lambda g, j=j, src=src, a=a: g.transpose(out=bank7b[0:64, j * 128:(j + 1) * 128], in_=src[:, a * 64:(a + 1) * 64],
                                                               identity=identb[:]),
                 reads=[b_qt[sl], b_kt[sl], b_const], writes=[b_bank[7]])
        P.op("act", lambda g: g.copy(out=qkT_t[sl][0:64, :], in_=bank7b[0:64, :]), reads=[b_bank[7]], writes=[b_qkT[sl]])

    def stageHI(i):
        sl = i % 2
        vb, bvb = vb_of(i)
        qkT = qkT_t[sl]
        Sb_v = Sb_t[sl].rearrange("p (h e) -> p h e", h=4)
        for h in range(4):
            P.op("pe", lambda g, h=h: g.matmul(out=banks[2][:, h * 128:(h + 1) * 128], lhsT=qkT[0:64, (4 + h) * 128:(5 + h) * 128],
                                              rhs=qkT[0:64, h * 128:(h + 1) * 128], start=True, stop=True),
                 reads=[b_qkT[sl]], writes=[b_bank[2]])
        P.op("dve", lambda g: g.tensor_tensor(out=Pm, in0=banks[2][:, 0:512], in1=maskb[:], op=ALU.mult),
             reads=[b_bank[2], b_const], writes=[b_Pm])
        for h in range(4):
            P.op("pe", lambda g, h=h: g.matmul(out=banks[7][:, h * 128:(h + 1) * 128], lhsT=vb[:, h * 128:(h + 1) * 128],
                                              rhs=Pm[:, h * 128:(h + 1) * 128], start=True, stop=False),
                 reads=[bvb, b_Pm], writes=[b_bank[7]])
            P.op("pe", lambda g, h=h: g.matmul(out=banks[7][:, h * 128:(h + 1) * 128], lhsT=Sb_v[0:64, h, :],
                                              rhs=qkT[0:64, h * 128:(h + 1) * 128], start=False, stop=True),
                 reads=[b_Sb[sl], b_qkT[sl]], writes=[b_bank[7]])
        P.op("act", lambda g: g.activation(out=sq_t[sl], in_=banks[7][:, 0:512], func=AF.Square), reads=[b_bank[7]], writes=[b_sq[sl]])

    def stageJ(i):
        oi = i - NT_PRE
        sl = i % 2
        P.op("pe", lambda g: g.matmul(out=banks[2][:, 0:512], lhsT=onesb[:], rhs=sq_t[sl], start=True, stop=True),
             reads=[b_sq[sl], b_const], writes=[b_bank[2]])
        P.op("act", lambda g: g.activation(out=rstd, in_=banks[2][:, 0:512], func=AF.Ln, bias=EPS), reads=[b_bank[2]], writes=[b_rstd])
        P.op("act", lambda g: g.activation(out=rstd, in_=rstd, func=AF.Exp, scale=-0.5), reads=[b_rstd], writes=[b_rstd])
        P.op("dve", lambda g: g.tensor_tensor(out=y1_all_v[:, :, oi * 128:(oi + 1) * 128],
                                              in0=banks[7][:, 0:512].rearrange("p (h t) -> p h t", h=4),
                                              in1=rstd.rearrange("p (h t) -> p h t", h=4), op=ALU.mult),
             reads=[b_bank[7], b_rstd], writes=[b_y1[oi]])

    T0 = int(os.environ.get('K_T0', 0))
    T1 = int(os.environ.get('K_T1', NT_ALL))
    pre_stages = [stageA, stageB, stageC, stageD, stageE, lambda i: (stageF(i), stageS(i))]
    npre = min(T1, NT_PRE)
    for step in range(T0, npre + len(pre_stages) - 1):
        for k, st in enumerate(pre_stages):
            t = step - k
            if T0 <= t < npre:
                st(t)
    b_carry = Buf("carry")
    carry_v = carry_t[:].rearrange("p (c j) -> p c j", c=4)
    hT47, b_h47 = hT_of(NT_PRE - 1)
    for cc in range(4):
        for k in range(8):
            P.op("pe", lambda g, cc=cc, k=k: g.matmul(out=banks[2][:, 0:2], lhsT=Wgbcu[:, k, 1024 + cc * 128:1024 + (cc + 1) * 128],
                                                     rhs=hT47[:, k, 126:128], start=(k == 0), stop=(k == 7)),
                 reads=[b_h47, b_wg], writes=[b_bank[2]])
        for k in range(8):
            P.op("pe", lambda g, cc=cc, k=k: g.matmul(out=banks[3][:, 0:2], lhsT=Wgbcu[:, k, 1536 + cc * 128:1536 + (cc + 1) * 128],
                                                     rhs=hT47[:, k, 126:128], start=(k == 0), stop=(k == 7)),
                 reads=[b_h47, b_wg], writes=[b_bank[3]])
        P.op("act", lambda g: g.copy(out=small[:, 4:6], in_=banks[2][:, 0:2]), reads=[b_bank[2]], writes=[b_ss[0]])
        P.op("dve", lambda g, cc=cc: g.tensor_tensor(out=carry_v[:, cc, :], in0=banks[3][:, 0:2], in1=small[:, 4:6], op=ALU.mult),
             reads=[b_bank[3], b_ss[0]], writes=[b_carry])
    P.barrier()
    own_stages = [(stageJ, 7), (stageA, 0), (stageB, 1), (stageC, 2), (stageD, 3), (stageE, 4), (stageFo, 5), (stageHI, 6)]
    o0 = max(T0, NT_PRE)
    for step in range(o0, T1 + 7):
        for st, k in own_stages:
            t = step - k
            if o0 <= t < T1:
                st(t)

    if stop_after in ("p1", "p1s"):
        P.barrier(); P.emit(); return nc, es
    P.barrier()
    b_xr = [Buf("xr%d" % i) for i in range(NT_OWN)]
    for t in range(NT_OWN):
        ld("sp", xres[:, t * D:(t + 1) * D], xs_d[(NT_PRE + t) * 128:(NT_PRE + t + 1) * 128, :], b_xr[t], P.dsem())
    b_gB = Buf("gB")
    ds_gB = P.dsem()
    ld("sp", gB[:], gmoe_d.broadcast_to([128, D]), b_gB, ds_gB)

    c2 = Carver(scr2, 7200)
    gs = c2.f32(512)
    Ct = c2.f32(512)
    cu = c2.f32(520)
    acc = c2.f32(512)
    yg = c2.bf16(2048).rearrange("p (c t) -> p c t", c=4)
    ycv = c2.bf16(2048).rearrange("p (c t) -> p c t", c=4)
    h2_t = [c2.f32(1024) for _ in range(2)]
    h2Tf = c2.f32(1024).rearrange("p (k t) -> p k t", k=8)
    b_gs, b_Ct, b_cu, b_acc = Buf("gs"), Buf("Ct"), Buf("cu"), Buf("acc")
    b_yg, b_ycv, b_h2Tf, b_Lg = Buf("yg"), Buf("ycv"), Buf("h2Tf"), Buf("Lg")
    b_h2 = [Buf("h2a"), Buf("h2b")]

    Lg_v = Lg[:].rearrange("p (t n) -> p t n", t=NT_OWN)
    L2 = int(os.environ.get('K_L2', 99))
    b_s2 = [[Buf("s2_%d_%d" % (a, b)) for b in range(2)] for a in range(2)]
    for blk in range(4 if L2 >= 1 else 0):
        tcols = slice(blk * 512, (blk + 1) * 512)
        bts = [b_hT[blk * 4 + t] for t in range(4)]
        for cc in range(4):
            bb = 4 * (cc % 2)
            for k in range(8):
                P.op("pe", lambda g, cc=cc, k=k, bb=bb, tcols=tcols: g.matmul(out=banks[bb][:, 0:512], lhsT=Wgbcu[:, k, cc * 128:(cc + 1) * 128],
                                                                rhs=hT_all_v[:, k, tcols], start=(k == 0), stop=(k == 7)),
                     reads=bts + [b_wg], writes=[b_bank[bb]])
            P.op("act", lambda g, bb=bb: g.activation(out=gs, in_=banks[bb][:, 0:512], func=AF.Silu), reads=[b_bank[bb]], writes=[b_gs])
            P.op("dve", lambda g, cc=cc, tcols=tcols: g.scalar_tensor_tensor(out=yg[:, cc, :], in0=y1_all_v[:, cc, tcols], scalar=ggo[:, 0:1],
                                                                in1=gs, op0=ALU.mult, op1=ALU.mult),
                 reads=[b_gs, b_const] + [b_y1[blk * 4 + t] for t in range(4)], writes=[b_yg])
            for (bk, off) in ((bb + 1, 512), (bb + 2, 1024), (bb + 3, 1536)):
                for k in range(8):
                    P.op("pe", lambda g, cc=cc, k=k, bk=bk, off=off, tcols=tcols: g.matmul(
                        out=banks[bk][:, 0:512], lhsT=Wgbcu[:, k, off + cc * 128:off + (cc + 1) * 128],
                        rhs=hT_all_v[:, k, tcols], start=(k == 0), stop=(k == 7)),
                        reads=bts + [b_wg], writes=[b_bank[bk]])
            P.op("act", lambda g, bb=bb: g.copy(out=Ct, in_=banks[bb + 2][:, 0:512]), reads=[b_bank[bb + 2]], writes=[b_Ct])
            P.op("act", lambda g, cc=cc: g.copy(out=cu[:, 0:2], in_=carry_v[:, cc, :]), reads=[b_carry], writes=[b_cu])
            P.op("dve", lambda g, bb=bb: g.tensor_tensor(out=cu[:, 2:514], in0=banks[bb + 3][:, 0:512], in1=Ct, op=ALU.mult),
                 reads=[b_bank[bb + 3], b_Ct], writes=[b_cu])
            P.op("act", lambda g, cc=cc: g.copy(out=carry_v[:, cc, :], in_=cu[:, 512:514]), reads=[b_cu], writes=[b_carry])
            P.op("dve", lambda g, cc=cc: g.tensor_scalar(out=acc, in0=cu[:, 2:514], scalar1=wconv[:, cc * 3 + 2:cc * 3 + 3], scalar2=None,
                                                         op0=ALU.mult),
                 reads=[b_cu, b_const], writes=[b_acc])
            P.op("dve", lambda g, cc=cc: g.scalar_tensor_tensor(out=acc, in0=cu[:, 1:513], scalar=wconv[:, cc * 3 + 1:cc * 3 + 2], in1=acc,
                                                                op0=ALU.mult, op1=ALU.add),
                 reads=[b_cu, b_const], writes=[b_acc])
            P.op("dve", lambda g, cc=cc: g.scalar_tensor_tensor(out=acc, in0=cu[:, 0:512], scalar=wconv[:, cc * 3:cc * 3 + 1], in1=acc,
                                                                op0=ALU.mult, op1=ALU.add),
                 reads=[b_cu, b_const], writes=[b_acc])
            P.op("dve", lambda g, cc=cc, bb=bb: g.tensor_tensor(out=ycv[:, cc, :], in0=banks[bb + 1][:, 0:512], in1=acc, op=ALU.mult),
                 reads=[b_bank[bb + 1], b_acc], writes=[b_ycv])

        if blk == 3:
            load_expert(0, extra=[b_wg])

        def st2P(t):
            ti = blk * 4 + t
            sl = t % 2
            xr = xres[:, ti * D:(ti + 1) * D]
            for half in range(2):
                for kc in range(8):
                    src = yg if kc < 4 else ycv
                    bsrc = b_yg if kc < 4 else b_ycv
                    P.op("pe", lambda g, half=half, kc=kc, src=src: g.matmul(
                        out=banks[half][:, 0:512], lhsT=src[:, kc % 4, t * 128:(t + 1) * 128],
                        rhs=Wout[:, kc, half * 512:(half + 1) * 512], start=(kc == 0), stop=(kc == 7)),
                        reads=[bsrc, b_wo], writes=[b_bank[half]])
                P.op("dve", lambda g, half=half: g.tensor_tensor(out=xr[:, half * 512:(half + 1) * 512], in0=banks[half][:, 0:512],
                                                                 in1=xr[:, half * 512:(half + 1) * 512], op=ALU.add),
                     reads=[b_bank[half]], writes=[b_xr[ti]])
            c = 2 + 8 * sl
            rms_stats(xr, b_xr[ti], c, c + 1, h2_t[sl].bitcast(BF16)[:, 0:1024], b_h2[sl], b_s2[sl][0], b_s2[sl][1])
            P.op("dve", lambda g: g.scalar_tensor_tensor(out=h2_t[sl], in0=xr, scalar=small[:, c + 1:c + 2], in1=gB[:], op0=ALU.mult, op1=ALU.mult),
                 reads=[b_xr[ti], b_s2[sl][1], b_gB], writes=[b_h2[sl]])

        def st2Q(t):
            ti = blk * 4 + t
            sl = t % 2
            h2 = h2_t[sl]
            for k in range(8):
                bk = 2 + k // 4
                P.op("pe", lambda g, k=k, bk=bk: g.matmul(out=banks[bk][:, (k % 4) * 128:(k % 4 + 1) * 128], lhsT=h2[:, k * 128:(k + 1) * 128],
                                                         rhs=identf[:], start=True, stop=True),
                     reads=[b_h2[sl], b_const], writes=[b_bank[bk]])
            for hh in range(2):
                P.op("dve", lambda g, hh=hh: g.tensor_copy(out=h2Tf[:, hh * 4:(hh + 1) * 4, :],
                                                           in_=banks[2 + hh][:, 0:512].rearrange("p (k t) -> p k t", k=4)),
                     reads=[b_bank[2 + hh]], writes=[b_h2Tf])
            P.op("act", lambda g: g.copy(out=hT_all_v[:, :, ti * 128:(ti + 1) * 128], in_=h2Tf),
                 reads=[b_h2Tf], writes=[b_hT[ti]])

        def st2R(t):
            ti = blk * 4 + t
            for k in range(8):
                P.op("pe", lambda g, k=k: g.matmul(out=banks[4][:, 0:20], lhsT=h2Tf[:, k, :], rhs=wr[:, k * 20:(k + 1) * 20],
                                                  start=(k == 0), stop=(k == 7)),
                     reads=[b_h2Tf, b_const], writes=[b_bank[4]])
            P.op("dve", lambda g: g.tensor_tensor(out=Lg_v[:, ti, :], in0=banks[4][:, 0:20], in1=brb[:], op=ALU.add),
                 reads=[b_bank[4], b_const], writes=[b_Lg])

        for step in range(6):
            if step < 4:
                st2P(step)
            if 2 <= step:
                st2R(step - 2)
            if 1 <= step < 5:
                st2Q(step - 1)

    if stop_after == "p2":
        P.barrier(); P.emit(); return nc, es
    P.barrier()
    b_g5 = Buf("g5")
    ds_g5 = P.dsem()
    ld("sp", gA[:], gple_d.broadcast_to([128, D]), b_g5, ds_g5)
    ld("sp", gB[:], gfin_d.broadcast_to([128, D]), b_g5, ds_g5)
    c3 = Carver(scr2, 7200)
    T16 = NT_OWN
    gmax = c3.f32(16)
    ohg = c3.f32(64)
    ge = c3.f32(64)
    gsum = c3.f32(16)
    pg = c3.f32(16)
    gw = c3.f32(64)
    m1 = c3.f32(64)
    eq1 = c3.f32(256)
    el2 = c3.f32(256)
    m2 = c3.f32(64)
    sel = c3.f32(256)
    dd = c3.f32(256)
    ex = c3.f32(256)
    den = c3.f32(64)
    rden = c3.f32(64)
    wq = c3.f32(256)
    b_r = Buf("route")
    gl = Lg_v[:, :, 0:4]
    el4 = Lg_v[:, :, 4:20].rearrange("p t (g j) -> p t g j", g=4)

    def v3(ap, n=4):
        return ap.rearrange("p (t g) -> p t g", g=n)

    def v4(ap):
        return ap.rearrange("p (t g j) -> p t g j", g=4, j=4)

    def bc3(ap16):
        return ap16.unsqueeze(2).broadcast_to([128, T16, 4])

    def bc4(ap64):
        return ap64.rearrange("p (t g) -> p t g", g=4).unsqueeze(3).broadcast_to([128, T16, 4, 4])

    R = dict(reads=[b_r, b_Lg], writes=[b_r])
    P.op("dve", lambda g: g.tensor_reduce(out=gmax, in_=gl, axis=AX.X, op=ALU.max), **R)
    P.op("dve", lambda g: g.tensor_tensor(out=v3(ohg), in0=gl, in1=bc3(gmax), op=ALU.is_equal), **R)
    P.op("dve", lambda g: g.tensor_tensor(out=v3(ge), in0=gl, in1=bc3(gmax), op=ALU.subtract), **R)
    P.op("act", lambda g: g.activation(out=ge, in_=ge, func=AF.Exp), **R)
    P.op("dve", lambda g: g.tensor_reduce(out=gsum, in_=v3(ge), axis=AX.X, op=ALU.add), **R)
    P.op("dve", lambda g: g.reciprocal(out=pg, in_=gsum), **R)
    P.op("dve", lambda g: g.tensor_tensor(out=v3(gw), in0=v3(ohg), in1=bc3(pg), op=ALU.mult), **R)
    P.op("dve", lambda g: g.tensor_reduce(out=v3(m1), in_=el4, axis=AX.X, op=ALU.max), **R)
    P.op("dve", lambda g: g.tensor_tensor(out=v4(eq1), in0=el4, in1=bc4(m1), op=ALU.is_equal), **R)
    P.op("dve", lambda g: g.scalar_tensor_tensor(out=v4(el2), in0=v4(eq1), scalar=-1e30, in1=el4, op0=ALU.mult, op1=ALU.add), **R)
    P.op("dve", lambda g: g.tensor_reduce(out=v3(m2), in_=v4(el2), axis=AX.X, op=ALU.max), **R)
    P.op("dve", lambda g: g.tensor_tensor(out=v4(sel), in0=el4, in1=bc4(m2), op=ALU.is_ge), **R)
    P.op("dve", lambda g: g.tensor_tensor(out=v4(dd), in0=el4, in1=bc4(m1), op=ALU.subtract), **R)
    P.op("act", lambda g: g.activation(out=ex, in_=dd, func=AF.Exp), **R)
    P.op("dve", lambda g: g.tensor_tensor(out=ex, in0=ex, in1=sel, op=ALU.mult), **R)
    P.op("dve", lambda g: g.tensor_reduce(out=v3(den), in_=v4(ex), axis=AX.X, op=ALU.add), **R)
    P.op("dve", lambda g: g.reciprocal(out=rden, in_=den), **R)
    P.op("dve", lambda g: g.tensor_tensor(out=v4(wq), in0=v4(ex), in1=bc4(rden), op=ALU.mult), **R)
    b_comb = Buf("comb")
    P.op("dve", lambda g: g.tensor_tensor(out=v4(comb[:]), in0=v4(wq), in1=bc4(gw), op=ALU.mult), reads=[b_r], writes=[b_comb])
    comb_v = comb[:].rearrange("p (t e) -> p t e", e=16)

    if stop_after == "p3":
        P.barrier(); P.emit(); return nc, es
    P.barrier()
    c4 = Carver(scr2, 7200)
    hid = [c4.bf16(2048).rearrange("p (c t) -> p c t", c=4) for _ in range(2)]
    sg = [c4.f32(512) for _ in range(2)]
    b_hid = [Buf("hid0"), Buf("hid1")]
    b_sg = [Buf("sg0"), Buf("sg1")]
    cnt4 = {"it": 0, "dn": 0}

    def moe_gu(e, blk):
        s = e % 2
        Wg_e, Wu_e, Wd_e = WE[s]
        tcols = slice(blk * 512, (blk + 1) * 512)
        bts = [b_hT[blk * 4 + t] for t in range(4)]
        hs = (e * 4 + blk) % 2
        for hc in range(4):
            pb = (cnt4["it"] % 2) * 2
            cnt4["it"] += 1
            ss_ = cnt4["it"] % 2
            for k in range(8):
                P.op("pe", lambda g, k=k, hc=hc, pb=pb: g.matmul(
                    out=banks[pb][:, 0:512], lhsT=Wg_e[:, k, hc * 128:(hc + 1) * 128], rhs=hT_all_v[:, k, tcols],
                    start=(k == 0), stop=(k == 7)), reads=bts + [b_we[s]], writes=[b_bank[pb]])
            for k in range(8):
                P.op("pe", lambda g, k=k, hc=hc, pb=pb: g.matmul(
                    out=banks[pb + 1][:, 0:512], lhsT=Wu_e[:, k, hc * 128:(hc + 1) * 128], rhs=hT_all_v[:, k, tcols],
                    start=(k == 0), stop=(k == 7)), reads=bts + [b_we[s]], writes=[b_bank[pb + 1]])
            P.op("act", lambda g, pb=pb, ss_=ss_: g.activation(out=sg[ss_], in_=banks[pb][:, 0:512], func=AF.Silu),
                 reads=[b_bank[pb]], writes=[b_sg[ss_]])
            P.op("dve", lambda g, pb=pb, ss_=ss_, hc=hc: g.tensor_tensor(out=hid[hs][:, hc, :], in0=banks[pb + 1][:, 0:512],
                                                                         in1=sg[ss_], op=ALU.mult),
                 reads=[b_bank[pb + 1], b_sg[ss_]], writes=[b_hid[hs]])

    def moe_dn(e, blk):
        s = e % 2
        Wg_e, Wu_e, Wd_e = WE[s]
        hs = (e * 4 + blk) % 2
        for t in range(4):
            ti = blk * 4 + t
            xr = xres[:, ti * D:(ti + 1) * D]
            db = 4 + (cnt4["dn"] % 2) * 2
            cnt4["dn"] += 1
            for half in range(2):
                for hc in range(4):
                    P.op("pe", lambda g, t=t, half=half, hc=hc, db=db: g.matmul(
                        out=banks[db + half][:, 0:512], lhsT=hid[hs][:, hc, t * 128:(t + 1) * 128],
                        rhs=Wd_e[:, hc, half * 512:(half + 1) * 512], start=(hc == 0), stop=(hc == 3)),
                        reads=[b_hid[hs], b_we[s]], writes=[b_bank[db + half]])
                P.op("dve", lambda g, half=half, db=db, xr=xr, ti=ti: g.scalar_tensor_tensor(
                    out=xr[:, half * 512:(half + 1) * 512], in0=banks[db + half][:, 0:512], scalar=comb_v[:, ti, e:e + 1],
                    in1=xr[:, half * 512:(half + 1) * 512], op0=ALU.mult, op1=ALU.add),
                    reads=[b_bank[db + half], b_comb], writes=[b_xr[ti]])

    prev = None
    for e in range(NE):
        if e > 0:
            load_expert(e)
        for blk in range(4):
            moe_gu(e, blk)
            if prev is not None:
                moe_dn(*prev)
            prev = (e, blk)
            if e == NE - 1 and blk == 0:
                ld("pool", Wpg, wpg_d.rearrange("(k p) c -> p k c", p=128), [b_w5, b_we[0]], ds_w5)
                ld("pool", Wpp, wpp_d.rearrange("(k p) c -> p k c", p=128), [b_w5, b_we[0]], ds_w5)
    moe_dn(*prev)

    if stop_after == "p4":
        P.barrier(); P.emit(); return nc, es
    P.barrier()
    c5 = Carver(scr2, 7200)
    pt = [c5.f32(256) for _ in range(2)]
    ptb = [c5.bf16(256) for _ in range(2)]
    pT = [c5.bf16(256).rearrange("p (k t) -> p k t", k=2) for _ in range(2)]
    h3b = [c5.bf16(1024) for _ in range(2)]
    h3T = [c5.bf16(1024).rearrange("p (k t) -> p k t", k=8) for _ in range(2)]
    sig = c5.f32(1024)
    outst = [c5.f32(1024) for _ in range(2)]
    b_pt = [Buf("pt0"), Buf("pt1")]
    ds_pt = [P.dsem(), P.dsem()]
    b_ptb, b_pT = [Buf("ptb0"), Buf("ptb1")], [Buf("pT0"), Buf("pT1")]
    b_h3b, b_h3T = [Buf("h3b0"), Buf("h3b1")], [Buf("h3T0"), Buf("h3T1")]
    b_sig = Buf("sig")
    b_out = [Buf("o0"), Buf("o1")]
    ds_out = [P.dsem(), P.dsem()]
    b_s5 = [[Buf("s5_%d_%d" % (a, b)) for b in range(4)] for a in range(2)]
    bank1b = banks[1][:, 0:512].bitcast(BF16)
    last_store = []

    def st5X(ti):
        sl = ti % 2
        xr = xres[:, ti * D:(ti + 1) * D]
        ld("sp", pt[sl], p_d[ti * 128:(ti + 1) * 128, :], b_pt[sl], ds_pt[sl])
        c = 4 + 8 * sl
        rms_stats(xr, b_xr[ti], c, c + 1, h3b[sl], b_h3b[sl], b_s5[sl][0], b_s5[sl][1])
        P.op("dve", lambda g: g.scalar_tensor_tensor(out=h3b[sl], in0=xr, scalar=small[:, c + 1:c + 2], in1=gA[:], op0=ALU.mult, op1=ALU.mult),
             reads=[b_xr[ti], b_s5[sl][1], b_g5], writes=[b_h3b[sl]])
        P.op("dve", lambda g: g.tensor_copy(out=ptb[sl], in_=pt[sl]), reads=[b_pt[sl]], writes=[b_ptb[sl]])

    def st5Y(ti):
        sl = ti % 2
        for k in range(8):
            P.op("pe", lambda g, k=k: g.transpose(out=bank0b[:, k * 128:(k + 1) * 128], in_=h3b[sl][:, k * 128:(k + 1) * 128], identity=identb[:]),
                 reads=[b_h3b[sl], b_const], writes=[b_bank[0]])
        P.op("dve", lambda g: g.tensor_copy(out=h3T[sl], in_=bank0b.rearrange("p (k t) -> p k t", k=8)), reads=[b_bank[0]], writes=[b_h3T[sl]])
        for k in range(2):
            P.op("pe", lambda g, k=k: g.transpose(out=bank1b[:, k * 128:(k + 1) * 128], in_=ptb[sl][:, k * 128:(k + 1) * 128], identity=identb[:]),
                 reads=[b_ptb[sl], b_const], writes=[b_bank[1]])
        P.op("dve", lambda g: g.tensor_copy(out=pT[sl], in_=bank1b[:, 0:256].rearrange("p (k t) -> p k t", k=2)), reads=[b_bank[1]], writes=[b_pT[sl]])

    def st5Z(ti):
        sl = ti % 2
        xr = xres[:, ti * D:(ti + 1) * D]
        for half in range(2):
            for k in range(8):
                P.op("pe", lambda g, k=k, half=half: g.matmul(out=banks[2 + half][:, 0:512], lhsT=h3T[sl][:, k, :],
                                                             rhs=Wpg[:, k, half * 512:(half + 1) * 512], start=(k == 0), stop=(k == 7)),
                     reads=[b_h3T[sl], b_w5], writes=[b_bank[2 + half]])
            for k in range(2):
                P.op("pe", lambda g, k=k, half=half: g.matmul(out=banks[4 + half][:, 0:512], lhsT=pT[sl][:, k, :],
                                                             rhs=Wpp[:, k, half * 512:(half + 1) * 512], start=(k == 0), stop=(k == 1)),
                     reads=[b_pT[sl], b_w5], writes=[b_bank[4 + half]])
            P.op("act", lambda g, half=half: g.activation(out=sig[:, half * 512:(half + 1) * 512], in_=banks[2 + half][:, 0:512], func=AF.Sigmoid),
                 reads=[b_bank[2 + half]], writes=[b_sig])
            P.op("dve", lambda g, half=half: g.tensor_tensor(out=sig[:, half * 512:(half + 1) * 512], in0=banks[4 + half][:, 0:512],
                                                             in1=sig[:, half * 512:(half + 1) * 512], op=ALU.mult),
                 reads=[b_bank[4 + half], b_sig], writes=[b_sig])
        P.op("dve", lambda g: g.tensor_tensor(out=xr, in0=sig, in1=xr, op=ALU.add), reads=[b_sig], writes=[b_xr[ti]])

    def st5W(ti):
        sl = ti % 2
        xr = xres[:, ti * D:(ti + 1) * D]
        c = 6 + 8 * sl
        rms_stats(xr, b_xr[ti], c, c + 1, outst[sl].bitcast(BF16)[:, 0:1024], b_out[sl], b_s5[sl][2], b_s5[sl][3])
        P.op("dve", lambda g: g.scalar_tensor_tensor(out=outst[sl], in0=xr, scalar=small[:, c + 1:c + 2], in1=gB[:], op0=ALU.mult, op1=ALU.mult),
             reads=[b_xr[ti], b_s5[sl][3], b_g5], writes=[b_out[sl]])
        tok = P.op("pool", lambda g: g.dma_start(out=y_d[ti * 128:(ti + 1) * 128, :], in_=outst[sl]),
                   reads=[b_out[sl]], dsem=ds_out[sl])
        last_store.append(tok)

    st5 = [st5X, st5Y, st5Z, st5W]
    for step in range(NT_OWN + len(st5) - 1):
        for k, st in enumerate(st5):
            t = step - k
            if 0 <= t < NT_OWN:
                st(t)
    P.wait_only("pool", last_store[-2:])
    P.wait_only("sp", last_store[-2:])
    P.emit()
    return nc, es


_CACHE = {}


def _consts():
    s = np.arange(128)[:, None]
    t = np.arange(128)[None, :]
    tril = np.where(s <= t, -1.0 / 16.0, 0.0).astype(np.float32)
    triu = np.where(s > t, -1.0 / 16.0, 0.0).astype(np.float32)
    mask = np.tile((s <= t).astype(np.float32), (1, 4))
    return {
        "c_ident": np.eye(128, dtype=np.float32),
        "c_tril": tril,
        "c_triu": triu,
        "c_mask": np.ascontiguousarray(mask),
        "c_ones": np.full((128, 128), 1.0 / 128.0, np.float32),
        "c_neg": np.full((128, 1), -1.0 / 16.0, np.float32),
    }


def kernel(x, p, g_mix, w_in, w_gla_gate, b_gla_gate, g_gla_out, w_conv, w_out,
           g_moe, w_group, b_group, w_router, b_router, w_exp_gate, w_exp_up, w_exp_down,
           g_ple, w_ple_gate, w_ple_proj, g_final):
    f = lambda a: np.ascontiguousarray(np.asarray(a, dtype=np.float32))
    x = f(x); p = f(p)
    if "nc" not in _CACHE:
        _CACHE["nc"] = build_program()
    nc, _es = _CACHE["nc"]
    wg_aug = np.zeros((32, 256), np.float32)
    wg_aug[0:16] = f(w_gla_gate)[0]
    wg_aug[16] = f(b_gla_gate)[0]
    shared = {
        "w_in": f(w_in)[0], "w_out": f(w_out)[0],
        "w_exp_gate": f(w_exp_gate)[0].reshape(NE * D, 512),
        "w_exp_up": f(w_exp_up)[0].reshape(NE * D, 512),
        "w_exp_down": f(w_exp_down)[0].reshape(NE * 512, D),
        "w_ple_gate": f(w_ple_gate)[0], "w_ple_proj": f(w_ple_proj)[0],
        "g_mix": f(g_mix).reshape(1, D), "g_moe": f(g_moe).reshape(1, D), "g_ple": f(g_ple).reshape(1, D),
        "g_final": f(g_final).reshape(1, D),
        "w_rt": np.ascontiguousarray(np.concatenate([f(w_group)[0], f(w_router)[0]], axis=1)),
        "b_rt": np.ascontiguousarray(np.concatenate([f(b_group)[0], f(b_router)[0]], axis=0).reshape(1, 20)),
        "wg_aug": wg_aug,
        "wconv_t": np.ascontiguousarray(f(w_conv)[0].reshape(3, 4, 128).transpose(2, 1, 0).reshape(128, 12)),
        "ggo": np.ascontiguousarray(f(g_gla_out)[0].reshape(128, 1)),
    }
    shared.update(_consts())
    in_maps = []
    for c in range(8):
        b, j = c // 4, c % 4
        xs = np.zeros((NT_ALL * 128, D), np.float32)
        n = 2048 * (j + 1)
        xs[NT_ALL * 128 - n:] = x[b, 0:n]
        m = dict(shared)
        m["xs"] = xs
        m["p_own"] = np.ascontiguousarray(p[0, b, 2048 * j:2048 * (j + 1)])
        in_maps.append(m)
    res = run_bass_kernel_spmd(nc, in_maps, core_ids=list(range(8)))
    out = np.empty((2, 8192, D), np.float32)
    for c in range(8):
        b, j = c // 4, c % 4
        out[b, 2048 * j:2048 * (j + 1)] = res.results[c]["y"]
    return out
```

```python
import numpy as np
from contextlib import ExitStack
import concourse.bass as bass
import concourse.mybir as mybir
from concourse.bass_utils import run_bass_kernel_spmd

F32 = mybir.dt.float32
BF16 = mybir.dt.bfloat16
AF = mybir.ActivationFunctionType
ALU = mybir.AluOpType
AX = mybir.AxisListType

EPS = 1e-6
NT_ALL = 64
NT_OWN = 16
NT_PRE = NT_ALL - NT_OWN
D = 1024
DIN = 3088
NE = 16


class Buf:
    __slots__ = ("name", "w", "r")

    def __init__(self, name):
        self.name = name
        self.w = None
        self.r = []


class DSem:
    def __init__(self, h):
        self.h = h
        self.count = 0
        self.nobar = False


class Prog:
    def __init__(self, nc, es):
        self.nc = nc
        self.es = es
        self.names = ["pe", "act", "dve", "pool", "sp"]
        self.streams = {k: [] for k in self.names}
        self.sem = {k: es.enter_context(nc.semaphore("c_" + k)) for k in ["pe", "act", "dve"]}
        self.cnt = {k: 0 for k in self.sem}
        self.known = {k: {} for k in self.names}
        self.handles = {}
        self.dsems = []
        self.nds = 0

    def dsem(self):
        self.nds += 1
        s = DSem(self.es.enter_context(self.nc.semaphore("d%d" % self.nds)))
        self.dsems.append(s)
        return s

    def _filter(self, e, toks):
        need = {}
        for (s, v) in toks:
            if e == "pe" and s is self.sem["pe"]:
                continue
            k = id(s)
            self.handles[k] = s
            if need.get(k, 0) < v:
                need[k] = v
        out = []
        kn = self.known[e]
        for k, v in need.items():
            if kn.get(k, 0) < v:
                kn[k] = v
                out.append((self.handles[k], v))
        return out

    def op(self, e, fn, reads=(), writes=(), dsem=None):
        toks = []
        for b in reads:
            if b.w is not None:
                toks.append(b.w)
        for b in writes:
            toks.extend(b.r)
            if b.w is not None:
                toks.append(b.w)
        waits = self._filter(e, toks)
        if dsem is None:
            self.cnt[e] += 1
            tok = (self.sem[e], self.cnt[e])
            inc = 1
        else:
            dsem.count += 16
            tok = (dsem.h, dsem.count)
            inc = 16
        self.streams[e].append((waits, fn, tok[0], inc))
        for b in writes:
            b.w = tok
            b.r = []
        for b in reads:
            b.r.append(tok)
        return tok

    def wait_only(self, e, toks):
        waits = self._filter(e, toks)
        if waits:
            self.streams[e].append((waits, None, None, 0))

    def barrier(self):
        toks = [(self.sem[k], self.cnt[k]) for k in self.sem if self.cnt[k] > 0]
        toks += [(d.h, d.count) for d in self.dsems if d.count > 0 and not d.nobar]
        for e in self.names:
            self.wait_only(e, toks)

    def emit(self):
        nc = self.nc
        with nc.Block() as block:
            decos = {"pe": block.tensor, "act": block.scalar, "dve": block.vector,
                     "pool": block.gpsimd, "sp": block.sync}
            for k in self.names:
                stream = self.streams[k]

                def body(engine, stream=stream):
                    for waits, fn, sem, inc in stream:
                        for (s, v) in waits:
                            engine.wait_ge(s, v)
                        if fn is not None:
                            ins = fn(engine)
                            ins.then_inc(sem, inc)

                decos[k](body)


def build_program(stop_after=None):
    nc = bass.Bass("TRN2", target_bir_lowering=False)
    es = ExitStack()

    def din(name, shape):
        return nc.dram_tensor(name, list(shape), F32, kind="ExternalInput").ap()

    xs_d = din("xs", [NT_ALL * 128, D])
    p_d = din("p_own", [NT_OWN * 128, 256])
    w_in_d = din("w_in", [D, DIN])
    w_out_d = din("w_out", [D, D])
    wg_d = din("w_exp_gate", [NE * D, 512])
    wu_d = din("w_exp_up", [NE * D, 512])
    wd_d = din("w_exp_down", [NE * 512, D])
    wpg_d = din("w_ple_gate", [D, D])
    wpp_d = din("w_ple_proj", [256, D])
    gmix_d = din("g_mix", [1, D])
    gmoe_d = din("g_moe", [1, D])
    gple_d = din("g_ple", [1, D])
    gfin_d = din("g_final", [1, D])
    wr_d = din("w_rt", [D, 20])
    br_d = din("b_rt", [1, 20])
    wgate_d = din("wg_aug", [32, 256])
    wconv_d = din("wconv_t", [128, 12])
    ggo_d = din("ggo", [128, 1])
    cid_d = din("c_ident", [128, 128])
    ctl_d = din("c_tril", [128, 128])
    ctu_d = din("c_triu", [128, 128])
    cmask_d = din("c_mask", [128, 512])
    cones_d = din("c_ones", [128, 128])
    cneg_d = din("c_neg", [128, 1])
    y_d = nc.dram_tensor("y", [NT_OWN * 128, D], F32, kind="ExternalOutput").ap()

    def sb(name, shape, dt):
        return es.enter_context(nc.sbuf_tensor("s_" + name, list(shape), dt))

    xres = sb("xres", [128, NT_OWN * D], F32)
    hT_all = sb("hT_all", [128, 8 * 2048], BF16)
    y1_all = sb("y1_all", [128, 4 * 2048], BF16)
    wbig = sb("wbig", [128, 24704], BF16)
    scr2 = sb("scr2", [128, 7200], F32)
    carry_t = sb("carry", [128, 8], F32)
    identb = sb("identb", [128, 128], BF16)
    identf = sb("identf", [128, 128], F32)
    tril = sb("tril", [128, 128], F32)
    triu = sb("triu", [128, 128], F32)
    maskb = sb("maskb", [128, 512], BF16)
    onesb = sb("onesb", [128, 128], BF16)
    negcol = sb("negcol", [128, 1], F32)
    gA = sb("gA", [128, D], F32)
    gB = sb("gB", [128, D], F32)
    wr = sb("wr", [128, 8 * 20], F32)
    brb = sb("brb", [128, 20], F32)
    wgate = sb("wgate", [32, 256], F32)
    wconv = sb("wconv", [128, 12], F32)
    ggo = sb("ggo", [128, 1], F32)
    Lg = sb("Lg", [128, NT_OWN * 20], F32)
    comb = sb("comb", [128, NT_OWN * 16], F32)
    small = sb("small", [128, 16], F32)

    banks = [es.enter_context(nc.psum_tensor("bank%d" % i, [128, 512], F32)) for i in range(8)]

    P = Prog(nc, es)

    class Carver:
        def __init__(self, base, nwords):
            self.base = base
            self.off = 0
            self.n = nwords

        def f32(self, n):
            a = self.base[:, self.off:self.off + n]
            self.off += n
            assert self.off <= self.n, (self.off, self.n)
            return a

        def bf16(self, n):
            assert n % 2 == 0
            return self.f32(n // 2).bitcast(BF16)

    b_const = Buf("const")
    ds_const = P.dsem()

    def ld(e, out_ap, in_ap, buf, dsem):
        bufs = buf if isinstance(buf, (list, tuple)) else [buf]
        return P.op(e, lambda g: g.dma_start(out=out_ap, in_=in_ap), writes=list(bufs), dsem=dsem)

    ld("sp", identf[:], cid_d, b_const, ds_const)
    ld("sp", tril[:], ctl_d, b_const, ds_const)
    ld("sp", triu[:], ctu_d, b_const, ds_const)
    ld("sp", negcol[:], cneg_d, b_const, ds_const)
    ld("sp", gA[:], gmix_d.broadcast_to([128, D]), b_const, ds_const)
    ld("sp", wr[:].rearrange("p (k n) -> p k n", k=8), wr_d.rearrange("(k p) n -> p k n", p=128), b_const, ds_const)
    ld("sp", brb[:], br_d.broadcast_to([128, 20]), b_const, ds_const)
    ld("sp", wgate[:], wgate_d, b_const, ds_const)
    ld("sp", wconv[:], wconv_d, b_const, ds_const)
    ld("sp", ggo[:], ggo_d, b_const, ds_const)
    ds_const2 = P.dsem()
    ld("pool", identb[:], cid_d, b_const, ds_const2)
    ld("pool", maskb[:], cmask_d, b_const, ds_const2)
    ld("pool", onesb[:], cones_d, b_const, ds_const2)

    c1 = Carver(xres, NT_OWN * D)
    Wqkva = c1.bf16(8 * 1296).rearrange("p (k c) -> p k c", k=8)
    xs_t = [c1.f32(1024) for _ in range(2)]
    hb_t = [c1.bf16(1024) for _ in range(2)]
    hts_off = c1.off
    hTs = [c1.bf16(1024).rearrange("p (k t) -> p k t", k=8) for _ in range(4)]
    aT_t = [c1.f32(128) for _ in range(2)]
    e1 = c1.f32(256)
    sp_t = [c1.f32(256) for _ in range(2)]
    Er = c1.f32(256)
    El_t = [c1.f32(8) for _ in range(2)]
    kd_t = [c1.bf16(256) for _ in range(2)]
    vb_t = [c1.bf16(512) for _ in range(2)]
    Eq = c1.f32(256)
    Ek = c1.f32(256)
    qt_t = [c1.bf16(256) for _ in range(2)]
    kt_t = [c1.bf16(256) for _ in range(2)]
    qkT0 = c1.bf16(1024)
    Pm = c1.bf16(512)
    S = c1.f32(512)
    Sb0 = c1.bf16(512)
    sq0 = c1.bf16(512)
    rstd = c1.f32(512)
    c1o = Carver(xres, hts_off + 1536)
    c1o.off = hts_off
    vb4 = [vb_t[0], vb_t[1], c1o.bf16(512), c1o.bf16(512)]
    qkT_t = [qkT0, c1o.bf16(1024)]
    Sb_t = [Sb0, c1o.bf16(512)]
    sq_t = [sq0, c1o.bf16(512)]

    w_in_v = w_in_d.rearrange("(k p) c -> p k c", p=128)
    b_wqkva = Buf("wqkva")
    ds_w1 = P.dsem()
    b_wa = Buf("wa")
    ds_wa = P.dsem()
    ld("pool", Wqkva[:, :, 1024:1040], w_in_v[:, :, 1536:1552], b_wa, ds_wa)
    ld("pool", Wqkva[:, :, 256:1024], w_in_v[:, :, 256:1024], b_wqkva, ds_w1)
    b_wq = Buf("wq")
    ds_wq = P.dsem()
    ld("pool", Wqkva[:, :, 0:256], w_in_v[:, :, 0:256], b_wq, ds_wq)

    Wgbcu = wbig[:, 0:8 * 2048].rearrange("p (k c) -> p k c", k=8)
    Wout = wbig[:, 8 * 2048:8 * 3072].rearrange("p (k c) -> p k c", k=8)
    b_wg = Buf("wgbcu")
    b_wo = Buf("wout")
    ds_w2 = P.dsem()
    ds_wo = P.dsem()
    ld("pool", Wgbcu[:, :, 0:512], w_in_v[:, :, 1024:1536], b_wg, ds_w2)
    ld("pool", Wgbcu[:, :, 512:2048], w_in_v[:, :, 1552:3088], b_wg, ds_w2)
    ld("pool", Wout, w_out_d.rearrange("(k p) c -> p k c", p=128), b_wo, ds_wo)
    WE = []
    for s_ in range(2):
        base = s_ * 12288
        WE.append((wbig[:, base:base + 4096].rearrange("p (k c) -> p k c", k=8),
                   wbig[:, base + 4096:base + 8192].rearrange("p (k c) -> p k c", k=8),
                   wbig[:, base + 8192:base + 12288].rearrange("p (k c) -> p k c", k=4)))
    b_we = [Buf("we0"), Buf("we1")]
    ds_we = [P.dsem(), P.dsem()]
    ds_we[0].nobar = True
    ds_we[1].nobar = True
    Wpg = wbig[:, 0:8192].rearrange("p (k c) -> p k c", k=8)
    Wpp = wbig[:, 8192:8192 + 2048].rearrange("p (k c) -> p k c", k=2)
    b_w5 = Buf("w5")
    ds_w5 = P.dsem()
    ds_w5.nobar = True

    def load_expert(e, extra=()):
        s_ = e % 2
        Wg_e, Wu_e, Wd_e = WE[s_]
        bufs = [b_we[s_]] + list(extra)
        ld("pool", Wg_e, wg_d[e * D:(e + 1) * D, :].rearrange("(k p) c -> p k c", p=128), bufs, ds_we[s_])
        ld("pool", Wu_e, wu_d[e * D:(e + 1) * D, :].rearrange("(k p) c -> p k c", p=128), bufs, ds_we[s_])
        ld("pool", Wd_e, wd_d[e * 512:(e + 1) * 512, :].rearrange("(k p) c -> p k c", p=128), bufs, ds_we[s_])

    if stop_after == "consts":
        P.barrier(); P.emit(); return nc, es
    import os
    b_xs = [Buf("xs0"), Buf("xs1")]
    ds_xs = [P.dsem(), P.dsem()]
    b_hb = [Buf("hb0"), Buf("hb1")]
    b_hTs = [Buf("hTs%d" % i) for i in range(4)]
    b_hT = [Buf("hT%d" % i) for i in range(NT_OWN)]
    b_y1 = [Buf("y1_%d" % i) for i in range(NT_OWN)]
    b_bank = [Buf("bank%d" % i) for i in range(8)]
    b_aTp = Buf("aTp")
    b_lastp = Buf("lastp")
    b_zp = Buf("zp")
    b_aT = [Buf("aT0"), Buf("aT1")]
    b_e1 = Buf("e1")
    b_sp = [Buf("sp0"), Buf("sp1")]
    b_Eq, b_Ek, b_Er = Buf("Eq"), Buf("Ek"), Buf("Er")
    b_El = [Buf("El0"), Buf("El1")]
    b_kd = [Buf("kd0"), Buf("kd1")]
    b_vb = [Buf("vb0"), Buf("vb1")]
    b_qt, b_kt = [Buf("qt0"), Buf("qt1")], [Buf("kt0"), Buf("kt1")]
    b_Pm, b_S, b_rstd = Buf("Pm"), Buf("S"), Buf("rstd")
    b_qkT, b_Sb, b_sq = [Buf("qkT0"), Buf("qkT1")], [Buf("Sb0"), Buf("Sb1")], [Buf("sq0"), Buf("sq1")]
    b_vb4 = [b_vb[0], b_vb[1], Buf("vb2"), Buf("vb3")]

    def vb_of(i):
        if i >= NT_PRE:
            return vb4[i % 4], b_vb4[i % 4]
        return vb_t[i % 2], b_vb[i % 2]
    b_ss = [Buf("ss0"), Buf("ss1")]
    b_rs = [Buf("rs0"), Buf("rs1")]

    bank0b = banks[0][:, 0:512].bitcast(BF16)
    bank7b = banks[7][:, 0:512].bitcast(BF16)

    hT_all_v = hT_all[:].rearrange("p (k t) -> p k t", k=8)
    y1_all_v = y1_all[:].rearrange("p (h t) -> p h t", h=4)
    S_v = S.rearrange("p (h e) -> p h e", h=4)

    P.op("dve", lambda g: g.memset(S[0:64, :], 0.0), writes=[b_S])
    for a_ in range(2):
        P.op("dve", lambda g, a_=a_: g.memset(aT_t[a_][0:32, :], 1.0), writes=[b_aT[a_]])

    def rms_stats(src_ap, b_src, ss_col, rs_col, junk, b_junk, bss=None, brs=None):
        bss = bss or b_ss[0]
        brs = brs or b_rs[0]
        P.op("act", lambda g: g.activation(out=junk, in_=src_ap, func=AF.Square, accum_out=small[:, ss_col:ss_col + 1]),
             reads=[b_src], writes=[b_junk, bss])
        P.op("act", lambda g: g.activation(out=small[:, rs_col:rs_col + 1], in_=small[:, ss_col:ss_col + 1], func=AF.Ln,
                                           scale=1.0 / D, bias=EPS),
             reads=[bss], writes=[brs])
        P.op("act", lambda g: g.activation(out=small[:, rs_col:rs_col + 1], in_=small[:, rs_col:rs_col + 1], func=AF.Exp, scale=-0.5),
             reads=[brs], writes=[brs])

    def hT_of(i):
        if i >= NT_PRE:
            oi = i - NT_PRE
            return hT_all_v[:, :, oi * 128:(oi + 1) * 128], b_hT[oi]
        return hTs[i % 4], b_hTs[i % 4]

    def stageA(i):
        sl = i % 2
        xt = xs_t[sl]
        ld("sp", xt, xs_d[i * 128:(i + 1) * 128, :], b_xs[sl], ds_xs[sl])
        hb = hb_t[sl]
        rms_stats(xt, b_xs[sl], 8 * sl, 8 * sl + 1, hb, b_hb[sl], b_ss[sl], b_rs[sl])
        P.op("dve", lambda g: g.scalar_tensor_tensor(out=hb, in0=xt, scalar=small[:, 8 * sl + 1:8 * sl + 2], in1=gA[:],
                                                     op0=ALU.mult, op1=ALU.mult),
             reads=[b_xs[sl], b_rs[sl], b_const], writes=[b_hb[sl]])

    def stageB(i):
        sl = i % 2
        hb = hb_t[sl]
        hT, bh = hT_of(i)
        for k in range(8):
            P.op("pe", lambda g, k=k: g.transpose(out=bank0b[:, k * 128:(k + 1) * 128], in_=hb[:, k * 128:(k + 1) * 128],
                                                 identity=identb[:]),
                 reads=[b_hb[sl], b_const], writes=[b_bank[0]])
        P.op("act", lambda g: g.copy(out=hT, in_=bank0b.rearrange("p (k t) -> p k t", k=8)),
             reads=[b_bank[0]], writes=[bh])

    def stageC(i):
        sl = i % 2
        hT, bh = hT_of(i)
        for k in range(8):
            P.op("pe", lambda g, k=k: g.matmul(out=banks[1][0:16, 0:128], lhsT=Wqkva[:, k, 1024:1040], rhs=hT[:, k, :],
                                              start=(k == 0), stop=(k == 7)),
                 reads=[bh, b_wa], writes=[b_bank[1]])
        P.op("act", lambda g: g.copy(out=aT_t[sl][0:16, :], in_=banks[1][0:16, 0:128]), reads=[b_bank[1]], writes=[b_aT[sl]])

    def stageD(i):
        sl = i % 2
        P.op("pe", lambda g: g.matmul(out=banks[2][:, 256:512], lhsT=aT_t[sl][0:17, :], rhs=wgate[0:17, :], start=True, stop=True),
             reads=[b_aT[sl], b_const], writes=[b_bank[2]])
        P.op("act", lambda g: g.activation(out=e1, in_=banks[2][:, 256:512], func=AF.Exp, scale=-1.0), reads=[b_bank[2]], writes=[b_e1])
        P.op("act", lambda g: g.activation(out=sp_t[sl], in_=e1, func=AF.Ln, bias=1.0), reads=[b_e1], writes=[b_sp[sl]])

    def stageE(i):
        own = i >= NT_PRE
        sl = i % 2
        hT, bh = hT_of(i)
        spb = sp_t[sl]
        if own:
            P.op("pe", lambda g: g.matmul(out=banks[3][:, 0:256], lhsT=tril[:], rhs=spb, start=True, stop=True),
                 reads=[b_sp[sl], b_const], writes=[b_bank[3]])
        P.op("pe", lambda g: g.matmul(out=banks[3][:, 256:512], lhsT=triu[:], rhs=spb, start=True, stop=True),
             reads=[b_sp[sl], b_const], writes=[b_bank[3]])
        for h in range(4):
            P.op("pe", lambda g, h=h: g.matmul(out=banks[7][0:64, 128 + h:129 + h], lhsT=spb[:, h * 64:(h + 1) * 64], rhs=negcol[:],
                                              start=True, stop=True),
                 reads=[b_sp[sl], b_const], writes=[b_bank[7]])
        if own:
            ncol, c0 = 512, 0
        else:
            ncol, c0 = 256, 256
        for k in range(8):
            P.op("pe", lambda g, k=k: g.matmul(out=banks[4][:, 0:ncol], lhsT=hT[:, k, :], rhs=Wqkva[:, k, c0:c0 + ncol],
                                              start=(k == 0), stop=(k == 7)),
                 reads=[bh, b_wqkva, b_wq], writes=[b_bank[4]])
        for k in range(8):
            P.op("pe", lambda g, k=k: g.matmul(out=banks[5][:, 0:512], lhsT=hT[:, k, :], rhs=Wqkva[:, k, 512:1024],
                                              start=(k == 0), stop=(k == 7)),
                 reads=[bh, b_wqkva], writes=[b_bank[5]])
        P.op("act", lambda g: g.activation(out=Er, in_=banks[3][:, 256:512], func=AF.Exp), reads=[b_bank[3]], writes=[b_Er])
        if own:
            P.op("act", lambda g: g.activation(out=Eq, in_=banks[3][:, 0:256], func=AF.Exp), reads=[b_bank[3]], writes=[b_Eq])
            P.op("act", lambda g: g.activation(out=Ek, in_=banks[3][:, 0:256], func=AF.Exp, scale=-1.0), reads=[b_bank[3]], writes=[b_Ek])
        P.op("act", lambda g: g.activation(out=El_t[sl][0:64, 0:4], in_=banks[7][0:64, 128:132], func=AF.Exp),
             reads=[b_bank[7]], writes=[b_El[sl]])
        kcol = 256 if own else 0
        P.op("dve", lambda g: g.tensor_tensor(out=kd_t[sl], in0=banks[4][:, kcol:kcol + 256], in1=Er, op=ALU.mult),
             reads=[b_bank[4], b_Er], writes=[b_kd[sl]])
        if own:
            P.op("dve", lambda g: g.scalar_tensor_tensor(out=qt_t[sl], in0=banks[4][:, 0:256], scalar=0.125, in1=Eq, op0=ALU.mult, op1=ALU.mult),
                 reads=[b_bank[4], b_Eq], writes=[b_qt[sl]])
            P.op("dve", lambda g: g.tensor_tensor(out=kt_t[sl], in0=banks[4][:, 256:512], in1=Ek, op=ALU.mult),
                 reads=[b_bank[4], b_Ek], writes=[b_kt[sl]])
        vb, bvb = vb_of(i)
        P.op("dve", lambda g: g.tensor_copy(out=vb, in_=banks[5][:, 0:512]), reads=[b_bank[5]], writes=[bvb])

    def stageF(i):
        sl = i % 2
        kd = kd_t[sl]
        vb, bvb = vb_of(i)
        for h in range(4):
            P.op("pe", lambda g, h=h: g.matmul(out=banks[6][0:64, h * 128:(h + 1) * 128], lhsT=kd[:, h * 64:(h + 1) * 64],
                                              rhs=vb[:, h * 128:(h + 1) * 128], start=True, stop=True),
                 reads=[b_kd[sl], bvb], writes=[b_bank[6]])

    def stageS(i):
        sl = i % 2
        P.op("dve", lambda g: g.tensor_tensor(out=S_v[0:64, :, :], in0=S_v[0:64, :, :],
                                              in1=El_t[sl][0:64, 0:4].unsqueeze(2).broadcast_to([64, 4, 128]), op=ALU.mult),
             reads=[b_S, b_El[sl]], writes=[b_S])
        P.op("dve", lambda g: g.tensor_tensor(out=S[0:64, :], in0=S[0:64, :], in1=banks[6][0:64, 0:512], op=ALU.add),
             reads=[b_S, b_bank[6]], writes=[b_S])

    def stageFo(i):
        sl = i % 2
        stageF(i)
        P.op("act", lambda g: g.copy(out=Sb_t[sl][0:64, :], in_=S[0:64, :]), reads=[b_S], writes=[b_Sb[sl]])
        stageS(i)
        for j in range(8):
            src = qt_t[sl] if j < 4 else kt_t[sl]
            a = j % 4
            P.op("pe", lambda g, j=j, src=src, a=a: g.transpose(out=bank7b[0:64, j * 128:(j + 1) * 128], in_=src[:, a * 64:(a + 1) * 64],
                                                               identity=identb[:]),
                 reads=[b_qt[sl], b_kt[sl], b_const], writes=[b_bank[7]])
        P.op("act", lambda g: g.copy(out=qkT_t[sl][0:64, :], in_=bank7b[0:64, :]), reads=[b_bank[7]], writes=[b_qkT[sl]])

    def stageHI(i):
        sl = i % 2
        vb, bvb = vb_of(i)
        qkT = qkT_t[sl]
        Sb_v = Sb_t[sl].rearrange("p (h e) -> p h e", h=4)
        for h in range(4):
            P.op("pe", lambda g, h=h: g.matmul(out=banks[2][:, h * 128:(h + 1) * 128], lhsT=qkT[0:64, (4 + h) * 128:(5 + h) * 128],
                                              rhs=qkT[0:64, h * 128:(h + 1) * 128], start=True, stop=True),
                 reads=[b_qkT[sl]], writes=[b_bank[2]])
        P.op("dve", lambda g: g.tensor_tensor(out=Pm, in0=banks[2][:, 0:512], in1=maskb[:], op=ALU.mult),
             reads=[b_bank[2], b_const], writes=[b_Pm])
        for h in range(4):
            P.op("pe", lambda g, h=h: g.matmul(out=banks[7][:, h * 128:(h + 1) * 128], lhsT=vb[:, h * 128:(h + 1) * 128],
                                              rhs=Pm[:, h * 128:(h + 1) * 128], start=True, stop=False),
                 reads=[bvb, b_Pm], writes=[b_bank[7]])
            P.op("pe", lambda g, h=h: g.matmul(out=banks[7][:, h * 128:(h + 1) * 128], lhsT=Sb_v[0:64, h, :],
                                              rhs=qkT[0:64, h * 128:(h + 1) * 128], start=False, stop=True),
                 reads=[b_Sb[sl], b_qkT[sl]], writes=[b_bank[7]])
        P.op("act", lambda g: g.activation(out=sq_t[sl], in_=banks[7][:, 0:512], func=AF.Square), reads=[b_bank[7]], writes=[b_sq[sl]])

    def stageJ(i):
        oi = i - NT_PRE
        sl = i % 2
        P.op("pe", lambda g: g.matmul(out=banks[2][:, 0:512], lhsT=onesb[:], rhs=sq_t[sl], start=True, stop=True),
             reads=[b_sq[sl], b_const], writes=[b_bank[2]])
        P.op("act", lambda g: g.activation(out=rstd, in_=banks[2][:, 0:512], func=AF.Ln, bias=EPS), reads=[b_bank[2]], writes=[b_rstd])
        P.op("act", lambda g: g.activation(out=rstd, in_=rstd, func=AF.Exp, scale=-0.5), reads=[b_rstd], writes=[b_rstd])
        P.op("dve", lambda g: g.tensor_tensor(out=y1_all_v[:, :, oi * 128:(oi + 1) * 128],
                                              in0=banks[7][:, 0:512].rearrange("p (h t) -> p h t", h=4),
                                              in1=rstd.rearrange("p (h t) -> p h t", h=4), op=ALU.mult),
             reads=[b_bank[7], b_rstd], writes=[b_y1[oi]])

    T0 = int(os.environ.get('K_T0', 0))
    T1 = int(os.environ.get('K_T1', NT_ALL))
    pre_stages = [stageA, stageB, stageC, stageD, stageE, lambda i: (stageF(i), stageS(i))]
    npre = min(T1, NT_PRE)
    for step in range(T0, npre + len(pre_stages) - 1):
        for k, st in enumerate(pre_stages):
            t = step - k
            if T0 <= t < npre:
                st(t)
    b_carry = Buf("carry")
    carry_v = carry_t[:].rearrange("p (c j) -> p c j", c=4)
    hT47, b_h47 = hT_of(NT_PRE - 1)
    for cc in range(4):
        for k in range(8):
            P.op("pe", lambda g, cc=cc, k=k: g.matmul(out=banks[2][:, 0:2], lhsT=Wgbcu[:, k, 1024 + cc * 128:1024 + (cc + 1) * 128],
                                                     rhs=hT47[:, k, 126:128], start=(k == 0), stop=(k == 7)),
                 reads=[b_h47, b_wg], writes=[b_bank[2]])
        for k in range(8):
            P.op("pe", lambda g, cc=cc, k=k: g.matmul(out=banks[3][:, 0:2], lhsT=Wgbcu[:, k, 1536 + cc * 128:1536 + (cc + 1) * 128],
                                                     rhs=hT47[:, k, 126:128], start=(k == 0), stop=(k == 7)),
                 reads=[b_h47, b_wg], writes=[b_bank[3]])
        P.op("act", lambda g: g.copy(out=small[:, 4:6], in_=banks[2][:, 0:2]), reads=[b_bank[2]], writes=[b_ss[0]])
        P.op("dve", lambda g, cc=cc: g.tensor_tensor(out=carry_v[:, cc, :], in0=banks[3][:, 0:2], in1=small[:, 4:6], op=ALU.mult),
             reads=[b_bank[3], b_ss[0]], writes=[b_carry])
    for dst, src in ((b_vb4[2], b_hTs[0]), (b_vb4[3], b_hTs[0]), (b_qkT[1], b_hTs[1]),
                     (b_Sb[1], b_hTs[2]), (b_sq[1], b_hTs[2])):
        dst.w = src.w
        dst.r = list(src.r)
    own_stages = [(stageJ, 7), (stageA, 0), (stageB, 1), (stageC, 2), (stageD, 3), (stageE, 4), (stageFo, 5), (stageHI, 6)]
    o0 = max(T0, NT_PRE)
    for step in range(o0, T1 + 7):
        for st, k in own_stages:
            t = step - k
            if o0 <= t < T1:
                st(t)

    if stop_after in ("p1", "p1s"):
        P.barrier(); P.emit(); return nc, es
    P.barrier()
    b_xr = [Buf("xr%d" % i) for i in range(NT_OWN)]
    for t in range(NT_OWN):
        ld("sp", xres[:, t * D:(t + 1) * D], xs_d[(NT_PRE + t) * 128:(NT_PRE + t + 1) * 128, :], b_xr[t], P.dsem())
    b_gB = Buf("gB")
    ds_gB = P.dsem()
    ld("sp", gB[:], gmoe_d.broadcast_to([128, D]), b_gB, ds_gB)

    c2 = Carver(scr2, 7200)
    gs = c2.f32(512)
    Ct = c2.f32(512)
    cu = c2.f32(520)
    acc = c2.f32(512)
    yg = c2.bf16(2048).rearrange("p (c t) -> p c t", c=4)
    ycv = c2.bf16(2048).rearrange("p (c t) -> p c t", c=4)
    h2_t = [c2.f32(1024) for _ in range(2)]
    h2Tf = c2.f32(1024).rearrange("p (k t) -> p k t", k=8)
    b_gs, b_Ct, b_cu, b_acc = Buf("gs"), Buf("Ct"), Buf("cu"), Buf("acc")
    b_yg, b_ycv, b_h2Tf, b_Lg = Buf("yg"), Buf("ycv"), Buf("h2Tf"), Buf("Lg")
    b_h2 = [Buf("h2a"), Buf("h2b")]

    Lg_v = Lg[:].rearrange("p (t n) -> p t n", t=NT_OWN)
    L2 = int(os.environ.get('K_L2', 99))
    b_s2 = [[Buf("s2_%d_%d" % (a, b)) for b in range(2)] for a in range(2)]
    for blk in range(4 if L2 >= 1 else 0):
        tcols = slice(blk * 512, (blk + 1) * 512)
        bts = [b_hT[blk * 4 + t] for t in range(4)]
        for cc in range(4):
            bb = 4 * (cc % 2)
            for k in range(8):
                P.op("pe", lambda g, cc=cc, k=k, bb=bb, tcols=tcols: g.matmul(out=banks[bb][:, 0:512], lhsT=Wgbcu[:, k, cc * 128:(cc + 1) * 128],
                                                                rhs=hT_all_v[:, k, tcols], start=(k == 0), stop=(k == 7)),
                     reads=bts + [b_wg], writes=[b_bank[bb]])
            P.op("act", lambda g, bb=bb: g.activation(out=gs, in_=banks[bb][:, 0:512], func=AF.Silu), reads=[b_bank[bb]], writes=[b_gs])
            P.op("dve", lambda g, cc=cc, tcols=tcols: g.scalar_tensor_tensor(out=yg[:, cc, :], in0=y1_all_v[:, cc, tcols], scalar=ggo[:, 0:1],
                                                                in1=gs, op0=ALU.mult, op1=ALU.mult),
                 reads=[b_gs, b_const] + [b_y1[blk * 4 + t] for t in range(4)], writes=[b_yg])
            for (bk, off) in ((bb + 1, 512), (bb + 2, 1024), (bb + 3, 1536)):
                for k in range(8):
                    P.op("pe", lambda g, cc=cc, k=k, bk=bk, off=off, tcols=tcols: g.matmul(
                        out=banks[bk][:, 0:512], lhsT=Wgbcu[:, k, off + cc * 128:off + (cc + 1) * 128],
                        rhs=hT_all_v[:, k, tcols], start=(k == 0), stop=(k == 7)),
                        reads=bts + [b_wg], writes=[b_bank[bk]])
            P.op("act", lambda g, bb=bb: g.copy(out=Ct, in_=banks[bb + 2][:, 0:512]), reads=[b_bank[bb + 2]], writes=[b_Ct])
            P.op("act", lambda g, cc=cc: g.copy(out=cu[:, 0:2], in_=carry_v[:, cc, :]), reads=[b_carry], writes=[b_cu])
            P.op("dve", lambda g, bb=bb: g.tensor_tensor(out=cu[:, 2:514], in0=banks[bb + 3][:, 0:512], in1=Ct, op=ALU.mult),
                 reads=[b_bank[bb + 3], b_Ct], writes=[b_cu])
            P.op("act", lambda g, cc=cc: g.copy(out=carry_v[:, cc, :], in_=cu[:, 512:514]), reads=[b_cu], writes=[b_carry])
            P.op("dve", lambda g, cc=cc: g.tensor_scalar(out=acc, in0=cu[:, 2:514], scalar1=wconv[:, cc * 3 + 2:cc * 3 + 3], scalar2=None,
                                                         op0=ALU.mult),
                 reads=[b_cu, b_const], writes=[b_acc])
            P.op("dve", lambda g, cc=cc: g.scalar_tensor_tensor(out=acc, in0=cu[:, 1:513], scalar=wconv[:, cc * 3 + 1:cc * 3 + 2], in1=acc,
                                                                op0=ALU.mult, op1=ALU.add),
                 reads=[b_cu, b_const], writes=[b_acc])
            P.op("dve", lambda g, cc=cc: g.scalar_tensor_tensor(out=acc, in0=cu[:, 0:512], scalar=wconv[:, cc * 3:cc * 3 + 1], in1=acc,
                                                                op0=ALU.mult, op1=ALU.add),
                 reads=[b_cu, b_const], writes=[b_acc])
            P.op("dve", lambda g, cc=cc, bb=bb: g.tensor_tensor(out=ycv[:, cc, :], in0=banks[bb + 1][:, 0:512], in1=acc, op=ALU.mult),
                 reads=[b_bank[bb + 1], b_acc], writes=[b_ycv])

        if blk == 3:
            load_expert(0, extra=[b_wg])

        def st2P(t):
            ti = blk * 4 + t
            sl = t % 2
            xr = xres[:, ti * D:(ti + 1) * D]
            for half in range(2):
                for kc in range(8):
                    src = yg if kc < 4 else ycv
                    bsrc = b_yg if kc < 4 else b_ycv
                    P.op("pe", lambda g, half=half, kc=kc, src=src: g.matmul(
                        out=banks[half][:, 0:512], lhsT=src[:, kc % 4, t * 128:(t + 1) * 128],
                        rhs=Wout[:, kc, half * 512:(half + 1) * 512], start=(kc == 0), stop=(kc == 7)),
                        reads=[bsrc, b_wo], writes=[b_bank[half]])
                P.op("dve", lambda g, half=half: g.tensor_tensor(out=xr[:, half * 512:(half + 1) * 512], in0=banks[half][:, 0:512],
                                                                 in1=xr[:, half * 512:(half + 1) * 512], op=ALU.add),
                     reads=[b_bank[half]], writes=[b_xr[ti]])
            c = 2 + 8 * sl
            rms_stats(xr, b_xr[ti], c, c + 1, h2_t[sl].bitcast(BF16)[:, 0:1024], b_h2[sl], b_s2[sl][0], b_s2[sl][1])
            P.op("dve", lambda g: g.scalar_tensor_tensor(out=h2_t[sl], in0=xr, scalar=small[:, c + 1:c + 2], in1=gB[:], op0=ALU.mult, op1=ALU.mult),
                 reads=[b_xr[ti], b_s2[sl][1], b_gB], writes=[b_h2[sl]])

        def st2Q(t):
            ti = blk * 4 + t
            sl = t % 2
            h2 = h2_t[sl]
            for k in range(8):
                bk = 2 + k // 4
                P.op("pe", lambda g, k=k, bk=bk: g.matmul(out=banks[bk][:, (k % 4) * 128:(k % 4 + 1) * 128], lhsT=h2[:, k * 128:(k + 1) * 128],
                                                         rhs=identf[:], start=True, stop=True),
                     reads=[b_h2[sl], b_const], writes=[b_bank[bk]])
            for hh in range(2):
                P.op("dve", lambda g, hh=hh: g.tensor_copy(out=h2Tf[:, hh * 4:(hh + 1) * 4, :],
                                                           in_=banks[2 + hh][:, 0:512].rearrange("p (k t) -> p k t", k=4)),
                     reads=[b_bank[2 + hh]], writes=[b_h2Tf])
            P.op("act", lambda g: g.copy(out=hT_all_v[:, :, ti * 128:(ti + 1) * 128], in_=h2Tf),
                 reads=[b_h2Tf], writes=[b_hT[ti]])

        def st2R(t):
            ti = blk * 4 + t
            for k in range(8):
                P.op("pe", lambda g, k=k: g.matmul(out=banks[4][:, 0:20], lhsT=h2Tf[:, k, :], rhs=wr[:, k * 20:(k + 1) * 20],
                                                  start=(k == 0), stop=(k == 7)),
                     reads=[b_h2Tf, b_const], writes=[b_bank[4]])
            P.op("dve", lambda g: g.tensor_tensor(out=Lg_v[:, ti, :], in0=banks[4][:, 0:20], in1=brb[:], op=ALU.add),
                 reads=[b_bank[4], b_const], writes=[b_Lg])

        for step in range(6):
            if step < 4:
                st2P(step)
            if 2 <= step:
                st2R(step - 2)
            if 1 <= step < 5:
                st2Q(step - 1)

    if stop_after == "p2":
        P.barrier(); P.emit(); return nc, es
    P.barrier()
    b_g5 = Buf("g5")
    ds_g5 = P.dsem()
    ld("sp", gA[:], gple_d.broadcast_to([128, D]), b_g5, ds_g5)
    ld("sp", gB[:], gfin_d.broadcast_to([128, D]), b_g5, ds_g5)
    c3 = Carver(scr2, 7200)
    T16 = NT_OWN
    gmax = c3.f32(16)
    ohg = c3.f32(64)
    ge = c3.f32(64)
    gsum = c3.f32(16)
    pg = c3.f32(16)
    gw = c3.f32(64)
    m1 = c3.f32(64)
    eq1 = c3.f32(256)
    el2 = c3.f32(256)
    m2 = c3.f32(64)
    sel = c3.f32(256)
    dd = c3.f32(256)
    ex = c3.f32(256)
    den = c3.f32(64)
    rden = c3.f32(64)
    wq = c3.f32(256)
    b_r = Buf("route")
    gl = Lg_v[:, :, 0:4]
    el4 = Lg_v[:, :, 4:20].rearrange("p t (g j) -> p t g j", g=4)

    def v3(ap, n=4):
        return ap.rearrange("p (t g) -> p t g", g=n)

    def v4(ap):
        return ap.rearrange("p (t g j) -> p t g j", g=4, j=4)

    def bc3(ap16):
        return ap16.unsqueeze(2).broadcast_to([128, T16, 4])

    def bc4(ap64):
        return ap64.rearrange("p (t g) -> p t g", g=4).unsqueeze(3).broadcast_to([128, T16, 4, 4])

    R = dict(reads=[b_r, b_Lg], writes=[b_r])
    P.op("dve", lambda g: g.tensor_reduce(out=gmax, in_=gl, axis=AX.X, op=ALU.max), **R)
    P.op("dve", lambda g: g.tensor_tensor(out=v3(ohg), in0=gl, in1=bc3(gmax), op=ALU.is_equal), **R)
    P.op("dve", lambda g: g.tensor_tensor(out=v3(ge), in0=gl, in1=bc3(gmax), op=ALU.subtract), **R)
    P.op("act", lambda g: g.activation(out=ge, in_=ge, func=AF.Exp), **R)
    P.op("dve", lambda g: g.tensor_reduce(out=gsum, in_=v3(ge), axis=AX.X, op=ALU.add), **R)
    P.op("dve", lambda g: g.reciprocal(out=pg, in_=gsum), **R)
    P.op("dve", lambda g: g.tensor_tensor(out=v3(gw), in0=v3(ohg), in1=bc3(pg), op=ALU.mult), **R)
    P.op("dve", lambda g: g.tensor_reduce(out=v3(m1), in_=el4, axis=AX.X, op=ALU.max), **R)
    P.op("dve", lambda g: g.tensor_tensor(out=v4(eq1), in0=el4, in1=bc4(m1), op=ALU.is_equal), **R)
    P.op("dve", lambda g: g.scalar_tensor_tensor(out=v4(el2), in0=v4(eq1), scalar=-1e30, in1=el4, op0=ALU.mult, op1=ALU.add), **R)
    P.op("dve", lambda g: g.tensor_reduce(out=v3(m2), in_=v4(el2), axis=AX.X, op=ALU.max), **R)
    P.op("dve", lambda g: g.tensor_tensor(out=v4(sel), in0=el4, in1=bc4(m2), op=ALU.is_ge), **R)
    P.op("dve", lambda g: g.tensor_tensor(out=v4(dd), in0=el4, in1=bc4(m1), op=ALU.subtract), **R)
    P.op("act", lambda g: g.activation(out=ex, in_=dd, func=AF.Exp), **R)
    P.op("dve", lambda g: g.tensor_tensor(out=ex, in0=ex, in1=sel, op=ALU.mult), **R)
    P.op("dve", lambda g: g.tensor_reduce(out=v3(den), in_=v4(ex), axis=AX.X, op=ALU.add), **R)
    P.op("dve", lambda g: g.reciprocal(out=rden, in_=den), **R)
    P.op("dve", lambda g: g.tensor_tensor(out=v4(wq), in0=v4(ex), in1=bc4(rden), op=ALU.mult), **R)
    b_comb = Buf("comb")
    P.op("dve", lambda g: g.tensor_tensor(out=v4(comb[:]), in0=v4(wq), in1=bc4(gw), op=ALU.mult), reads=[b_r], writes=[b_comb])
    comb_v = comb[:].rearrange("p (t e) -> p t e", e=16)

    if stop_after == "p3":
        P.barrier(); P.emit(); return nc, es
    P.barrier()
    c4 = Carver(scr2, 7200)
    hid = [c4.bf16(2048).rearrange("p (c t) -> p c t", c=4) for _ in range(2)]
    sg = [c4.f32(512) for _ in range(2)]
    b_hid = [Buf("hid0"), Buf("hid1")]
    b_sg = [Buf("sg0"), Buf("sg1")]
    cnt4 = {"it": 0, "dn": 0}

    def moe_gu(e, blk):
        s = e % 2
        Wg_e, Wu_e, Wd_e = WE[s]
        tcols = slice(blk * 512, (blk + 1) * 512)
        bts = [b_hT[blk * 4 + t] for t in range(4)]
        hs = (e * 4 + blk) % 2
        for hc in range(4):
            pb = (cnt4["it"] % 2) * 2
            cnt4["it"] += 1
            ss_ = cnt4["it"] % 2
            for k in range(8):
                P.op("pe", lambda g, k=k, hc=hc, pb=pb: g.matmul(
                    out=banks[pb][:, 0:512], lhsT=Wg_e[:, k, hc * 128:(hc + 1) * 128], rhs=hT_all_v[:, k, tcols],
                    start=(k == 0), stop=(k == 7)), reads=bts + [b_we[s]], writes=[b_bank[pb]])
            for k in range(8):
                P.op("pe", lambda g, k=k, hc=hc, pb=pb: g.matmul(
                    out=banks[pb + 1][:, 0:512], lhsT=Wu_e[:, k, hc * 128:(hc + 1) * 128], rhs=hT_all_v[:, k, tcols],
                    start=(k == 0), stop=(k == 7)), reads=bts + [b_we[s]], writes=[b_bank[pb + 1]])
            P.op("act", lambda g, pb=pb, ss_=ss_: g.activation(out=sg[ss_], in_=banks[pb][:, 0:512], func=AF.Silu),
                 reads=[b_bank[pb]], writes=[b_sg[ss_]])
            P.op("dve", lambda g, pb=pb, ss_=ss_, hc=hc: g.tensor_tensor(out=hid[hs][:, hc, :], in0=banks[pb + 1][:, 0:512],
                                                                         in1=sg[ss_], op=ALU.mult),
                 reads=[b_bank[pb + 1], b_sg[ss_]], writes=[b_hid[hs]])

    def moe_dn(e, blk):
        s = e % 2
        Wg_e, Wu_e, Wd_e = WE[s]
        hs = (e * 4 + blk) % 2
        for t in range(4):
            ti = blk * 4 + t
            xr = xres[:, ti * D:(ti + 1) * D]
            db = 4 + (cnt4["dn"] % 2) * 2
            cnt4["dn"] += 1
            for half in range(2):
                for hc in range(4):
                    P.op("pe", lambda g, t=t, half=half, hc=hc, db=db: g.matmul(
                        out=banks[db + half][:, 0:512], lhsT=hid[hs][:, hc, t * 128:(t + 1) * 128],
                        rhs=Wd_e[:, hc, half * 512:(half + 1) * 512], start=(hc == 0), stop=(hc == 3)),
                        reads=[b_hid[hs], b_we[s]], writes=[b_bank[db + half]])
                P.op("dve", lambda g, half=half, db=db, xr=xr, ti=ti: g.scalar_tensor_tensor(
                    out=xr[:, half * 512:(half + 1) * 512], in0=banks[db + half][:, 0:512], scalar=comb_v[:, ti, e:e + 1],
                    in1=xr[:, half * 512:(half + 1) * 512], op0=ALU.mult, op1=ALU.add),
                    reads=[b_bank[db + half], b_comb], writes=[b_xr[ti]])

    prev = None
    for e in range(NE):
        if e > 0:
            load_expert(e)
        for blk in range(4):
            moe_gu(e, blk)
            if prev is not None:
                moe_dn(*prev)
            prev = (e, blk)
            if e == NE - 1 and blk == 0:
                ld("pool", Wpg, wpg_d.rearrange("(k p) c -> p k c", p=128), [b_w5, b_we[0]], ds_w5)
                ld("pool", Wpp, wpp_d.rearrange("(k p) c -> p k c", p=128), [b_w5, b_we[0]], ds_w5)
    moe_dn(*prev)

    if stop_after == "p4":
        P.barrier(); P.emit(); return nc, es
    P.barrier()
    c5 = Carver(scr2, 7200)
    pt = [c5.f32(256) for _ in range(2)]
    ptb = [c5.bf16(256) for _ in range(2)]
    pT = [c5.bf16(256).rearrange("p (k t) -> p k t", k=2) for _ in range(2)]
    h3b = [c5.bf16(1024) for _ in range(2)]
    h3T = [c5.bf16(1024).rearrange("p (k t) -> p k t", k=8) for _ in range(2)]
    sig = c5.f32(1024)
    outst = [c5.f32(1024) for _ in range(2)]
    b_pt = [Buf("pt0"), Buf("pt1")]
    ds_pt = [P.dsem(), P.dsem()]
    b_ptb, b_pT = [Buf("ptb0"), Buf("ptb1")], [Buf("pT0"), Buf("pT1")]
    b_h3b, b_h3T = [Buf("h3b0"), Buf("h3b1")], [Buf("h3T0"), Buf("h3T1")]
    b_sig = Buf("sig")
    b_out = [Buf("o0"), Buf("o1")]
    ds_out = [P.dsem(), P.dsem()]
    b_s5 = [[Buf("s5_%d_%d" % (a, b)) for b in range(4)] for a in range(2)]
    bank1b = banks[1][:, 0:512].bitcast(BF16)
    last_store = []

    def st5X(ti):
        sl = ti % 2
        xr = xres[:, ti * D:(ti + 1) * D]
        ld("sp", pt[sl], p_d[ti * 128:(ti + 1) * 128, :], b_pt[sl], ds_pt[sl])
        c = 4 + 8 * sl
        rms_stats(xr, b_xr[ti], c, c + 1, h3b[sl], b_h3b[sl], b_s5[sl][0], b_s5[sl][1])
        P.op("dve", lambda g: g.scalar_tensor_tensor(out=h3b[sl], in0=xr, scalar=small[:, c + 1:c + 2], in1=gA[:], op0=ALU.mult, op1=ALU.mult),
             reads=[b_xr[ti], b_s5[sl][1], b_g5], writes=[b_h3b[sl]])
        P.op("dve", lambda g: g.tensor_copy(out=ptb[sl], in_=pt[sl]), reads=[b_pt[sl]], writes=[b_ptb[sl]])

    def st5Y(ti):
        sl = ti % 2
        for k in range(8):
            P.op("pe", lambda g, k=k: g.transpose(out=bank0b[:, k * 128:(k + 1) * 128], in_=h3b[sl][:, k * 128:(k + 1) * 128], identity=identb[:]),
                 reads=[b_h3b[sl], b_const], writes=[b_bank[0]])
        P.op("dve", lambda g: g.tensor_copy(out=h3T[sl], in_=bank0b.rearrange("p (k t) -> p k t", k=8)), reads=[b_bank[0]], writes=[b_h3T[sl]])
        for k in range(2):
            P.op("pe", lambda g, k=k: g.transpose(out=bank1b[:, k * 128:(k + 1) * 128], in_=ptb[sl][:, k * 128:(k + 1) * 128], identity=identb[:]),
                 reads=[b_ptb[sl], b_const], writes=[b_bank[1]])
        P.op("dve", lambda g: g.tensor_copy(out=pT[sl], in_=bank1b[:, 0:256].rearrange("p (k t) -> p k t", k=2)), reads=[b_bank[1]], writes=[b_pT[sl]])

    def st5Z(ti):
        sl = ti % 2
        xr = xres[:, ti * D:(ti + 1) * D]
        for half in range(2):
            for k in range(8):
                P.op("pe", lambda g, k=k, half=half: g.matmul(out=banks[2 + half][:, 0:512], lhsT=h3T[sl][:, k, :],
                                                             rhs=Wpg[:, k, half * 512:(half + 1) * 512], start=(k == 0), stop=(k == 7)),
                     reads=[b_h3T[sl], b_w5], writes=[b_bank[2 + half]])
            for k in range(2):
                P.op("pe", lambda g, k=k, half=half: g.matmul(out=banks[4 + half][:, 0:512], lhsT=pT[sl][:, k, :],
                                                             rhs=Wpp[:, k, half * 512:(half + 1) * 512], start=(k == 0), stop=(k == 1)),
                     reads=[b_pT[sl], b_w5], writes=[b_bank[4 + half]])
            P.op("act", lambda g, half=half: g.activation(out=sig[:, half * 512:(half + 1) * 512], in_=banks[2 + half][:, 0:512], func=AF.Sigmoid),
                 reads=[b_bank[2 + half]], writes=[b_sig])
            P.op("dve", lambda g, half=half: g.tensor_tensor(out=sig[:, half * 512:(half + 1) * 512], in0=banks[4 + half][:, 0:512],
                                                             in1=sig[:, half * 512:(half + 1) * 512], op=ALU.mult),
                 reads=[b_bank[4 + half], b_sig], writes=[b_sig])
        P.op("dve", lambda g: g.tensor_tensor(out=xr, in0=sig, in1=xr, op=ALU.add), reads=[b_sig], writes=[b_xr[ti]])

    def st5W(ti):
        sl = ti % 2
        xr = xres[:, ti * D:(ti + 1) * D]
        c = 6 + 8 * sl
        rms_stats(xr, b_xr[ti], c, c + 1, outst[sl].bitcast(BF16)[:, 0:1024], b_out[sl], b_s5[sl][2], b_s5[sl][3])
        P.op("dve", lambda g: g.scalar_tensor_tensor(out=outst[sl], in0=xr, scalar=small[:, c + 1:c + 2], in1=gB[:], op0=ALU.mult, op1=ALU.mult),
             reads=[b_xr[ti], b_s5[sl][3], b_g5], writes=[b_out[sl]])
        tok = P.op("pool", lambda g: g.dma_start(out=y_d[ti * 128:(ti + 1) * 128, :], in_=outst[sl]),
                   reads=[b_out[sl]], dsem=ds_out[sl])
        last_store.append(tok)

    st5 = [st5X, st5Y, st5Z, st5W]
    for step in range(NT_OWN + len(st5) - 1):
        for k, st in enumerate(st5):
            t = step - k
            if 0 <= t < NT_OWN:
                st(t)
    P.wait_only("pool", last_store[-2:])
    P.wait_only("sp", last_store[-2:])
    P.emit()
    return nc, es


_CACHE = {}


def _consts():
    s = np.arange(128)[:, None]
    t = np.arange(128)[None, :]
    tril = np.where(s <= t, -1.0 / 16.0, 0.0).astype(np.float32)
    triu = np.where(s > t, -1.0 / 16.0, 0.0).astype(np.float32)
    mask = np.tile((s <= t).astype(np.float32), (1, 4))
    return {
        "c_ident": np.eye(128, dtype=np.float32),
        "c_tril": tril,
        "c_triu": triu,
        "c_mask": np.ascontiguousarray(mask),
        "c_ones": np.full((128, 128), 1.0 / 128.0, np.float32),
        "c_neg": np.full((128, 1), -1.0 / 16.0, np.float32),
    }


def kernel(x, p, g_mix, w_in, w_gla_gate, b_gla_gate, g_gla_out, w_conv, w_out,
           g_moe, w_group, b_group, w_router, b_router, w_exp_gate, w_exp_up, w_exp_down,
           g_ple, w_ple_gate, w_ple_proj, g_final):
    f = lambda a: np.ascontiguousarray(np.asarray(a, dtype=np.float32))
    x = f(x); p = f(p)
    if "nc" not in _CACHE:
        _CACHE["nc"] = build_program()
    nc, _es = _CACHE["nc"]
    wg_aug = np.zeros((32, 256), np.float32)
    wg_aug[0:16] = f(w_gla_gate)[0]
    wg_aug[16] = f(b_gla_gate)[0]
    shared = {
        "w_in": f(w_in)[0], "w_out": f(w_out)[0],
        "w_exp_gate": f(w_exp_gate)[0].reshape(NE * D, 512),
        "w_exp_up": f(w_exp_up)[0].reshape(NE * D, 512),
        "w_exp_down": f(w_exp_down)[0].reshape(NE * 512, D),
        "w_ple_gate": f(w_ple_gate)[0], "w_ple_proj": f(w_ple_proj)[0],
        "g_mix": f(g_mix).reshape(1, D), "g_moe": f(g_moe).reshape(1, D), "g_ple": f(g_ple).reshape(1, D),
        "g_final": f(g_final).reshape(1, D),
        "w_rt": np.ascontiguousarray(np.concatenate([f(w_group)[0], f(w_router)[0]], axis=1)),
        "b_rt": np.ascontiguousarray(np.concatenate([f(b_group)[0], f(b_router)[0]], axis=0).reshape(1, 20)),
        "wg_aug": wg_aug,
        "wconv_t": np.ascontiguousarray(f(w_conv)[0].reshape(3, 4, 128).transpose(2, 1, 0).reshape(128, 12)),
        "ggo": np.ascontiguousarray(f(g_gla_out)[0].reshape(128, 1)),
    }
    shared.update(_consts())
    in_maps = []
    for c in range(8):
        b, j = c // 4, c % 4
        xs = np.zeros((NT_ALL * 128, D), np.float32)
        n = 2048 * (j + 1)
        xs[NT_ALL * 128 - n:] = x[b, 0:n]
        m = dict(shared)
        m["xs"] = xs
        m["p_own"] = np.ascontiguousarray(p[0, b, 2048 * j:2048 * (j + 1)])
        in_maps.append(m)
    res = run_bass_kernel_spmd(nc, in_maps, core_ids=list(range(8)))
    out = np.empty((2, 8192, D), np.float32)
    for c in range(8):
        b, j = c // 4, c % 4
        out[b, 2048 * j:2048 * (j + 1)] = res.results[c]["y"]
    return out
```

```python
import numpy as np
from contextlib import ExitStack
import concourse.bass as bass
import concourse.mybir as mybir
from concourse.bass_utils import run_bass_kernel_spmd

F32 = mybir.dt.float32
BF16 = mybir.dt.bfloat16
AF = mybir.ActivationFunctionType
ALU = mybir.AluOpType
AX = mybir.AxisListType

EPS = 1e-6
NT_ALL = 64
NT_OWN = 16
NT_PRE = NT_ALL - NT_OWN
D = 1024
DIN = 3088
NE = 16


class Buf:
    __slots__ = ("name", "w", "r")

    def __init__(self, name):
        self.name = name
        self.w = None
        self.r = []


class DSem:
    def __init__(self, h):
        self.h = h
        self.count = 0
        self.nobar = False


class Prog:
    def __init__(self, nc, es):
        self.nc = nc
        self.es = es
        self.names = ["pe", "act", "dve", "pool", "sp"]
        self.streams = {k: [] for k in self.names}
        self.sem = {k: es.enter_context(nc.semaphore("c_" + k)) for k in ["pe", "act", "dve"]}
        self.cnt = {k: 0 for k in self.sem}
        self.known = {k: {} for k in self.names}
        self.handles = {}
        self.dsems = []
        self.nds = 0

    def dsem(self):
        self.nds += 1
        s = DSem(self.es.enter_context(self.nc.semaphore("d%d" % self.nds)))
        self.dsems.append(s)
        return s

    def _filter(self, e, toks):
        need = {}
        for (s, v) in toks:
            if e == "pe" and s is self.sem["pe"]:
                continue
            k = id(s)
            self.handles[k] = s
            if need.get(k, 0) < v:
                need[k] = v
        out = []
        kn = self.known[e]
        for k, v in need.items():
            if kn.get(k, 0) < v:
                kn[k] = v
                out.append((self.handles[k], v))
        return out

    def op(self, e, fn, reads=(), writes=(), dsem=None):
        toks = []
        for b in reads:
            if b.w is not None:
                toks.append(b.w)
        for b in writes:
            toks.extend(b.r)
            if b.w is not None:
                toks.append(b.w)
        waits = self._filter(e, toks)
        if dsem is None:
            self.cnt[e] += 1
            tok = (self.sem[e], self.cnt[e])
            inc = 1
        else:
            dsem.count += 16
            tok = (dsem.h, dsem.count)
            inc = 16
        self.streams[e].append((waits, fn, tok[0], inc))
        for b in writes:
            b.w = tok
            b.r = []
        for b in reads:
            b.r.append(tok)
        return tok

    def wait_only(self, e, toks):
        waits = self._filter(e, toks)
        if waits:
            self.streams[e].append((waits, None, None, 0))

    def barrier(self):
        toks = [(self.sem[k], self.cnt[k]) for k in self.sem if self.cnt[k] > 0]
        toks += [(d.h, d.count) for d in self.dsems if d.count > 0 and not d.nobar]
        for e in self.names:
            self.wait_only(e, toks)

    def emit(self):
        nc = self.nc
        with nc.Block() as block:
            decos = {"pe": block.tensor, "act": block.scalar, "dve": block.vector,
                     "pool": block.gpsimd, "sp": block.sync}
            for k in self.names:
                stream = self.streams[k]

                def body(engine, stream=stream):
                    for waits, fn, sem, inc in stream:
                        for (s, v) in waits:
                            engine.wait_ge(s, v)
                        if fn is not None:
                            ins = fn(engine)
                            ins.then_inc(sem, inc)

                decos[k](body)


def build_program(stop_after=None):
    nc = bass.Bass("TRN2", target_bir_lowering=False)
    es = ExitStack()

    def din(name, shape):
        return nc.dram_tensor(name, list(shape), F32, kind="ExternalInput").ap()

    xs_d = din("xs", [NT_ALL * 128, D])
    p_d = din("p_own", [NT_OWN * 128, 256])
    w_in_d = din("w_in", [D, DIN])
    w_out_d = din("w_out", [D, D])
    wg_d = din("w_exp_gate", [NE * D, 512])
    wu_d = din("w_exp_up", [NE * D, 512])
    wd_d = din("w_exp_down", [NE * 512, D])
    wpg_d = din("w_ple_gate", [D, D])
    wpp_d = din("w_ple_proj", [256, D])
    gmix_d = din("g_mix", [1, D])
    gmoe_d = din("g_moe", [1, D])
    gple_d = din("g_ple", [1, D])
    gfin_d = din("g_final", [1, D])
    wr_d = din("w_rt", [D, 20])
    br_d = din("b_rt", [1, 20])
    wgate_d = din("wg_aug", [32, 256])
    wconv_d = din("wconv_t", [128, 12])
    ggo_d = din("ggo", [128, 1])
    cid_d = din("c_ident", [128, 128])
    ctl_d = din("c_tril", [128, 128])
    ctu_d = din("c_triu", [128, 128])
    cmask_d = din("c_mask", [128, 512])
    cones_d = din("c_ones", [128, 128])
    cneg_d = din("c_neg", [128, 1])
    y_d = nc.dram_tensor("y", [NT_OWN * 128, D], F32, kind="ExternalOutput").ap()

    def sb(name, shape, dt):
        return es.enter_context(nc.sbuf_tensor("s_" + name, list(shape), dt))

    xres = sb("xres", [128, NT_OWN * D], F32)
    hT_all = sb("hT_all", [128, 8 * 2048], BF16)
    y1_all = sb("y1_all", [128, 4 * 2048], BF16)
    wbig = sb("wbig", [128, 24704], BF16)
    scr2 = sb("scr2", [128, 7200], F32)
    carry_t = sb("carry", [128, 8], F32)
    identb = sb("identb", [128, 128], BF16)
    identf = sb("identf", [128, 128], F32)
    tril = sb("tril", [128, 128], F32)
    triu = sb("triu", [128, 128], F32)
    maskb = sb("maskb", [128, 512], BF16)
    onesb = sb("onesb", [128, 128], BF16)
    negcol = sb("negcol", [128, 1], F32)
    gA = sb("gA", [128, D], F32)
    gB = sb("gB", [128, D], F32)
    wr = sb("wr", [128, 8 * 20], F32)
    brb = sb("brb", [128, 20], F32)
    wgate = sb("wgate", [32, 256], F32)
    wconv = sb("wconv", [128, 12], F32)
    ggo = sb("ggo", [128, 1], F32)
    Lg = sb("Lg", [128, NT_OWN * 20], F32)
    comb = sb("comb", [128, NT_OWN * 16], F32)
    small = sb("small", [128, 16], F32)
    LTs = sb("LTs", [32, 128], F32)

    banks = [es.enter_context(nc.psum_tensor("bank%d" % i, [128, 512], F32)) for i in range(8)]

    P = Prog(nc, es)

    class Carver:
        def __init__(self, base, nwords):
            self.base = base
            self.off = 0
            self.n = nwords

        def f32(self, n):
            a = self.base[:, self.off:self.off + n]
            self.off += n
            assert self.off <= self.n, (self.off, self.n)
            return a

        def bf16(self, n):
            assert n % 2 == 0
            return self.f32(n // 2).bitcast(BF16)

    b_const = Buf("const")
    ds_const = P.dsem()

    def ld(e, out_ap, in_ap, buf, dsem):
        bufs = buf if isinstance(buf, (list, tuple)) else [buf]
        return P.op(e, lambda g: g.dma_start(out=out_ap, in_=in_ap), writes=list(bufs), dsem=dsem)

    ld("sp", identf[:], cid_d, b_const, ds_const)
    ld("sp", tril[:], ctl_d, b_const, ds_const)
    ld("sp", triu[:], ctu_d, b_const, ds_const)
    ld("sp", negcol[:], cneg_d, b_const, ds_const)
    ld("sp", gA[:], gmix_d.broadcast_to([128, D]), b_const, ds_const)
    ld("sp", wr[:].rearrange("p (k n) -> p k n", k=8), wr_d.rearrange("(k p) n -> p k n", p=128), b_const, ds_const)
    ld("sp", brb[:], br_d.broadcast_to([128, 20]), b_const, ds_const)
    ld("sp", wgate[:], wgate_d, b_const, ds_const)
    ld("sp", wconv[:], wconv_d, b_const, ds_const)
    ld("sp", ggo[:], ggo_d, b_const, ds_const)
    ds_const2 = P.dsem()
    ld("pool", identb[:], cid_d, b_const, ds_const2)
    ld("pool", maskb[:], cmask_d, b_const, ds_const2)
    ld("pool", onesb[:], cones_d, b_const, ds_const2)

    c1 = Carver(xres, NT_OWN * D)
    Wqkva = c1.bf16(8 * 1296).rearrange("p (k c) -> p k c", k=8)
    xs_t = [c1.f32(1024) for _ in range(2)]
    hb_t = [c1.bf16(1024) for _ in range(2)]
    hts_off = c1.off
    hTs = [c1.bf16(1024).rearrange("p (k t) -> p k t", k=8) for _ in range(4)]
    aT_t = [c1.f32(128) for _ in range(2)]
    e1 = c1.f32(256)
    sp_t = [c1.f32(256) for _ in range(2)]
    Er = c1.f32(256)
    El_t = [c1.f32(8) for _ in range(2)]
    kd_t = [c1.bf16(256) for _ in range(2)]
    vb_t = [c1.bf16(512) for _ in range(2)]
    Eq = c1.f32(256)
    Ek = c1.f32(256)
    qt_t = [c1.bf16(256) for _ in range(2)]
    kt_t = [c1.bf16(256) for _ in range(2)]
    qkT0 = c1.bf16(1024)
    Pm = c1.bf16(512)
    S = c1.f32(512)
    Sb0 = c1.bf16(512)
    sq0 = c1.bf16(512)
    rstd = c1.f32(512)
    c1o = Carver(xres, hts_off + 1536)
    c1o.off = hts_off
    vb4 = [vb_t[0], vb_t[1], c1o.bf16(512), c1o.bf16(512)]
    qkT_t = [qkT0, c1o.bf16(1024)]
    Sb_t = [Sb0, c1o.bf16(512)]
    sq_t = [sq0, c1o.bf16(512)]

    w_in_v = w_in_d.rearrange("(k p) c -> p k c", p=128)
    b_wqkva = Buf("wqkva")
    ds_w1 = P.dsem()
    b_wa = Buf("wa")
    ds_wa = P.dsem()
    ld("pool", Wqkva[:, :, 1024:1040], w_in_v[:, :, 1536:1552], b_wa, ds_wa)
    ld("pool", Wqkva[:, :, 256:1024], w_in_v[:, :, 256:1024], b_wqkva, ds_w1)
    b_wq = Buf("wq")
    ds_wq = P.dsem()
    ld("pool", Wqkva[:, :, 0:256], w_in_v[:, :, 0:256], b_wq, ds_wq)

    Wgbcu = wbig[:, 0:8 * 2048].rearrange("p (k c) -> p k c", k=8)
    Wout = wbig[:, 8 * 2048:8 * 3072].rearrange("p (k c) -> p k c", k=8)
    b_wg = Buf("wgbcu")
    b_wo = Buf("wout")
    ds_w2 = P.dsem()
    ds_wo = P.dsem()
    ld("pool", Wgbcu[:, :, 0:512], w_in_v[:, :, 1024:1536], b_wg, ds_w2)
    ld("pool", Wgbcu[:, :, 512:2048], w_in_v[:, :, 1552:3088], b_wg, ds_w2)
    ld("pool", Wout, w_out_d.rearrange("(k p) c -> p k c", p=128), b_wo, ds_wo)
    WE = []
    for s_ in range(2):
        base = s_ * 12288
        WE.append((wbig[:, base:base + 4096].rearrange("p (k c) -> p k c", k=8),
                   wbig[:, base + 4096:base + 8192].rearrange("p (k c) -> p k c", k=8),
                   wbig[:, base + 8192:base + 12288].rearrange("p (k c) -> p k c", k=4)))
    b_we = [Buf("we0"), Buf("we1")]
    ds_we = [P.dsem(), P.dsem()]
    ds_we[0].nobar = True
    ds_we[1].nobar = True
    Wpg = wbig[:, 0:8192].rearrange("p (k c) -> p k c", k=8)
    Wpp = wbig[:, 8192:8192 + 2048].rearrange("p (k c) -> p k c", k=2)
    b_w5 = Buf("w5")
    ds_w5 = P.dsem()
    ds_w5.nobar = True

    def load_expert(e, extra=()):
        s_ = e % 2
        Wg_e, Wu_e, Wd_e = WE[s_]
        bufs = [b_we[s_]] + list(extra)
        ld("pool", Wg_e, wg_d[e * D:(e + 1) * D, :].rearrange("(k p) c -> p k c", p=128), bufs, ds_we[s_])
        ld("pool", Wu_e, wu_d[e * D:(e + 1) * D, :].rearrange("(k p) c -> p k c", p=128), bufs, ds_we[s_])
        ld("pool", Wd_e, wd_d[e * 512:(e + 1) * 512, :].rearrange("(k p) c -> p k c", p=128), bufs, ds_we[s_])

    if stop_after == "consts":
        P.barrier(); P.emit(); return nc, es
    import os
    b_xs = [Buf("xs0"), Buf("xs1")]
    ds_xs = [P.dsem(), P.dsem()]
    b_hb = [Buf("hb0"), Buf("hb1")]
    b_hTs = [Buf("hTs%d" % i) for i in range(4)]
    b_hT = [Buf("hT%d" % i) for i in range(NT_OWN)]
    b_y1 = [Buf("y1_%d" % i) for i in range(NT_OWN)]
    b_bank = [Buf("bank%d" % i) for i in range(8)]
    b_aTp = Buf("aTp")
    b_lastp = Buf("lastp")
    b_zp = Buf("zp")
    b_aT = [Buf("aT0"), Buf("aT1")]
    b_e1 = Buf("e1")
    b_sp = [Buf("sp0"), Buf("sp1")]
    b_Eq, b_Ek, b_Er = Buf("Eq"), Buf("Ek"), Buf("Er")
    b_El = [Buf("El0"), Buf("El1")]
    b_kd = [Buf("kd0"), Buf("kd1")]
    b_vb = [Buf("vb0"), Buf("vb1")]
    b_qt, b_kt = [Buf("qt0"), Buf("qt1")], [Buf("kt0"), Buf("kt1")]
    b_Pm, b_S, b_rstd = Buf("Pm"), Buf("S"), Buf("rstd")
    b_qkT, b_Sb, b_sq = [Buf("qkT0"), Buf("qkT1")], [Buf("Sb0"), Buf("Sb1")], [Buf("sq0"), Buf("sq1")]
    b_vb4 = [b_vb[0], b_vb[1], Buf("vb2"), Buf("vb3")]

    def vb_of(i):
        if i >= NT_PRE:
            return vb4[i % 4], b_vb4[i % 4]
        return vb_t[i % 2], b_vb[i % 2]
    b_ss = [Buf("ss0"), Buf("ss1")]
    b_rs = [Buf("rs0"), Buf("rs1")]

    bank0b = banks[0][:, 0:512].bitcast(BF16)
    bank7b = banks[7][:, 0:512].bitcast(BF16)

    hT_all_v = hT_all[:].rearrange("p (k t) -> p k t", k=8)
    y1_all_v = y1_all[:].rearrange("p (h t) -> p h t", h=4)
    S_v = S.rearrange("p (h e) -> p h e", h=4)

    P.op("dve", lambda g: g.memset(S[0:64, :], 0.0), writes=[b_S])
    for a_ in range(2):
        P.op("dve", lambda g, a_=a_: g.memset(aT_t[a_][0:32, :], 1.0), writes=[b_aT[a_]])

    def rms_stats(src_ap, b_src, ss_col, rs_col, junk, b_junk, bss=None, brs=None):
        bss = bss or b_ss[0]
        brs = brs or b_rs[0]
        P.op("act", lambda g: g.activation(out=junk, in_=src_ap, func=AF.Square, accum_out=small[:, ss_col:ss_col + 1]),
             reads=[b_src], writes=[b_junk, bss])
        P.op("act", lambda g: g.activation(out=small[:, rs_col:rs_col + 1], in_=small[:, ss_col:ss_col + 1], func=AF.Ln,
                                           scale=1.0 / D, bias=EPS),
             reads=[bss], writes=[brs])
        P.op("act", lambda g: g.activation(out=small[:, rs_col:rs_col + 1], in_=small[:, rs_col:rs_col + 1], func=AF.Exp, scale=-0.5),
             reads=[brs], writes=[brs])

    def hT_of(i):
        if i >= NT_PRE:
            oi = i - NT_PRE
            return hT_all_v[:, :, oi * 128:(oi + 1) * 128], b_hT[oi]
        return hTs[i % 4], b_hTs[i % 4]

    def stageA(i):
        sl = i % 2
        xt = xs_t[sl]
        ld("sp", xt, xs_d[i * 128:(i + 1) * 128, :], b_xs[sl], ds_xs[sl])
        hb = hb_t[sl]
        rms_stats(xt, b_xs[sl], 8 * sl, 8 * sl + 1, hb, b_hb[sl], b_ss[sl], b_rs[sl])
        P.op("dve", lambda g: g.scalar_tensor_tensor(out=hb, in0=xt, scalar=small[:, 8 * sl + 1:8 * sl + 2], in1=gA[:],
                                                     op0=ALU.mult, op1=ALU.mult),
             reads=[b_xs[sl], b_rs[sl], b_const], writes=[b_hb[sl]])

    def stageB(i):
        sl = i % 2
        hb = hb_t[sl]
        hT, bh = hT_of(i)
        for k in range(8):
            P.op("pe", lambda g, k=k: g.transpose(out=bank0b[:, k * 128:(k + 1) * 128], in_=hb[:, k * 128:(k + 1) * 128],
                                                 identity=identb[:]),
                 reads=[b_hb[sl], b_const], writes=[b_bank[0]])
        P.op("act", lambda g: g.copy(out=hT, in_=bank0b.rearrange("p (k t) -> p k t", k=8)),
             reads=[b_bank[0]], writes=[bh])

    def stageC(i):
        sl = i % 2
        hT, bh = hT_of(i)
        for k in range(8):
            P.op("pe", lambda g, k=k: g.matmul(out=banks[1][0:16, 0:128], lhsT=Wqkva[:, k, 1024:1040], rhs=hT[:, k, :],
                                              start=(k == 0), stop=(k == 7)),
                 reads=[bh, b_wa], writes=[b_bank[1]])
        P.op("act", lambda g: g.copy(out=aT_t[sl][0:16, :], in_=banks[1][0:16, 0:128]), reads=[b_bank[1]], writes=[b_aT[sl]])

    def stageD(i):
        sl = i % 2
        P.op("pe", lambda g: g.matmul(out=banks[2][:, 256:512], lhsT=aT_t[sl][0:17, :], rhs=wgate[0:17, :], start=True, stop=True),
             reads=[b_aT[sl], b_const], writes=[b_bank[2]])
        P.op("act", lambda g: g.activation(out=e1, in_=banks[2][:, 256:512], func=AF.Exp, scale=-1.0), reads=[b_bank[2]], writes=[b_e1])
        P.op("act", lambda g: g.activation(out=sp_t[sl], in_=e1, func=AF.Ln, bias=1.0), reads=[b_e1], writes=[b_sp[sl]])

    def stageE(i):
        own = i >= NT_PRE
        sl = i % 2
        hT, bh = hT_of(i)
        spb = sp_t[sl]
        if own:
            P.op("pe", lambda g: g.matmul(out=banks[3][:, 0:256], lhsT=tril[:], rhs=spb, start=True, stop=True),
                 reads=[b_sp[sl], b_const], writes=[b_bank[3]])
        P.op("pe", lambda g: g.matmul(out=banks[3][:, 256:512], lhsT=triu[:], rhs=spb, start=True, stop=True),
             reads=[b_sp[sl], b_const], writes=[b_bank[3]])
        for h in range(4):
            P.op("pe", lambda g, h=h: g.matmul(out=banks[7][0:64, 128 + h:129 + h], lhsT=spb[:, h * 64:(h + 1) * 64], rhs=negcol[:],
                                              start=True, stop=True),
                 reads=[b_sp[sl], b_const], writes=[b_bank[7]])
        if own:
            ncol, c0 = 512, 0
        else:
            ncol, c0 = 256, 256
        for k in range(8):
            P.op("pe", lambda g, k=k: g.matmul(out=banks[4][:, 0:ncol], lhsT=hT[:, k, :], rhs=Wqkva[:, k, c0:c0 + ncol],
                                              start=(k == 0), stop=(k == 7)),
                 reads=[bh, b_wqkva, b_wq], writes=[b_bank[4]])
        for k in range(8):
            P.op("pe", lambda g, k=k: g.matmul(out=banks[5][:, 0:512], lhsT=hT[:, k, :], rhs=Wqkva[:, k, 512:1024],
                                              start=(k == 0), stop=(k == 7)),
                 reads=[bh, b_wqkva], writes=[b_bank[5]])
        P.op("act", lambda g: g.activation(out=Er, in_=banks[3][:, 256:512], func=AF.Exp), reads=[b_bank[3]], writes=[b_Er])
        if own:
            P.op("act", lambda g: g.activation(out=Eq, in_=banks[3][:, 0:256], func=AF.Exp), reads=[b_bank[3]], writes=[b_Eq])
            P.op("act", lambda g: g.activation(out=Ek, in_=banks[3][:, 0:256], func=AF.Exp, scale=-1.0), reads=[b_bank[3]], writes=[b_Ek])
        P.op("act", lambda g: g.activation(out=El_t[sl][0:64, 0:4], in_=banks[7][0:64, 128:132], func=AF.Exp),
             reads=[b_bank[7]], writes=[b_El[sl]])
        kcol = 256 if own else 0
        P.op("dve", lambda g: g.tensor_tensor(out=kd_t[sl], in0=banks[4][:, kcol:kcol + 256], in1=Er, op=ALU.mult),
             reads=[b_bank[4], b_Er], writes=[b_kd[sl]])
        if own:
            P.op("dve", lambda g: g.scalar_tensor_tensor(out=qt_t[sl], in0=banks[4][:, 0:256], scalar=0.125, in1=Eq, op0=ALU.mult, op1=ALU.mult),
                 reads=[b_bank[4], b_Eq], writes=[b_qt[sl]])
            P.op("dve", lambda g: g.tensor_tensor(out=kt_t[sl], in0=banks[4][:, 256:512], in1=Ek, op=ALU.mult),
                 reads=[b_bank[4], b_Ek], writes=[b_kt[sl]])
        vb, bvb = vb_of(i)
        P.op("dve", lambda g: g.tensor_copy(out=vb, in_=banks[5][:, 0:512]), reads=[b_bank[5]], writes=[bvb])

    def stageF(i):
        sl = i % 2
        kd = kd_t[sl]
        vb, bvb = vb_of(i)
        for h in range(4):
            P.op("pe", lambda g, h=h: g.matmul(out=banks[6][0:64, h * 128:(h + 1) * 128], lhsT=kd[:, h * 64:(h + 1) * 64],
                                              rhs=vb[:, h * 128:(h + 1) * 128], start=True, stop=True),
                 reads=[b_kd[sl], bvb], writes=[b_bank[6]])

    def stageS(i):
        sl = i % 2
        P.op("dve", lambda g: g.tensor_tensor(out=S_v[0:64, :, :], in0=S_v[0:64, :, :],
                                              in1=El_t[sl][0:64, 0:4].unsqueeze(2).broadcast_to([64, 4, 128]), op=ALU.mult),
             reads=[b_S, b_El[sl]], writes=[b_S])
        P.op("dve", lambda g: g.tensor_tensor(out=S[0:64, :], in0=S[0:64, :], in1=banks[6][0:64, 0:512], op=ALU.add),
             reads=[b_S, b_bank[6]], writes=[b_S])

    def stageFo(i):
        sl = i % 2
        stageF(i)
        P.op("act", lambda g: g.copy(out=Sb_t[sl][0:64, :], in_=S[0:64, :]), reads=[b_S], writes=[b_Sb[sl]])
        stageS(i)
        for j in range(8):
            src = qt_t[sl] if j < 4 else kt_t[sl]
            a = j % 4
            P.op("pe", lambda g, j=j, src=src, a=a: g.transpose(out=bank7b[0:64, j * 128:(j + 1) * 128], in_=src[:, a * 64:(a + 1) * 64],
                                                               identity=identb[:]),
                 reads=[b_qt[sl], b_kt[sl], b_const], writes=[b_bank[7]])
        P.op("act", lambda g: g.copy(out=qkT_t[sl][0:64, :], in_=bank7b[0:64, :]), reads=[b_bank[7]], writes=[b_qkT[sl]])

    def stageHI(i):
        sl = i % 2
        vb, bvb = vb_of(i)
        qkT = qkT_t[sl]
        Sb_v = Sb_t[sl].rearrange("p (h e) -> p h e", h=4)
        for h in range(4):
            P.op("pe", lambda g, h=h: g.matmul(out=banks[2][:, h * 128:(h + 1) * 128], lhsT=qkT[0:64, (4 + h) * 128:(5 + h) * 128],
                                              rhs=qkT[0:64, h * 128:(h + 1) * 128], start=True, stop=True),
                 reads=[b_qkT[sl]], writes=[b_bank[2]])
        P.op("dve", lambda g: g.tensor_tensor(out=Pm, in0=banks[2][:, 0:512], in1=maskb[:], op=ALU.mult),
             reads=[b_bank[2], b_const], writes=[b_Pm])
        for h in range(4):
            P.op("pe", lambda g, h=h: g.matmul(out=banks[7][:, h * 128:(h + 1) * 128], lhsT=vb[:, h * 128:(h + 1) * 128],
                                              rhs=Pm[:, h * 128:(h + 1) * 128], start=True, stop=False),
                 reads=[bvb, b_Pm], writes=[b_bank[7]])
            P.op("pe", lambda g, h=h: g.matmul(out=banks[7][:, h * 128:(h + 1) * 128], lhsT=Sb_v[0:64, h, :],
                                              rhs=qkT[0:64, h * 128:(h + 1) * 128], start=False, stop=True),
                 reads=[b_Sb[sl], b_qkT[sl]], writes=[b_bank[7]])
        P.op("act", lambda g: g.activation(out=sq_t[sl], in_=banks[7][:, 0:512], func=AF.Square), reads=[b_bank[7]], writes=[b_sq[sl]])

    def stageJ(i):
        oi = i - NT_PRE
        sl = i % 2
        P.op("pe", lambda g: g.matmul(out=banks[2][:, 0:512], lhsT=onesb[:], rhs=sq_t[sl], start=True, stop=True),
             reads=[b_sq[sl], b_const], writes=[b_bank[2]])
        P.op("act", lambda g: g.activation(out=rstd, in_=banks[2][:, 0:512], func=AF.Ln, bias=EPS), reads=[b_bank[2]], writes=[b_rstd])
        P.op("act", lambda g: g.activation(out=rstd, in_=rstd, func=AF.Exp, scale=-0.5), reads=[b_rstd], writes=[b_rstd])
        P.op("dve", lambda g: g.tensor_tensor(out=y1_all_v[:, :, oi * 128:(oi + 1) * 128],
                                              in0=banks[7][:, 0:512].rearrange("p (h t) -> p h t", h=4),
                                              in1=rstd.rearrange("p (h t) -> p h t", h=4), op=ALU.mult),
             reads=[b_bank[7], b_rstd], writes=[b_y1[oi]])

    T0 = int(os.environ.get('K_T0', 0))
    T1 = int(os.environ.get('K_T1', NT_ALL))
    pre_stages = [stageA, stageB, stageC, stageD, stageE, lambda i: (stageF(i), stageS(i))]
    npre = min(T1, NT_PRE)
    for step in range(T0, npre + len(pre_stages) - 1):
        for k, st in enumerate(pre_stages):
            t = step - k
            if T0 <= t < npre:
                st(t)
    b_carry = Buf("carry")
    carry_v = carry_t[:].rearrange("p (c j) -> p c j", c=4)
    hT47, b_h47 = hT_of(NT_PRE - 1)
    for cc in range(4):
        for k in range(8):
            P.op("pe", lambda g, cc=cc, k=k: g.matmul(out=banks[2][:, 0:2], lhsT=Wgbcu[:, k, 1024 + cc * 128:1024 + (cc + 1) * 128],
                                                     rhs=hT47[:, k, 126:128], start=(k == 0), stop=(k == 7)),
                 reads=[b_h47, b_wg], writes=[b_bank[2]])
        for k in range(8):
            P.op("pe", lambda g, cc=cc, k=k: g.matmul(out=banks[3][:, 0:2], lhsT=Wgbcu[:, k, 1536 + cc * 128:1536 + (cc + 1) * 128],
                                                     rhs=hT47[:, k, 126:128], start=(k == 0), stop=(k == 7)),
                 reads=[b_h47, b_wg], writes=[b_bank[3]])
        P.op("act", lambda g: g.copy(out=small[:, 4:6], in_=banks[2][:, 0:2]), reads=[b_bank[2]], writes=[b_ss[0]])
        P.op("dve", lambda g, cc=cc: g.tensor_tensor(out=carry_v[:, cc, :], in0=banks[3][:, 0:2], in1=small[:, 4:6], op=ALU.mult),
             reads=[b_bank[3], b_ss[0]], writes=[b_carry])
    for dst, src in ((b_vb4[2], b_hTs[0]), (b_vb4[3], b_hTs[0]), (b_qkT[1], b_hTs[1]),
                     (b_Sb[1], b_hTs[2]), (b_sq[1], b_hTs[2])):
        dst.w = src.w
        dst.r = list(src.r)
    own_stages = [(stageJ, 7), (stageA, 0), (stageB, 1), (stageC, 2), (stageD, 3), (stageE, 4), (stageFo, 5), (stageHI, 6)]
    o0 = max(T0, NT_PRE)
    for step in range(o0, T1 + 7):
        for st, k in own_stages:
            t = step - k
            if o0 <= t < T1:
                st(t)

    if stop_after in ("p1", "p1s"):
        P.barrier(); P.emit(); return nc, es
    P.barrier()
    b_xr = [Buf("xr%d" % i) for i in range(NT_OWN)]
    for t in range(NT_OWN):
        ld("sp", xres[:, t * D:(t + 1) * D], xs_d[(NT_PRE + t) * 128:(NT_PRE + t + 1) * 128, :], b_xr[t], P.dsem())
    b_gB = Buf("gB")
    ds_gB = P.dsem()
    ld("sp", gB[:], gmoe_d.broadcast_to([128, D]), b_gB, ds_gB)

    c2 = Carver(scr2, 7200)
    gs = c2.f32(512)
    Ct = c2.f32(512)
    cu = c2.f32(520)
    acc = c2.f32(512)
    yg = c2.bf16(2048).rearrange("p (c t) -> p c t", c=4)
    ycv = c2.bf16(2048).rearrange("p (c t) -> p c t", c=4)
    h2_t = [c2.f32(1024) for _ in range(2)]
    h2Tf = c2.f32(1024).rearrange("p (k t) -> p k t", k=8)
    b_gs, b_Ct, b_cu, b_acc = Buf("gs"), Buf("Ct"), Buf("cu"), Buf("acc")
    b_yg, b_ycv, b_h2Tf, b_Lg = Buf("yg"), Buf("ycv"), Buf("h2Tf"), Buf("Lg")
    b_h2 = [Buf("h2a"), Buf("h2b")]
    b_LTs = Buf("LTs")

    Lg_v = Lg[:].rearrange("p (t n) -> p t n", t=NT_OWN)
    L2 = int(os.environ.get('K_L2', 99))
    b_s2 = [[Buf("s2_%d_%d" % (a, b)) for b in range(2)] for a in range(2)]
    for blk in range(4 if L2 >= 1 else 0):
        tcols = slice(blk * 512, (blk + 1) * 512)
        bts = [b_hT[blk * 4 + t] for t in range(4)]
        for cc in range(4):
            bb = 4 * (cc % 2)
            for k in range(8):
                P.op("pe", lambda g, cc=cc, k=k, bb=bb, tcols=tcols: g.matmul(out=banks[bb][:, 0:512], lhsT=Wgbcu[:, k, cc * 128:(cc + 1) * 128],
                                                                rhs=hT_all_v[:, k, tcols], start=(k == 0), stop=(k == 7)),
                     reads=bts + [b_wg], writes=[b_bank[bb]])
            P.op("act", lambda g, bb=bb: g.activation(out=gs, in_=banks[bb][:, 0:512], func=AF.Silu), reads=[b_bank[bb]], writes=[b_gs])
            P.op("dve", lambda g, cc=cc, tcols=tcols: g.scalar_tensor_tensor(out=yg[:, cc, :], in0=y1_all_v[:, cc, tcols], scalar=ggo[:, 0:1],
                                                                in1=gs, op0=ALU.mult, op1=ALU.mult),
                 reads=[b_gs, b_const] + [b_y1[blk * 4 + t] for t in range(4)], writes=[b_yg])
            for (bk, off) in ((bb + 1, 512), (bb + 2, 1024), (bb + 3, 1536)):
                for k in range(8):
                    P.op("pe", lambda g, cc=cc, k=k, bk=bk, off=off, tcols=tcols: g.matmul(
                        out=banks[bk][:, 0:512], lhsT=Wgbcu[:, k, off + cc * 128:off + (cc + 1) * 128],
                        rhs=hT_all_v[:, k, tcols], start=(k == 0), stop=(k == 7)),
                        reads=bts + [b_wg], writes=[b_bank[bk]])
            P.op("act", lambda g, bb=bb: g.copy(out=Ct, in_=banks[bb + 2][:, 0:512]), reads=[b_bank[bb + 2]], writes=[b_Ct])
            P.op("act", lambda g, cc=cc: g.copy(out=cu[:, 0:2], in_=carry_v[:, cc, :]), reads=[b_carry], writes=[b_cu])
            P.op("dve", lambda g, bb=bb: g.tensor_tensor(out=cu[:, 2:514], in0=banks[bb + 3][:, 0:512], in1=Ct, op=ALU.mult),
                 reads=[b_bank[bb + 3], b_Ct], writes=[b_cu])
            P.op("act", lambda g, cc=cc: g.copy(out=carry_v[:, cc, :], in_=cu[:, 512:514]), reads=[b_cu], writes=[b_carry])
            P.op("dve", lambda g, cc=cc: g.tensor_scalar(out=acc, in0=cu[:, 2:514], scalar1=wconv[:, cc * 3 + 2:cc * 3 + 3], scalar2=None,
                                                         op0=ALU.mult),
                 reads=[b_cu, b_const], writes=[b_acc])
            P.op("dve", lambda g, cc=cc: g.scalar_tensor_tensor(out=acc, in0=cu[:, 1:513], scalar=wconv[:, cc * 3 + 1:cc * 3 + 2], in1=acc,
                                                                op0=ALU.mult, op1=ALU.add),
                 reads=[b_cu, b_const], writes=[b_acc])
            P.op("dve", lambda g, cc=cc: g.scalar_tensor_tensor(out=acc, in0=cu[:, 0:512], scalar=wconv[:, cc * 3:cc * 3 + 1], in1=acc,
                                                                op0=ALU.mult, op1=ALU.add),
                 reads=[b_cu, b_const], writes=[b_acc])
            P.op("dve", lambda g, cc=cc, bb=bb: g.tensor_tensor(out=ycv[:, cc, :], in0=banks[bb + 1][:, 0:512], in1=acc, op=ALU.mult),
                 reads=[b_bank[bb + 1], b_acc], writes=[b_ycv])

        if blk == 3:
            load_expert(0, extra=[b_wg])

        def st2P(t):
            ti = blk * 4 + t
            sl = t % 2
            xr = xres[:, ti * D:(ti + 1) * D]
            for half in range(2):
                for kc in range(8):
                    src = yg if kc < 4 else ycv
                    bsrc = b_yg if kc < 4 else b_ycv
                    P.op("pe", lambda g, half=half, kc=kc, src=src: g.matmul(
                        out=banks[half][:, 0:512], lhsT=src[:, kc % 4, t * 128:(t + 1) * 128],
                        rhs=Wout[:, kc, half * 512:(half + 1) * 512], start=(kc == 0), stop=(kc == 7)),
                        reads=[bsrc, b_wo], writes=[b_bank[half]])
                P.op("dve", lambda g, half=half: g.tensor_tensor(out=xr[:, half * 512:(half + 1) * 512], in0=banks[half][:, 0:512],
                                                                 in1=xr[:, half * 512:(half + 1) * 512], op=ALU.add),
                     reads=[b_bank[half]], writes=[b_xr[ti]])
            c = 2 + 8 * sl
            rms_stats(xr, b_xr[ti], c, c + 1, h2_t[sl].bitcast(BF16)[:, 0:1024], b_h2[sl], b_s2[sl][0], b_s2[sl][1])
            P.op("dve", lambda g: g.scalar_tensor_tensor(out=h2_t[sl], in0=xr, scalar=small[:, c + 1:c + 2], in1=gB[:], op0=ALU.mult, op1=ALU.mult),
                 reads=[b_xr[ti], b_s2[sl][1], b_gB], writes=[b_h2[sl]])

        def st2Q(t):
            ti = blk * 4 + t
            sl = t % 2
            h2 = h2_t[sl]
            for k in range(8):
                bk = 2 + k // 4
                P.op("pe", lambda g, k=k, bk=bk: g.matmul(out=banks[bk][:, (k % 4) * 128:(k % 4 + 1) * 128], lhsT=h2[:, k * 128:(k + 1) * 128],
                                                         rhs=identf[:], start=True, stop=True),
                     reads=[b_h2[sl], b_const], writes=[b_bank[bk]])
            for hh in range(2):
                P.op("dve", lambda g, hh=hh: g.tensor_copy(out=h2Tf[:, hh * 4:(hh + 1) * 4, :],
                                                           in_=banks[2 + hh][:, 0:512].rearrange("p (k t) -> p k t", k=4)),
                     reads=[b_bank[2 + hh]], writes=[b_h2Tf])
            P.op("act", lambda g: g.copy(out=hT_all_v[:, :, ti * 128:(ti + 1) * 128], in_=h2Tf),
                 reads=[b_h2Tf], writes=[b_hT[ti]])

        def st2R(t):
            for k in range(8):
                P.op("pe", lambda g, k=k: g.matmul(out=banks[4][0:20, 0:128], lhsT=wr[:, k * 20:(k + 1) * 20], rhs=h2Tf[:, k, :],
                                                  start=(k == 0), stop=(k == 7)),
                     reads=[b_h2Tf, b_const], writes=[b_bank[4]])
            P.op("act", lambda g: g.copy(out=LTs[0:20, :], in_=banks[4][0:20, 0:128]), reads=[b_bank[4]], writes=[b_LTs])

        def st2S(t):
            ti = blk * 4 + t
            P.op("pe", lambda g: g.matmul(out=banks[5][:, 0:20], lhsT=LTs[0:20, :], rhs=identf[0:20, 0:20], start=True, stop=True),
                 reads=[b_LTs, b_const], writes=[b_bank[5]])
            P.op("dve", lambda g: g.tensor_tensor(out=Lg_v[:, ti, :], in0=banks[5][:, 0:20], in1=brb[:], op=ALU.add),
                 reads=[b_bank[5], b_const], writes=[b_Lg])

        for step in range(7):
            if step < 4:
                st2P(step)
            if 3 <= step:
                st2S(step - 3)
            if 2 <= step < 6:
                st2R(step - 2)
            if 1 <= step < 5:
                st2Q(step - 1)

    if stop_after == "p2":
        P.barrier(); P.emit(); return nc, es
    P.barrier()
    b_g5 = Buf("g5")
    ds_g5 = P.dsem()
    ld("sp", gA[:], gple_d.broadcast_to([128, D]), b_g5, ds_g5)
    ld("sp", gB[:], gfin_d.broadcast_to([128, D]), b_g5, ds_g5)
    c3 = Carver(scr2, 7200)
    T16 = NT_OWN
    gmax = c3.f32(16)
    ohg = c3.f32(64)
    ge = c3.f32(64)
    gsum = c3.f32(16)
    pg = c3.f32(16)
    gw = c3.f32(64)
    m1 = c3.f32(64)
    eq1 = c3.f32(256)
    el2 = c3.f32(256)
    m2 = c3.f32(64)
    sel = c3.f32(256)
    dd = c3.f32(256)
    ex = c3.f32(256)
    den = c3.f32(64)
    rden = c3.f32(64)
    wq = c3.f32(256)
    b_r = Buf("route")
    gl = Lg_v[:, :, 0:4]
    el4 = Lg_v[:, :, 4:20].rearrange("p t (g j) -> p t g j", g=4)

    def v3(ap, n=4):
        return ap.rearrange("p (t g) -> p t g", g=n)

    def v4(ap):
        return ap.rearrange("p (t g j) -> p t g j", g=4, j=4)

    def bc3(ap16):
        return ap16.unsqueeze(2).broadcast_to([128, T16, 4])

    def bc4(ap64):
        return ap64.rearrange("p (t g) -> p t g", g=4).unsqueeze(3).broadcast_to([128, T16, 4, 4])

    R = dict(reads=[b_r, b_Lg], writes=[b_r])
    P.op("dve", lambda g: g.tensor_reduce(out=gmax, in_=gl, axis=AX.X, op=ALU.max), **R)
    P.op("dve", lambda g: g.tensor_tensor(out=v3(ohg), in0=gl, in1=bc3(gmax), op=ALU.is_equal), **R)
    P.op("dve", lambda g: g.tensor_tensor(out=v3(ge), in0=gl, in1=bc3(gmax), op=ALU.subtract), **R)
    P.op("act", lambda g: g.activation(out=ge, in_=ge, func=AF.Exp), **R)
    P.op("dve", lambda g: g.tensor_reduce(out=gsum, in_=v3(ge), axis=AX.X, op=ALU.add), **R)
    P.op("dve", lambda g: g.reciprocal(out=pg, in_=gsum), **R)
    P.op("dve", lambda g: g.tensor_tensor(out=v3(gw), in0=v3(ohg), in1=bc3(pg), op=ALU.mult), **R)
    P.op("dve", lambda g: g.tensor_reduce(out=v3(m1), in_=el4, axis=AX.X, op=ALU.max), **R)
    P.op("dve", lambda g: g.tensor_tensor(out=v4(eq1), in0=el4, in1=bc4(m1), op=ALU.is_equal), **R)
    P.op("dve", lambda g: g.scalar_tensor_tensor(out=v4(el2), in0=v4(eq1), scalar=-1e30, in1=el4, op0=ALU.mult, op1=ALU.add), **R)
    P.op("dve", lambda g: g.tensor_reduce(out=v3(m2), in_=v4(el2), axis=AX.X, op=ALU.max), **R)
    P.op("dve", lambda g: g.tensor_tensor(out=v4(sel), in0=el4, in1=bc4(m2), op=ALU.is_ge), **R)
    P.op("dve", lambda g: g.tensor_tensor(out=v4(dd), in0=el4, in1=bc4(m1), op=ALU.subtract), **R)
    P.op("act", lambda g: g.activation(out=ex, in_=dd, func=AF.Exp), **R)
    P.op("dve", lambda g: g.tensor_tensor(out=ex, in0=ex, in1=sel, op=ALU.mult), **R)
    P.op("dve", lambda g: g.tensor_reduce(out=v3(den), in_=v4(ex), axis=AX.X, op=ALU.add), **R)
    P.op("dve", lambda g: g.reciprocal(out=rden, in_=den), **R)
    P.op("dve", lambda g: g.tensor_tensor(out=v4(wq), in0=v4(ex), in1=bc4(rden), op=ALU.mult), **R)
    b_comb = Buf("comb")
    P.op("dve", lambda g: g.tensor_tensor(out=v4(comb[:]), in0=v4(wq), in1=bc4(gw), op=ALU.mult), reads=[b_r], writes=[b_comb])
    comb_v = comb[:].rearrange("p (t e) -> p t e", e=16)

    if stop_after == "p3":
        P.barrier(); P.emit(); return nc, es
    P.barrier()
    c4 = Carver(scr2, 7200)
    hid = [c4.bf16(2048).rearrange("p (c t) -> p c t", c=4) for _ in range(2)]
    sg = [c4.f32(512) for _ in range(2)]
    b_hid = [Buf("hid0"), Buf("hid1")]
    b_sg = [Buf("sg0"), Buf("sg1")]
    cnt4 = {"it": 0, "dn": 0}

    def moe_gu(e, blk):
        s = e % 2
        Wg_e, Wu_e, Wd_e = WE[s]
        tcols = slice(blk * 512, (blk + 1) * 512)
        bts = [b_hT[blk * 4 + t] for t in range(4)]
        hs = (e * 4 + blk) % 2
        for hc in range(4):
            pb = (cnt4["it"] % 2) * 2
            cnt4["it"] += 1
            ss_ = cnt4["it"] % 2
            for k in range(8):
                P.op("pe", lambda g, k=k, hc=hc, pb=pb: g.matmul(
                    out=banks[pb][:, 0:512], lhsT=Wg_e[:, k, hc * 128:(hc + 1) * 128], rhs=hT_all_v[:, k, tcols],
                    start=(k == 0), stop=(k == 7)), reads=bts + [b_we[s]], writes=[b_bank[pb]])
            for k in range(8):
                P.op("pe", lambda g, k=k, hc=hc, pb=pb: g.matmul(
                    out=banks[pb + 1][:, 0:512], lhsT=Wu_e[:, k, hc * 128:(hc + 1) * 128], rhs=hT_all_v[:, k, tcols],
                    start=(k == 0), stop=(k == 7)), reads=bts + [b_we[s]], writes=[b_bank[pb + 1]])
            P.op("act", lambda g, pb=pb, ss_=ss_: g.activation(out=sg[ss_], in_=banks[pb][:, 0:512], func=AF.Silu),
                 reads=[b_bank[pb]], writes=[b_sg[ss_]])
            P.op("dve", lambda g, pb=pb, ss_=ss_, hc=hc: g.tensor_tensor(out=hid[hs][:, hc, :], in0=banks[pb + 1][:, 0:512],
                                                                         in1=sg[ss_], op=ALU.mult),
                 reads=[b_bank[pb + 1], b_sg[ss_]], writes=[b_hid[hs]])

    def moe_dn(e, blk):
        s = e % 2
        Wg_e, Wu_e, Wd_e = WE[s]
        hs = (e * 4 + blk) % 2
        for t in range(4):
            ti = blk * 4 + t
            xr = xres[:, ti * D:(ti + 1) * D]
            db = 4 + (cnt4["dn"] % 2) * 2
            cnt4["dn"] += 1
            for half in range(2):
                for hc in range(4):
                    P.op("pe", lambda g, t=t, half=half, hc=hc, db=db: g.matmul(
                        out=banks[db + half][:, 0:512], lhsT=hid[hs][:, hc, t * 128:(t + 1) * 128],
                        rhs=Wd_e[:, hc, half * 512:(half + 1) * 512], start=(hc == 0), stop=(hc == 3)),
                        reads=[b_hid[hs], b_we[s]], writes=[b_bank[db + half]])
                P.op("dve", lambda g, half=half, db=db, xr=xr, ti=ti: g.scalar_tensor_tensor(
                    out=xr[:, half * 512:(half + 1) * 512], in0=banks[db + half][:, 0:512], scalar=comb_v[:, ti, e:e + 1],
                    in1=xr[:, half * 512:(half + 1) * 512], op0=ALU.mult, op1=ALU.add),
                    reads=[b_bank[db + half], b_comb], writes=[b_xr[ti]])

    prev = None
    for e in range(NE):
        if e > 0:
            load_expert(e)
        for blk in range(4):
            moe_gu(e, blk)
            if prev is not None:
                moe_dn(*prev)
            prev = (e, blk)
            if e == NE - 1 and blk == 0:
                ld("pool", Wpg, wpg_d.rearrange("(k p) c -> p k c", p=128), [b_w5, b_we[0]], ds_w5)
                ld("pool", Wpp, wpp_d.rearrange("(k p) c -> p k c", p=128), [b_w5, b_we[0]], ds_w5)
    moe_dn(*prev)

    if stop_after == "p4":
        P.barrier(); P.emit(); return nc, es
    P.barrier()
    c5 = Carver(scr2, 7200)
    pt = [c5.f32(256) for _ in range(2)]
    ptb = [c5.bf16(256) for _ in range(2)]
    pT = [c5.bf16(256).rearrange("p (k t) -> p k t", k=2) for _ in range(2)]
    h3b = [c5.bf16(1024) for _ in range(2)]
    h3T = [c5.bf16(1024).rearrange("p (k t) -> p k t", k=8) for _ in range(2)]
    sig = c5.f32(1024)
    outst = [c5.f32(1024) for _ in range(2)]
    b_pt = [Buf("pt0"), Buf("pt1")]
    ds_pt = [P.dsem(), P.dsem()]
    b_ptb, b_pT = [Buf("ptb0"), Buf("ptb1")], [Buf("pT0"), Buf("pT1")]
    b_h3b, b_h3T = [Buf("h3b0"), Buf("h3b1")], [Buf("h3T0"), Buf("h3T1")]
    b_sig = Buf("sig")
    b_out = [Buf("o0"), Buf("o1")]
    ds_out = [P.dsem(), P.dsem()]
    b_s5 = [[Buf("s5_%d_%d" % (a, b)) for b in range(4)] for a in range(2)]
    bank1b = banks[1][:, 0:512].bitcast(BF16)
    last_store = []

    def st5X(ti):
        sl = ti % 2
        xr = xres[:, ti * D:(ti + 1) * D]
        ld("sp", pt[sl], p_d[ti * 128:(ti + 1) * 128, :], b_pt[sl], ds_pt[sl])
        c = 4 + 8 * sl
        rms_stats(xr, b_xr[ti], c, c + 1, h3b[sl], b_h3b[sl], b_s5[sl][0], b_s5[sl][1])
        P.op("dve", lambda g: g.scalar_tensor_tensor(out=h3b[sl], in0=xr, scalar=small[:, c + 1:c + 2], in1=gA[:], op0=ALU.mult, op1=ALU.mult),
             reads=[b_xr[ti], b_s5[sl][1], b_g5], writes=[b_h3b[sl]])
        P.op("dve", lambda g: g.tensor_copy(out=ptb[sl], in_=pt[sl]), reads=[b_pt[sl]], writes=[b_ptb[sl]])

    def st5Y(ti):
        sl = ti % 2
        for k in range(8):
            P.op("pe", lambda g, k=k: g.transpose(out=bank0b[:, k * 128:(k + 1) * 128], in_=h3b[sl][:, k * 128:(k + 1) * 128], identity=identb[:]),
                 reads=[b_h3b[sl], b_const], writes=[b_bank[0]])
        P.op("dve", lambda g: g.tensor_copy(out=h3T[sl], in_=bank0b.rearrange("p (k t) -> p k t", k=8)), reads=[b_bank[0]], writes=[b_h3T[sl]])
        for k in range(2):
            P.op("pe", lambda g, k=k: g.transpose(out=bank1b[:, k * 128:(k + 1) * 128], in_=ptb[sl][:, k * 128:(k + 1) * 128], identity=identb[:]),
                 reads=[b_ptb[sl], b_const], writes=[b_bank[1]])
        P.op("dve", lambda g: g.tensor_copy(out=pT[sl], in_=bank1b[:, 0:256].rearrange("p (k t) -> p k t", k=2)), reads=[b_bank[1]], writes=[b_pT[sl]])

    def st5Z(ti):
        sl = ti % 2
        xr = xres[:, ti * D:(ti + 1) * D]
        for half in range(2):
            for k in range(8):
                P.op("pe", lambda g, k=k, half=half: g.matmul(out=banks[2 + half][:, 0:512], lhsT=h3T[sl][:, k, :],
                                                             rhs=Wpg[:, k, half * 512:(half + 1) * 512], start=(k == 0), stop=(k == 7)),
                     reads=[b_h3T[sl], b_w5], writes=[b_bank[2 + half]])
            for k in range(2):
                P.op("pe", lambda g, k=k, half=half: g.matmul(out=banks[4 + half][:, 0:512], lhsT=pT[sl][:, k, :],
                                                             rhs=Wpp[:, k, half * 512:(half + 1) * 512], start=(k == 0), stop=(k == 1)),
                     reads=[b_pT[sl], b_w5], writes=[b_bank[4 + half]])
            P.op("act", lambda g, half=half: g.activation(out=sig[:, half * 512:(half + 1) * 512], in_=banks[2 + half][:, 0:512], func=AF.Sigmoid),
                 reads=[b_bank[2 + half]], writes=[b_sig])
            P.op("dve", lambda g, half=half: g.tensor_tensor(out=sig[:, half * 512:(half + 1) * 512], in0=banks[4 + half][:, 0:512],
                                                             in1=sig[:, half * 512:(half + 1) * 512], op=ALU.mult),
                 reads=[b_bank[4 + half], b_sig], writes=[b_sig])
        P.op("dve", lambda g: g.tensor_tensor(out=xr, in0=sig, in1=xr, op=ALU.add), reads=[b_sig], writes=[b_xr[ti]])

    def st5W(ti):
        sl = ti % 2
        xr = xres[:, ti * D:(ti + 1) * D]
        c = 6 + 8 * sl
        rms_stats(xr, b_xr[ti], c, c + 1, outst[sl].bitcast(BF16)[:, 0:1024], b_out[sl], b_s5[sl][2], b_s5[sl][3])
        P.op("dve", lambda g: g.scalar_tensor_tensor(out=outst[sl], in0=xr, scalar=small[:, c + 1:c + 2], in1=gB[:], op0=ALU.mult, op1=ALU.mult),
             reads=[b_xr[ti], b_s5[sl][3], b_g5], writes=[b_out[sl]])
        tok = P.op("pool", lambda g: g.dma_start(out=y_d[ti * 128:(ti + 1) * 128, :], in_=outst[sl]),
                   reads=[b_out[sl]], dsem=ds_out[sl])
        last_store.append(tok)

    st5 = [st5X, st5Y, st5Z, st5W]
    for step in range(NT_OWN + len(st5) - 1):
        for k, st in enumerate(st5):
            t = step - k
            if 0 <= t < NT_OWN:
                st(t)
    P.wait_only("pool", last_store[-2:])
    P.wait_only("sp", last_store[-2:])
    P.emit()
    return nc, es


_CACHE = {}


def _consts():
    s = np.arange(128)[:, None]
    t = np.arange(128)[None, :]
    tril = np.where(s <= t, -1.0 / 16.0, 0.0).astype(np.float32)
    triu = np.where(s > t, -1.0 / 16.0, 0.0).astype(np.float32)
    mask = np.tile((s <= t).astype(np.float32), (1, 4))
    return {
        "c_ident": np.eye(128, dtype=np.float32),
        "c_tril": tril,
        "c_triu": triu,
        "c_mask": np.ascontiguousarray(mask),
        "c_ones": np.full((128, 128), 1.0 / 128.0, np.float32),
        "c_neg": np.full((128, 1), -1.0 / 16.0, np.float32),
    }


def kernel(x, p, g_mix, w_in, w_gla_gate, b_gla_gate, g_gla_out, w_conv, w_out,
           g_moe, w_group, b_group, w_router, b_router, w_exp_gate, w_exp_up, w_exp_down,
           g_ple, w_ple_gate, w_ple_proj, g_final):
    f = lambda a: np.ascontiguousarray(np.asarray(a, dtype=np.float32))
    x = f(x); p = f(p)
    if "nc" not in _CACHE:
        _CACHE["nc"] = build_program()
    nc, _es = _CACHE["nc"]
    wg_aug = np.zeros((32, 256), np.float32)
    wg_aug[0:16] = f(w_gla_gate)[0]
    wg_aug[16] = f(b_gla_gate)[0]
    shared = {
        "w_in": f(w_in)[0], "w_out": f(w_out)[0],
        "w_exp_gate": f(w_exp_gate)[0].reshape(NE * D, 512),
        "w_exp_up": f(w_exp_up)[0].reshape(NE * D, 512),
        "w_exp_down": f(w_exp_down)[0].reshape(NE * 512, D),
        "w_ple_gate": f(w_ple_gate)[0], "w_ple_proj": f(w_ple_proj)[0],
        "g_mix": f(g_mix).reshape(1, D), "g_moe": f(g_moe).reshape(1, D), "g_ple": f(g_ple).reshape(1, D),
        "g_final": f(g_final).reshape(1, D),
        "w_rt": np.ascontiguousarray(np.concatenate([f(w_group)[0], f(w_router)[0]], axis=1)),
        "b_rt": np.ascontiguousarray(np.concatenate([f(b_group)[0], f(b_router)[0]], axis=0).reshape(1, 20)),
        "wg_aug": wg_aug,
        "wconv_t": np.ascontiguousarray(f(w_conv)[0].reshape(3, 4, 128).transpose(2, 1, 0).reshape(128, 12)),
        "ggo": np.ascontiguousarray(f(g_gla_out)[0].reshape(128, 1)),
    }
    shared.update(_consts())
    in_maps = []
    for c in range(8):
        b, j = c // 4, c % 4
        xs = np.zeros((NT_ALL * 128, D), np.float32)
        n = 2048 * (j + 1)
        xs[NT_ALL * 128 - n:] = x[b, 0:n]
        m = dict(shared)
        m["xs"] = xs
        m["p_own"] = np.ascontiguousarray(p[0, b, 2048 * j:2048 * (j + 1)])
        in_maps.append(m)
    res = run_bass_kernel_spmd(nc, in_maps, core_ids=list(range(8)))
    out = np.empty((2, 8192, D), np.float32)
    for c in range(8):
        b, j = c // 4, c % 4
        out[b, 2048 * j:2048 * (j + 1)] = res.results[c]["y"]
    return out
```

```python
import numpy as np
from contextlib import ExitStack
import concourse.bass as bass
import concourse.mybir as mybir
from concourse.bass_utils import run_bass_kernel_spmd

F32 = mybir.dt.float32
BF16 = mybir.dt.bfloat16
AF = mybir.ActivationFunctionType
ALU = mybir.AluOpType
AX = mybir.AxisListType

EPS = 1e-6
NT_ALL = 64
NT_OWN = 16
NT_PRE = NT_ALL - NT_OWN
D = 1024
DIN = 3088
NE = 16


class Buf:
    __slots__ = ("name", "w", "r")

    def __init__(self, name):
        self.name = name
        self.w = None
        self.r = []


class DSem:
    def __init__(self, h):
        self.h = h
        self.count = 0
        self.nobar = False


class Prog:
    def __init__(self, nc, es):
        self.nc = nc
        self.es = es
        self.names = ["pe", "act", "dve", "pool", "sp"]
        self.streams = {k: [] for k in self.names}
        self.sem = {k: es.enter_context(nc.semaphore("c_" + k)) for k in ["pe", "act", "dve"]}
        self.cnt = {k: 0 for k in self.sem}
        self.known = {k: {} for k in self.names}
        self.handles = {}
        self.dsems = []
        self.nds = 0

    def dsem(self):
        self.nds += 1
        s = DSem(self.es.enter_context(self.nc.semaphore("d%d" % self.nds)))
        self.dsems.append(s)
        return s

    def _filter(self, e, toks):
        need = {}
        for (s, v) in toks:
            if e == "pe" and s is self.sem["pe"]:
                continue
            k = id(s)
            self.handles[k] = s
            if need.get(k, 0) < v:
                need[k] = v
        out = []
        kn = self.known[e]
        for k, v in need.items():
            if kn.get(k, 0) < v:
                kn[k] = v
                out.append((self.handles[k], v))
        return out

    def op(self, e, fn, reads=(), writes=(), dsem=None):
        toks = []
        for b in reads:
            if b.w is not None:
                toks.append(b.w)
        for b in writes:
            toks.extend(b.r)
            if b.w is not None:
                toks.append(b.w)
        waits = self._filter(e, toks)
        if dsem is None:
            self.cnt[e] += 1
            tok = (self.sem[e], self.cnt[e])
            inc = 1
        else:
            dsem.count += 16
            tok = (dsem.h, dsem.count)
            inc = 16
        self.streams[e].append((waits, fn, tok[0], inc))
        for b in writes:
            b.w = tok
            b.r = []
        for b in reads:
            b.r.append(tok)
        return tok

    def wait_only(self, e, toks):
        waits = self._filter(e, toks)
        if waits:
            self.streams[e].append((waits, None, None, 0))

    def barrier(self):
        toks = [(self.sem[k], self.cnt[k]) for k in self.sem if self.cnt[k] > 0]
        toks += [(d.h, d.count) for d in self.dsems if d.count > 0 and not d.nobar]
        for e in self.names:
            self.wait_only(e, toks)

    def emit(self):
        nc = self.nc
        with nc.Block() as block:
            decos = {"pe": block.tensor, "act": block.scalar, "dve": block.vector,
                     "pool": block.gpsimd, "sp": block.sync}
            for k in self.names:
                stream = self.streams[k]

                def body(engine, stream=stream):
                    for waits, fn, sem, inc in stream:
                        for (s, v) in waits:
                            engine.wait_ge(s, v)
                        if fn is not None:
                            ins = fn(engine)
                            ins.then_inc(sem, inc)

                decos[k](body)


def build_program(stop_after=None):
    nc = bass.Bass("TRN2", target_bir_lowering=False)
    es = ExitStack()

    def din(name, shape):
        return nc.dram_tensor(name, list(shape), F32, kind="ExternalInput").ap()

    xs_d = din("xs", [NT_ALL * 128, D])
    p_d = din("p_own", [NT_OWN * 128, 256])
    w_in_d = din("w_in", [D, DIN])
    w_out_d = din("w_out", [D, D])
    wg_d = din("w_exp_gate", [NE * D, 512])
    wu_d = din("w_exp_up", [NE * D, 512])
    wd_d = din("w_exp_down", [NE * 512, D])
    wpg_d = din("w_ple_gate", [D, D])
    wpp_d = din("w_ple_proj", [256, D])
    gmix_d = din("g_mix", [1, D])
    gmoe_d = din("g_moe", [1, D])
    gple_d = din("g_ple", [1, D])
    gfin_d = din("g_final", [1, D])
    wr_d = din("w_rt", [D, 20])
    br_d = din("b_rt", [1, 20])
    wgate_d = din("wg_aug", [32, 256])
    wconv_d = din("wconv_t", [128, 12])
    ggo_d = din("ggo", [128, 1])
    cid_d = din("c_ident", [128, 128])
    ctl_d = din("c_tril", [128, 128])
    ctu_d = din("c_triu", [128, 128])
    cmask_d = din("c_mask", [128, 512])
    cones_d = din("c_ones", [128, 128])
    cneg_d = din("c_neg", [128, 1])
    y_d = nc.dram_tensor("y", [NT_OWN * 128, D], F32, kind="ExternalOutput").ap()

    def sb(name, shape, dt):
        return es.enter_context(nc.sbuf_tensor("s_" + name, list(shape), dt))

    xres = sb("xres", [128, NT_OWN * D], F32)
    hT_all = sb("hT_all", [128, 8 * 2048], BF16)
    y1_all = sb("y1_all", [128, 4 * 2048], BF16)
    wbig = sb("wbig", [128, 24704], BF16)
    scr2 = sb("scr2", [128, 7200], F32)
    carry_t = sb("carry", [128, 8], F32)
    identb = sb("identb", [128, 128], BF16)
    identf = sb("identf", [128, 128], F32)
    tril = sb("tril", [128, 128], F32)
    triu = sb("triu", [128, 128], F32)
    maskb = sb("maskb", [128, 512], BF16)
    onesb = sb("onesb", [128, 128], BF16)
    negcol = sb("negcol", [128, 1], F32)
    gA = sb("gA", [128, D], F32)
    gB = sb("gB", [128, D], F32)
    wr = sb("wr", [128, 8 * 20], F32)
    brb = sb("brb", [128, 20], F32)
    wgate = sb("wgate", [32, 256], F32)
    wconv = sb("wconv", [128, 12], F32)
    ggo = sb("ggo", [128, 1], F32)
    Lg = sb("Lg", [128, NT_OWN * 20], F32)
    comb = sb("comb", [128, NT_OWN * 16], F32)
    small = sb("small", [128, 16], F32)
    LTs = sb("LTs", [32, 128], F32)

    banks = [es.enter_context(nc.psum_tensor("bank%d" % i, [128, 512], F32)) for i in range(8)]

    P = Prog(nc, es)

    class Carver:
        def __init__(self, base, nwords):
            self.base = base
            self.off = 0
            self.n = nwords

        def f32(self, n):
            a = self.base[:, self.off:self.off + n]
            self.off += n
            assert self.off <= self.n, (self.off, self.n)
            return a

        def bf16(self, n):
            assert n % 2 == 0
            return self.f32(n // 2).bitcast(BF16)

    b_const = Buf("const")
    ds_const = P.dsem()

    def ld(e, out_ap, in_ap, buf, dsem):
        bufs = buf if isinstance(buf, (list, tuple)) else [buf]
        return P.op(e, lambda g: g.dma_start(out=out_ap, in_=in_ap), writes=list(bufs), dsem=dsem)

    ld("sp", identf[:], cid_d, b_const, ds_const)
    ld("sp", tril[:], ctl_d, b_const, ds_const)
    ld("sp", triu[:], ctu_d, b_const, ds_const)
    ld("sp", negcol[:], cneg_d, b_const, ds_const)
    ld("sp", gA[:], gmix_d.broadcast_to([128, D]), b_const, ds_const)
    ld("sp", wr[:].rearrange("p (k n) -> p k n", k=8), wr_d.rearrange("(k p) n -> p k n", p=128), b_const, ds_const)
    ld("sp", brb[:], br_d.broadcast_to([128, 20]), b_const, ds_const)
    ld("sp", wgate[:], wgate_d, b_const, ds_const)
    ld("sp", wconv[:], wconv_d, b_const, ds_const)
    ld("sp", ggo[:], ggo_d, b_const, ds_const)
    ds_const2 = P.dsem()
    ld("pool", identb[:], cid_d, b_const, ds_const2)
    ld("pool", maskb[:], cmask_d, b_const, ds_const2)
    ld("pool", onesb[:], cones_d, b_const, ds_const2)

    c1 = Carver(xres, NT_OWN * D)
    Wqkva = c1.bf16(8 * 1296).rearrange("p (k c) -> p k c", k=8)
    xs_t = [c1.f32(1024) for _ in range(2)]
    hb_t = [c1.bf16(1024) for _ in range(2)]
    hts_off = c1.off
    hTs = [c1.bf16(1024).rearrange("p (k t) -> p k t", k=8) for _ in range(4)]
    aT_t = [c1.f32(128) for _ in range(2)]
    e1 = c1.f32(256)
    sp_t = [c1.f32(256) for _ in range(2)]
    Er = c1.f32(256)
    El_t = [c1.f32(8) for _ in range(2)]
    kd_t = [c1.bf16(256) for _ in range(2)]
    vb_t = [c1.bf16(512) for _ in range(2)]
    Eq = c1.f32(256)
    Ek = c1.f32(256)
    qt_t = [c1.bf16(256) for _ in range(2)]
    kt_t = [c1.bf16(256) for _ in range(2)]
    qkT0 = c1.bf16(1024)
    Pm = c1.bf16(512)
    S = c1.f32(512)
    Sb0 = c1.bf16(512)
    sq0 = c1.bf16(512)
    rstd = c1.f32(512)
    c1o = Carver(xres, hts_off + 1536)
    c1o.off = hts_off
    vb4 = [vb_t[0], vb_t[1], c1o.bf16(512), c1o.bf16(512)]
    qkT_t = [qkT0, c1o.bf16(1024)]
    Sb_t = [Sb0, c1o.bf16(512)]
    sq_t = [sq0, c1o.bf16(512)]

    w_in_v = w_in_d.rearrange("(k p) c -> p k c", p=128)
    b_wqkva = Buf("wqkva")
    ds_w1 = P.dsem()
    b_wa = Buf("wa")
    ds_wa = P.dsem()
    ld("pool", Wqkva[:, :, 1024:1040], w_in_v[:, :, 1536:1552], b_wa, ds_wa)
    ld("pool", Wqkva[:, :, 256:1024], w_in_v[:, :, 256:1024], b_wqkva, ds_w1)
    b_wq = Buf("wq")
    ds_wq = P.dsem()
    ld("pool", Wqkva[:, :, 0:256], w_in_v[:, :, 0:256], b_wq, ds_wq)

    Wgbcu = wbig[:, 0:8 * 2048].rearrange("p (k c) -> p k c", k=8)
    Wout = wbig[:, 8 * 2048:8 * 3072].rearrange("p (k c) -> p k c", k=8)
    b_wg = Buf("wgbcu")
    b_wo = Buf("wout")
    ds_w2 = P.dsem()
    ds_wo = P.dsem()
    ld("pool", Wgbcu[:, :, 0:512], w_in_v[:, :, 1024:1536], b_wg, ds_w2)
    ld("pool", Wgbcu[:, :, 512:2048], w_in_v[:, :, 1552:3088], b_wg, ds_w2)
    ld("pool", Wout, w_out_d.rearrange("(k p) c -> p k c", p=128), b_wo, ds_wo)
    WE = []
    for s_ in range(2):
        base = s_ * 12288
        WE.append((wbig[:, base:base + 4096].rearrange("p (k c) -> p k c", k=8),
                   wbig[:, base + 4096:base + 8192].rearrange("p (k c) -> p k c", k=8),
                   wbig[:, base + 8192:base + 12288].rearrange("p (k c) -> p k c", k=4)))
    b_we = [Buf("we0"), Buf("we1")]
    ds_we = [P.dsem(), P.dsem()]
    ds_we[0].nobar = True
    ds_we[1].nobar = True
    Wpg = wbig[:, 0:8192].rearrange("p (k c) -> p k c", k=8)
    Wpp = wbig[:, 8192:8192 + 2048].rearrange("p (k c) -> p k c", k=2)
    b_w5 = Buf("w5")
    ds_w5 = P.dsem()
    ds_w5.nobar = True

    def load_expert(e, extra=()):
        s_ = e % 2
        Wg_e, Wu_e, Wd_e = WE[s_]
        bufs = [b_we[s_]] + list(extra)
        ld("pool", Wg_e, wg_d[e * D:(e + 1) * D, :].rearrange("(k p) c -> p k c", p=128), bufs, ds_we[s_])
        ld("pool", Wu_e, wu_d[e * D:(e + 1) * D, :].rearrange("(k p) c -> p k c", p=128), bufs, ds_we[s_])
        ld("pool", Wd_e, wd_d[e * 512:(e + 1) * 512, :].rearrange("(k p) c -> p k c", p=128), bufs, ds_we[s_])

    if stop_after == "consts":
        P.barrier(); P.emit(); return nc, es
    import os
    b_xs = [Buf("xs0"), Buf("xs1")]
    ds_xs = [P.dsem(), P.dsem()]
    b_hb = [Buf("hb0"), Buf("hb1")]
    b_hTs = [Buf("hTs%d" % i) for i in range(4)]
    b_hT = [Buf("hT%d" % i) for i in range(NT_OWN)]
    b_y1 = [Buf("y1_%d" % i) for i in range(NT_OWN)]
    b_bank = [Buf("bank%d" % i) for i in range(8)]
    b_aTp = Buf("aTp")
    b_lastp = Buf("lastp")
    b_zp = Buf("zp")
    b_aT = [Buf("aT0"), Buf("aT1")]
    b_e1 = Buf("e1")
    b_sp = [Buf("sp0"), Buf("sp1")]
    b_Eq, b_Ek, b_Er = Buf("Eq"), Buf("Ek"), Buf("Er")
    b_El = [Buf("El0"), Buf("El1")]
    b_kd = [Buf("kd0"), Buf("kd1")]
    b_vb = [Buf("vb0"), Buf("vb1")]
    b_qt, b_kt = [Buf("qt0"), Buf("qt1")], [Buf("kt0"), Buf("kt1")]
    b_Pm, b_S, b_rstd = Buf("Pm"), Buf("S"), Buf("rstd")
    b_qkT, b_Sb, b_sq = [Buf("qkT0"), Buf("qkT1")], [Buf("Sb0"), Buf("Sb1")], [Buf("sq0"), Buf("sq1")]
    b_vb4 = [b_vb[0], b_vb[1], Buf("vb2"), Buf("vb3")]

    def vb_of(i):
        if i >= NT_PRE:
            return vb4[i % 4], b_vb4[i % 4]
        return vb_t[i % 2], b_vb[i % 2]
    b_ss = [Buf("ss0"), Buf("ss1")]
    b_rs = [Buf("rs0"), Buf("rs1")]

    bank0b = banks[0][:, 0:512].bitcast(BF16)
    bank7b = banks[7][:, 0:512].bitcast(BF16)

    hT_all_v = hT_all[:].rearrange("p (k t) -> p k t", k=8)
    y1_all_v = y1_all[:].rearrange("p (h t) -> p h t", h=4)
    S_v = S.rearrange("p (h e) -> p h e", h=4)

    P.op("dve", lambda g: g.memset(S[0:64, :], 0.0), writes=[b_S])
    for a_ in range(2):
        P.op("dve", lambda g, a_=a_: g.memset(aT_t[a_][0:32, :], 1.0), writes=[b_aT[a_]])

    def rms_stats(src_ap, b_src, ss_col, rs_col, junk, b_junk, bss=None, brs=None):
        bss = bss or b_ss[0]
        brs = brs or b_rs[0]
        P.op("act", lambda g: g.activation(out=junk, in_=src_ap, func=AF.Square, accum_out=small[:, ss_col:ss_col + 1]),
             reads=[b_src], writes=[b_junk, bss])
        P.op("act", lambda g: g.activation(out=small[:, rs_col:rs_col + 1], in_=small[:, ss_col:ss_col + 1], func=AF.Ln,
                                           scale=1.0 / D, bias=EPS),
             reads=[bss], writes=[brs])
        P.op("act", lambda g: g.activation(out=small[:, rs_col:rs_col + 1], in_=small[:, rs_col:rs_col + 1], func=AF.Exp, scale=-0.5),
             reads=[brs], writes=[brs])

    def hT_of(i):
        if i >= NT_PRE:
            oi = i - NT_PRE
            return hT_all_v[:, :, oi * 128:(oi + 1) * 128], b_hT[oi]
        return hTs[i % 4], b_hTs[i % 4]

    def stageA(i):
        sl = i % 2
        xt = xs_t[sl]
        ld("sp", xt, xs_d[i * 128:(i + 1) * 128, :], b_xs[sl], ds_xs[sl])
        hb = hb_t[sl]
        rms_stats(xt, b_xs[sl], 8 * sl, 8 * sl + 1, hb, b_hb[sl], b_ss[sl], b_rs[sl])
        P.op("dve", lambda g: g.scalar_tensor_tensor(out=hb, in0=xt, scalar=small[:, 8 * sl + 1:8 * sl + 2], in1=gA[:],
                                                     op0=ALU.mult, op1=ALU.mult),
             reads=[b_xs[sl], b_rs[sl], b_const], writes=[b_hb[sl]])

    def stageB(i):
        sl = i % 2
        hb = hb_t[sl]
        hT, bh = hT_of(i)
        for k in range(8):
            P.op("pe", lambda g, k=k: g.transpose(out=bank0b[:, k * 128:(k + 1) * 128], in_=hb[:, k * 128:(k + 1) * 128],
                                                 identity=identb[:]),
                 reads=[b_hb[sl], b_const], writes=[b_bank[0]])
        P.op("act", lambda g: g.copy(out=hT, in_=bank0b.rearrange("p (k t) -> p k t", k=8)),
             reads=[b_bank[0]], writes=[bh])

    def stageC(i):
        sl = i % 2
        hT, bh = hT_of(i)
        for k in range(8):
            P.op("pe", lambda g, k=k: g.matmul(out=banks[1][0:16, 0:128], lhsT=Wqkva[:, k, 1024:1040], rhs=hT[:, k, :],
                                              start=(k == 0), stop=(k == 7)),
                 reads=[bh, b_wa], writes=[b_bank[1]])
        P.op("act", lambda g: g.copy(out=aT_t[sl][0:16, :], in_=banks[1][0:16, 0:128]), reads=[b_bank[1]], writes=[b_aT[sl]])

    def stageD(i):
        sl = i % 2
        P.op("pe", lambda g: g.matmul(out=banks[2][:, 256:512], lhsT=aT_t[sl][0:17, :], rhs=wgate[0:17, :], start=True, stop=True),
             reads=[b_aT[sl], b_const], writes=[b_bank[2]])
        P.op("act", lambda g: g.activation(out=e1, in_=banks[2][:, 256:512], func=AF.Exp, scale=-1.0), reads=[b_bank[2]], writes=[b_e1])
        P.op("act", lambda g: g.activation(out=sp_t[sl], in_=e1, func=AF.Ln, bias=1.0), reads=[b_e1], writes=[b_sp[sl]])

    def stageE(i):
        own = i >= NT_PRE
        sl = i % 2
        hT, bh = hT_of(i)
        spb = sp_t[sl]
        if own:
            P.op("pe", lambda g: g.matmul(out=banks[3][:, 0:256], lhsT=tril[:], rhs=spb, start=True, stop=True),
                 reads=[b_sp[sl], b_const], writes=[b_bank[3]])
        P.op("pe", lambda g: g.matmul(out=banks[3][:, 256:512], lhsT=triu[:], rhs=spb, start=True, stop=True),
             reads=[b_sp[sl], b_const], writes=[b_bank[3]])
        for h in range(4):
            P.op("pe", lambda g, h=h: g.matmul(out=banks[7][0:64, 128 + h:129 + h], lhsT=spb[:, h * 64:(h + 1) * 64], rhs=negcol[:],
                                              start=True, stop=True),
                 reads=[b_sp[sl], b_const], writes=[b_bank[7]])
        if own:
            ncol, c0 = 512, 0
        else:
            ncol, c0 = 256, 256
        for k in range(8):
            P.op("pe", lambda g, k=k: g.matmul(out=banks[4][:, 0:ncol], lhsT=hT[:, k, :], rhs=Wqkva[:, k, c0:c0 + ncol],
                                              start=(k == 0), stop=(k == 7)),
                 reads=[bh, b_wqkva, b_wq], writes=[b_bank[4]])
        for k in range(8):
            P.op("pe", lambda g, k=k: g.matmul(out=banks[5][:, 0:512], lhsT=hT[:, k, :], rhs=Wqkva[:, k, 512:1024],
                                              start=(k == 0), stop=(k == 7)),
                 reads=[bh, b_wqkva], writes=[b_bank[5]])
        P.op("act", lambda g: g.activation(out=Er, in_=banks[3][:, 256:512], func=AF.Exp), reads=[b_bank[3]], writes=[b_Er])
        if own:
            P.op("act", lambda g: g.activation(out=Eq, in_=banks[3][:, 0:256], func=AF.Exp), reads=[b_bank[3]], writes=[b_Eq])
            P.op("act", lambda g: g.activation(out=Ek, in_=banks[3][:, 0:256], func=AF.Exp, scale=-1.0), reads=[b_bank[3]], writes=[b_Ek])
        P.op("act", lambda g: g.activation(out=El_t[sl][0:64, 0:4], in_=banks[7][0:64, 128:132], func=AF.Exp),
             reads=[b_bank[7]], writes=[b_El[sl]])
        kcol = 256 if own else 0
        P.op("dve", lambda g: g.tensor_tensor(out=kd_t[sl], in0=banks[4][:, kcol:kcol + 256], in1=Er, op=ALU.mult),
             reads=[b_bank[4], b_Er], writes=[b_kd[sl]])
        if own:
            P.op("dve", lambda g: g.scalar_tensor_tensor(out=qt_t[sl], in0=banks[4][:, 0:256], scalar=0.125, in1=Eq, op0=ALU.mult, op1=ALU.mult),
                 reads=[b_bank[4], b_Eq], writes=[b_qt[sl]])
            P.op("dve", lambda g: g.tensor_tensor(out=kt_t[sl], in0=banks[4][:, 256:512], in1=Ek, op=ALU.mult),
                 reads=[b_bank[4], b_Ek], writes=[b_kt[sl]])
        vb, bvb = vb_of(i)
        P.op("dve", lambda g: g.tensor_copy(out=vb, in_=banks[5][:, 0:512]), reads=[b_bank[5]], writes=[bvb])

    def stageF(i):
        sl = i % 2
        kd = kd_t[sl]
        vb, bvb = vb_of(i)
        for h in range(4):
            P.op("pe", lambda g, h=h: g.matmul(out=banks[6][0:64, h * 128:(h + 1) * 128], lhsT=kd[:, h * 64:(h + 1) * 64],
                                              rhs=vb[:, h * 128:(h + 1) * 128], start=True, stop=True),
                 reads=[b_kd[sl], bvb], writes=[b_bank[6]])

    def stageS(i):
        sl = i % 2
        P.op("dve", lambda g: g.tensor_tensor(out=S_v[0:64, :, :], in0=S_v[0:64, :, :],
                                              in1=El_t[sl][0:64, 0:4].unsqueeze(2).broadcast_to([64, 4, 128]), op=ALU.mult),
             reads=[b_S, b_El[sl]], writes=[b_S])
        P.op("dve", lambda g: g.tensor_tensor(out=S[0:64, :], in0=S[0:64, :], in1=banks[6][0:64, 0:512], op=ALU.add),
             reads=[b_S, b_bank[6]], writes=[b_S])

    def stageFo(i):
        sl = i % 2
        stageF(i)
        P.op("act", lambda g: g.copy(out=Sb_t[sl][0:64, :], in_=S[0:64, :]), reads=[b_S], writes=[b_Sb[sl]])
        stageS(i)
        for j in range(8):
            src = qt_t[sl] if j < 4 else kt_t[sl]
            a = j % 4
            P.op("pe", lambda g, j=j, src=src, a=a: g.transpose(out=bank7b[0:64, j * 128:(j + 1) * 128], in_=src[:, a * 64:(a + 1) * 64],
                                                               identity=identb[:]),
                 reads=[b_qt[sl], b_kt[sl], b_const], writes=[b_bank[7]])
        P.op("act", lambda g: g.copy(out=qkT_t[sl][0:64, :], in_=bank7b[0:64, :]), reads=[b_bank[7]], writes=[b_qkT[sl]])

    def stageHI(i):
        sl = i % 2
        vb, bvb = vb_of(i)
        qkT = qkT_t[sl]
        Sb_v = Sb_t[sl].rearrange("p (h e) -> p h e", h=4)
        for h in range(4):
            P.op("pe", lambda g, h=h: g.matmul(out=banks[2][:, h * 128:(h + 1) * 128], lhsT=qkT[0:64, (4 + h) * 128:(5 + h) * 128],
                                              rhs=qkT[0:64, h * 128:(h + 1) * 128], start=True, stop=True),
                 reads=[b_qkT[sl]], writes=[b_bank[2]])
        P.op("dve", lambda g: g.tensor_tensor(out=Pm, in0=banks[2][:, 0:512], in1=maskb[:], op=ALU.mult),
             reads=[b_bank[2], b_const], writes=[b_Pm])
        for h in range(4):
            P.op("pe", lambda g, h=h: g.matmul(out=banks[7][:, h * 128:(h + 1) * 128], lhsT=vb[:, h * 128:(h + 1) * 128],
                                              rhs=Pm[:, h * 128:(h + 1) * 128], start=True, stop=False),
                 reads=[bvb, b_Pm], writes=[b_bank[7]])
            P.op("pe", lambda g, h=h: g.matmul(out=banks[7][:, h * 128:(h + 1) * 128], lhsT=Sb_v[0:64, h, :],
                                              rhs=qkT[0:64, h * 128:(h + 1) * 128], start=False, stop=True),
                 reads=[b_Sb[sl], b_qkT[sl]], writes=[b_bank[7]])
        P.op("act", lambda g: g.activation(out=sq_t[sl], in_=banks[7][:, 0:512], func=AF.Square), reads=[b_bank[7]], writes=[b_sq[sl]])

    def stageJ(i):
        oi = i - NT_PRE
        sl = i % 2
        P.op("pe", lambda g: g.matmul(out=banks[2][:, 0:512], lhsT=onesb[:], rhs=sq_t[sl], start=True, stop=True),
             reads=[b_sq[sl], b_const], writes=[b_bank[2]])
        P.op("act", lambda g: g.activation(out=rstd, in_=banks[2][:, 0:512], func=AF.Ln, bias=EPS), reads=[b_bank[2]], writes=[b_rstd])
        P.op("act", lambda g: g.activation(out=rstd, in_=rstd, func=AF.Exp, scale=-0.5), reads=[b_rstd], writes=[b_rstd])
        P.op("dve", lambda g: g.tensor_tensor(out=y1_all_v[:, :, oi * 128:(oi + 1) * 128],
                                              in0=banks[7][:, 0:512].rearrange("p (h t) -> p h t", h=4),
                                              in1=rstd.rearrange("p (h t) -> p h t", h=4), op=ALU.mult),
             reads=[b_bank[7], b_rstd], writes=[b_y1[oi]])

    T0 = int(os.environ.get('K_T0', 0))
    T1 = int(os.environ.get('K_T1', NT_ALL))
    pre_stages = [stageA, stageB, stageC, stageD, stageE, lambda i: (stageF(i), stageS(i))]
    npre = min(T1, NT_PRE)
    for step in range(T0, npre + len(pre_stages) - 1):
        for k, st in enumerate(pre_stages):
            t = step - k
            if T0 <= t < npre:
                st(t)
    b_carry = Buf("carry")
    carry_v = carry_t[:].rearrange("p (c j) -> p c j", c=4)
    hT47, b_h47 = hT_of(NT_PRE - 1)
    for cc in range(4):
        for k in range(8):
            P.op("pe", lambda g, cc=cc, k=k: g.matmul(out=banks[2][:, 0:2], lhsT=Wgbcu[:, k, 1024 + cc * 128:1024 + (cc + 1) * 128],
                                                     rhs=hT47[:, k, 126:128], start=(k == 0), stop=(k == 7)),
                 reads=[b_h47, b_wg], writes=[b_bank[2]])
        for k in range(8):
            P.op("pe", lambda g, cc=cc, k=k: g.matmul(out=banks[3][:, 0:2], lhsT=Wgbcu[:, k, 1536 + cc * 128:1536 + (cc + 1) * 128],
                                                     rhs=hT47[:, k, 126:128], start=(k == 0), stop=(k == 7)),
                 reads=[b_h47, b_wg], writes=[b_bank[3]])
        P.op("act", lambda g: g.copy(out=small[:, 4:6], in_=banks[2][:, 0:2]), reads=[b_bank[2]], writes=[b_ss[0]])
        P.op("dve", lambda g, cc=cc: g.tensor_tensor(out=carry_v[:, cc, :], in0=banks[3][:, 0:2], in1=small[:, 4:6], op=ALU.mult),
             reads=[b_bank[3], b_ss[0]], writes=[b_carry])
    for dst, src in ((b_vb4[2], b_hTs[0]), (b_vb4[3], b_hTs[0]), (b_qkT[1], b_hTs[1]),
                     (b_Sb[1], b_hTs[2]), (b_sq[1], b_hTs[2])):
        dst.w = src.w
        dst.r = list(src.r)
    own_stages = [(stageJ, 7), (stageA, 0), (stageB, 1), (stageC, 2), (stageD, 3), (stageE, 4), (stageFo, 5), (stageHI, 6)]
    o0 = max(T0, NT_PRE)
    for step in range(o0, T1 + 7):
        for st, k in own_stages:
            t = step - k
            if o0 <= t < T1:
                st(t)

    if stop_after in ("p1", "p1s"):
        P.barrier(); P.emit(); return nc, es
    P.barrier()
    b_xr = [Buf("xr%d" % i) for i in range(NT_OWN)]
    for t in range(NT_OWN):
        ld("sp", xres[:, t * D:(t + 1) * D], xs_d[(NT_PRE + t) * 128:(NT_PRE + t + 1) * 128, :], b_xr[t], P.dsem())
    b_gB = Buf("gB")
    ds_gB = P.dsem()
    ld("sp", gB[:], gmoe_d.broadcast_to([128, D]), b_gB, ds_gB)

    c2 = Carver(scr2, 7200)
    gs = c2.f32(512)
    Ct = c2.f32(512)
    cu = c2.f32(520)
    acc = c2.f32(512)
    yg = c2.bf16(2048).rearrange("p (c t) -> p c t", c=4)
    ycv = c2.bf16(2048).rearrange("p (c t) -> p c t", c=4)
    h2_t = [c2.f32(1024) for _ in range(2)]
    h2Tf = c2.f32(1024).rearrange("p (k t) -> p k t", k=8)
    b_gs, b_Ct, b_cu, b_acc = Buf("gs"), Buf("Ct"), Buf("cu"), Buf("acc")
    b_yg, b_ycv, b_h2Tf, b_Lg = Buf("yg"), Buf("ycv"), Buf("h2Tf"), Buf("Lg")
    b_h2 = [Buf("h2a"), Buf("h2b")]
    b_LTs = Buf("LTs")

    Lg_v = Lg[:].rearrange("p (t n) -> p t n", t=NT_OWN)
    L2 = int(os.environ.get('K_L2', 99))
    b_s2 = [[Buf("s2_%d_%d" % (a, b)) for b in range(2)] for a in range(2)]
    for blk in range(4 if L2 >= 1 else 0):
        tcols = slice(blk * 512, (blk + 1) * 512)
        bts = [b_hT[blk * 4 + t] for t in range(4)]
        for cc in range(4):
            bb = 4 * (cc % 2)
            for k in range(8):
                P.op("pe", lambda g, cc=cc, k=k, bb=bb, tcols=tcols: g.matmul(out=banks[bb][:, 0:512], lhsT=Wgbcu[:, k, cc * 128:(cc + 1) * 128],
                                                                rhs=hT_all_v[:, k, tcols], start=(k == 0), stop=(k == 7)),
                     reads=bts + [b_wg], writes=[b_bank[bb]])
            P.op("act", lambda g, bb=bb: g.activation(out=gs, in_=banks[bb][:, 0:512], func=AF.Silu), reads=[b_bank[bb]], writes=[b_gs])
            P.op("dve", lambda g, cc=cc, tcols=tcols: g.scalar_tensor_tensor(out=yg[:, cc, :], in0=y1_all_v[:, cc, tcols], scalar=ggo[:, 0:1],
                                                                in1=gs, op0=ALU.mult, op1=ALU.mult),
                 reads=[b_gs, b_const] + [b_y1[blk * 4 + t] for t in range(4)], writes=[b_yg])
            for (bk, off) in ((bb + 1, 512), (bb + 2, 1024), (bb + 3, 1536)):
                for k in range(8):
                    P.op("pe", lambda g, cc=cc, k=k, bk=bk, off=off, tcols=tcols: g.matmul(
                        out=banks[bk][:, 0:512], lhsT=Wgbcu[:, k, off + cc * 128:off + (cc + 1) * 128],
                        rhs=hT_all_v[:, k, tcols], start=(k == 0), stop=(k == 7)),
                        reads=bts + [b_wg], writes=[b_bank[bk]])
            P.op("act", lambda g, bb=bb: g.copy(out=Ct, in_=banks[bb + 2][:, 0:512]), reads=[b_bank[bb + 2]], writes=[b_Ct])
            P.op("act", lambda g, cc=cc: g.copy(out=cu[:, 0:2], in_=carry_v[:, cc, :]), reads=[b_carry], writes=[b_cu])
            P.op("dve", lambda g, bb=bb: g.tensor_tensor(out=cu[:, 2:514], in0=banks[bb + 3][:, 0:512], in1=Ct, op=ALU.mult),
                 reads=[b_bank[bb + 3], b_Ct], writes=[b_cu])
            P.op("act", lambda g, cc=cc: g.copy(out=carry_v[:, cc, :], in_=cu[:, 512:514]), reads=[b_cu], writes=[b_carry])
            P.op("dve", lambda g, cc=cc: g.tensor_scalar(out=acc, in0=cu[:, 2:514], scalar1=wconv[:, cc * 3 + 2:cc * 3 + 3], scalar2=None,
                                                         op0=ALU.mult),
                 reads=[b_cu, b_const], writes=[b_acc])
            P.op("dve", lambda g, cc=cc: g.scalar_tensor_tensor(out=acc, in0=cu[:, 1:513], scalar=wconv[:, cc * 3 + 1:cc * 3 + 2], in1=acc,
                                                                op0=ALU.mult, op1=ALU.add),
                 reads=[b_cu, b_const], writes=[b_acc])
            P.op("dve", lambda g, cc=cc: g.scalar_tensor_tensor(out=acc, in0=cu[:, 0:512], scalar=wconv[:, cc * 3:cc * 3 + 1], in1=acc,
                                                                op0=ALU.mult, op1=ALU.add),
                 reads=[b_cu, b_const], writes=[b_acc])
            P.op("dve", lambda g, cc=cc, bb=bb: g.tensor_tensor(out=ycv[:, cc, :], in0=banks[bb + 1][:, 0:512], in1=acc, op=ALU.mult),
                 reads=[b_bank[bb + 1], b_acc], writes=[b_ycv])

        if blk == 3:
            load_expert(0, extra=[b_wg])

        def st2P(t):
            ti = blk * 4 + t
            sl = t % 2
            xr = xres[:, ti * D:(ti + 1) * D]
            for half in range(2):
                for kc in range(8):
                    src = yg if kc < 4 else ycv
                    bsrc = b_yg if kc < 4 else b_ycv
                    P.op("pe", lambda g, half=half, kc=kc, src=src: g.matmul(
                        out=banks[half][:, 0:512], lhsT=src[:, kc % 4, t * 128:(t + 1) * 128],
                        rhs=Wout[:, kc, half * 512:(half + 1) * 512], start=(kc == 0), stop=(kc == 7)),
                        reads=[bsrc, b_wo], writes=[b_bank[half]])
                P.op("dve", lambda g, half=half: g.tensor_tensor(out=xr[:, half * 512:(half + 1) * 512], in0=banks[half][:, 0:512],
                                                                 in1=xr[:, half * 512:(half + 1) * 512], op=ALU.add),
                     reads=[b_bank[half]], writes=[b_xr[ti]])
            c = 2 + 8 * sl
            rms_stats(xr, b_xr[ti], c, c + 1, h2_t[sl].bitcast(BF16)[:, 0:1024], b_h2[sl], b_s2[sl][0], b_s2[sl][1])
            P.op("dve", lambda g: g.scalar_tensor_tensor(out=h2_t[sl], in0=xr, scalar=small[:, c + 1:c + 2], in1=gB[:], op0=ALU.mult, op1=ALU.mult),
                 reads=[b_xr[ti], b_s2[sl][1], b_gB], writes=[b_h2[sl]])

        def st2Q(t):
            ti = blk * 4 + t
            sl = t % 2
            h2 = h2_t[sl]
            for k in range(8):
                bk = 2 + k // 4
                P.op("pe", lambda g, k=k, bk=bk: g.transpose(out=banks[bk][:, (k % 4) * 128:(k % 4 + 1) * 128], in_=h2[:, k * 128:(k + 1) * 128],
                                                            identity=identf[:]),
                     reads=[b_h2[sl], b_const], writes=[b_bank[bk]])
            for hh in range(2):
                P.op("dve", lambda g, hh=hh: g.tensor_copy(out=h2Tf[:, hh * 4:(hh + 1) * 4, :],
                                                           in_=banks[2 + hh][:, 0:512].rearrange("p (k t) -> p k t", k=4)),
                     reads=[b_bank[2 + hh]], writes=[b_h2Tf])
            P.op("act", lambda g: g.copy(out=hT_all_v[:, :, ti * 128:(ti + 1) * 128], in_=h2Tf),
                 reads=[b_h2Tf], writes=[b_hT[ti]])

        def st2R(t):
            for k in range(8):
                P.op("pe", lambda g, k=k: g.matmul(out=banks[4][0:20, 0:128], lhsT=wr[:, k * 20:(k + 1) * 20], rhs=h2Tf[:, k, :],
                                                  start=(k == 0), stop=(k == 7)),
                     reads=[b_h2Tf, b_const], writes=[b_bank[4]])
            P.op("act", lambda g: g.copy(out=LTs[0:20, :], in_=banks[4][0:20, 0:128]), reads=[b_bank[4]], writes=[b_LTs])

        def st2S(t):
            ti = blk * 4 + t
            P.op("pe", lambda g: g.matmul(out=banks[5][:, 0:20], lhsT=LTs[0:20, :], rhs=identf[0:20, 0:20], start=True, stop=True),
                 reads=[b_LTs, b_const], writes=[b_bank[5]])
            P.op("dve", lambda g: g.tensor_tensor(out=Lg_v[:, ti, :], in0=banks[5][:, 0:20], in1=brb[:], op=ALU.add),
                 reads=[b_bank[5], b_const], writes=[b_Lg])

        for step in range(7):
            if step < 4:
                st2P(step)
            if 3 <= step:
                st2S(step - 3)
            if 2 <= step < 6:
                st2R(step - 2)
            if 1 <= step < 5:
                st2Q(step - 1)

    if stop_after == "p2":
        P.barrier(); P.emit(); return nc, es
    P.barrier()
    b_g5 = Buf("g5")
    ds_g5 = P.dsem()
    ld("sp", gA[:], gple_d.broadcast_to([128, D]), b_g5, ds_g5)
    ld("sp", gB[:], gfin_d.broadcast_to([128, D]), b_g5, ds_g5)
    c3 = Carver(scr2, 7200)
    T16 = NT_OWN
    gmax = c3.f32(16)
    ohg = c3.f32(64)
    ge = c3.f32(64)
    gsum = c3.f32(16)
    pg = c3.f32(16)
    gw = c3.f32(64)
    m1 = c3.f32(64)
    eq1 = c3.f32(256)
    el2 = c3.f32(256)
    m2 = c3.f32(64)
    sel = c3.f32(256)
    dd = c3.f32(256)
    ex = c3.f32(256)
    den = c3.f32(64)
    rden = c3.f32(64)
    wq = c3.f32(256)
    b_r = Buf("route")
    gl = Lg_v[:, :, 0:4]
    el4 = Lg_v[:, :, 4:20].rearrange("p t (g j) -> p t g j", g=4)

    def v3(ap, n=4):
        return ap.rearrange("p (t g) -> p t g", g=n)

    def v4(ap):
        return ap.rearrange("p (t g j) -> p t g j", g=4, j=4)

    def bc3(ap16):
        return ap16.unsqueeze(2).broadcast_to([128, T16, 4])

    def bc4(ap64):
        return ap64.rearrange("p (t g) -> p t g", g=4).unsqueeze(3).broadcast_to([128, T16, 4, 4])

    R = dict(reads=[b_r, b_Lg], writes=[b_r])
    P.op("dve", lambda g: g.tensor_reduce(out=gmax, in_=gl, axis=AX.X, op=ALU.max), **R)
    P.op("dve", lambda g: g.tensor_tensor(out=v3(ohg), in0=gl, in1=bc3(gmax), op=ALU.is_equal), **R)
    P.op("dve", lambda g: g.tensor_tensor(out=v3(ge), in0=gl, in1=bc3(gmax), op=ALU.subtract), **R)
    P.op("act", lambda g: g.activation(out=ge, in_=ge, func=AF.Exp), **R)
    P.op("dve", lambda g: g.tensor_reduce(out=gsum, in_=v3(ge), axis=AX.X, op=ALU.add), **R)
    P.op("dve", lambda g: g.reciprocal(out=pg, in_=gsum), **R)
    P.op("dve", lambda g: g.tensor_tensor(out=v3(gw), in0=v3(ohg), in1=bc3(pg), op=ALU.mult), **R)
    P.op("dve", lambda g: g.tensor_reduce(out=v3(m1), in_=el4, axis=AX.X, op=ALU.max), **R)
    P.op("dve", lambda g: g.tensor_tensor(out=v4(eq1), in0=el4, in1=bc4(m1), op=ALU.is_equal), **R)
    P.op("dve", lambda g: g.scalar_tensor_tensor(out=v4(el2), in0=v4(eq1), scalar=-1e30, in1=el4, op0=ALU.mult, op1=ALU.add), **R)
    P.op("dve", lambda g: g.tensor_reduce(out=v3(m2), in_=v4(el2), axis=AX.X, op=ALU.max), **R)
    P.op("dve", lambda g: g.tensor_tensor(out=v4(sel), in0=el4, in1=bc4(m2), op=ALU.is_ge), **R)
    P.op("dve", lambda g: g.tensor_tensor(out=v4(dd), in0=el4, in1=bc4(m1), op=ALU.subtract), **R)
    P.op("act", lambda g: g.activation(out=ex, in_=dd, func=AF.Exp), **R)
    P.op("dve", lambda g: g.tensor_tensor(out=ex, in0=ex, in1=sel, op=ALU.mult), **R)
    P.op("dve", lambda g: g.tensor_reduce(out=v3(den), in_=v4(ex), axis=AX.X, op=ALU.add), **R)
    P.op("dve", lambda g: g.reciprocal(out=rden, in_=den), **R)
    P.op("dve", lambda g: g.tensor_tensor(out=v4(wq), in0=v4(ex), in1=bc4(rden), op=ALU.mult), **R)
    b_comb = Buf("comb")
    P.op("dve", lambda g: g.tensor_tensor(out=v4(comb[:]), in0=v4(wq), in1=bc4(gw), op=ALU.mult), reads=[b_r], writes=[b_comb])
    comb_v = comb[:].rearrange("p (t e) -> p t e", e=16)

    if stop_after == "p3":
        P.barrier(); P.emit(); return nc, es
    P.barrier()
    c4 = Carver(scr2, 7200)
    hid = [c4.bf16(2048).rearrange("p (c t) -> p c t", c=4) for _ in range(2)]
    sg = [c4.f32(512) for _ in range(2)]
    b_hid = [Buf("hid0"), Buf("hid1")]
    b_sg = [Buf("sg0"), Buf("sg1")]
    cnt4 = {"it": 0, "dn": 0}

    def moe_gu(e, blk):
        s = e % 2
        Wg_e, Wu_e, Wd_e = WE[s]
        tcols = slice(blk * 512, (blk + 1) * 512)
        bts = [b_hT[blk * 4 + t] for t in range(4)]
        hs = (e * 4 + blk) % 2
        for hc in range(4):
            pb = (cnt4["it"] % 2) * 2
            cnt4["it"] += 1
            ss_ = cnt4["it"] % 2
            for k in range(8):
                P.op("pe", lambda g, k=k, hc=hc, pb=pb: g.matmul(
                    out=banks[pb][:, 0:512], lhsT=Wg_e[:, k, hc * 128:(hc + 1) * 128], rhs=hT_all_v[:, k, tcols],
                    start=(k == 0), stop=(k == 7)), reads=bts + [b_we[s]], writes=[b_bank[pb]])
            for k in range(8):
                P.op("pe", lambda g, k=k, hc=hc, pb=pb: g.matmul(
                    out=banks[pb + 1][:, 0:512], lhsT=Wu_e[:, k, hc * 128:(hc + 1) * 128], rhs=hT_all_v[:, k, tcols],
                    start=(k == 0), stop=(k == 7)), reads=bts + [b_we[s]], writes=[b_bank[pb + 1]])
            P.op("act", lambda g, pb=pb, ss_=ss_: g.activation(out=sg[ss_], in_=banks[pb][:, 0:512], func=AF.Silu),
                 reads=[b_bank[pb]], writes=[b_sg[ss_]])
            P.op("dve", lambda g, pb=pb, ss_=ss_, hc=hc: g.tensor_tensor(out=hid[hs][:, hc, :], in0=banks[pb + 1][:, 0:512],
                                                                         in1=sg[ss_], op=ALU.mult),
                 reads=[b_bank[pb + 1], b_sg[ss_]], writes=[b_hid[hs]])

    def moe_dn(e, blk):
        s = e % 2
        Wg_e, Wu_e, Wd_e = WE[s]
        hs = (e * 4 + blk) % 2
        for t in range(4):
            ti = blk * 4 + t
            xr = xres[:, ti * D:(ti + 1) * D]
            db = 4 + (cnt4["dn"] % 2) * 2
            cnt4["dn"] += 1
            for half in range(2):
                for hc in range(4):
                    P.op("pe", lambda g, t=t, half=half, hc=hc, db=db: g.matmul(
                        out=banks[db + half][:, 0:512], lhsT=hid[hs][:, hc, t * 128:(t + 1) * 128],
                        rhs=Wd_e[:, hc, half * 512:(half + 1) * 512], start=(hc == 0), stop=(hc == 3)),
                        reads=[b_hid[hs], b_we[s]], writes=[b_bank[db + half]])
                P.op("dve", lambda g, half=half, db=db, xr=xr, ti=ti: g.scalar_tensor_tensor(
                    out=xr[:, half * 512:(half + 1) * 512], in0=banks[db + half][:, 0:512], scalar=comb_v[:, ti, e:e + 1],
                    in1=xr[:, half * 512:(half + 1) * 512], op0=ALU.mult, op1=ALU.add),
                    reads=[b_bank[db + half], b_comb], writes=[b_xr[ti]])

    prev = None
    for e in range(NE):
        if e > 0:
            load_expert(e)
        for blk in range(4):
            moe_gu(e, blk)
            if prev is not None:
                moe_dn(*prev)
            prev = (e, blk)
            if e == NE - 1 and blk == 0:
                ld("pool", Wpg, wpg_d.rearrange("(k p) c -> p k c", p=128), [b_w5, b_we[0]], ds_w5)
                ld("pool", Wpp, wpp_d.rearrange("(k p) c -> p k c", p=128), [b_w5, b_we[0]], ds_w5)
    moe_dn(*prev)

    if stop_after == "p4":
        P.barrier(); P.emit(); return nc, es
    P.barrier()
    c5 = Carver(scr2, 7200)
    pt = [c5.f32(256) for _ in range(2)]
    ptb = [c5.bf16(256) for _ in range(2)]
    pT = [c5.bf16(256).rearrange("p (k t) -> p k t", k=2) for _ in range(2)]
    h3b = [c5.bf16(1024) for _ in range(2)]
    h3T = [c5.bf16(1024).rearrange("p (k t) -> p k t", k=8) for _ in range(2)]
    sig = c5.f32(1024)
    outst = [c5.f32(1024) for _ in range(2)]
    b_pt = [Buf("pt0"), Buf("pt1")]
    ds_pt = [P.dsem(), P.dsem()]
    b_ptb, b_pT = [Buf("ptb0"), Buf("ptb1")], [Buf("pT0"), Buf("pT1")]
    b_h3b, b_h3T = [Buf("h3b0"), Buf("h3b1")], [Buf("h3T0"), Buf("h3T1")]
    b_sig = Buf("sig")
    b_out = [Buf("o0"), Buf("o1")]
    ds_out = [P.dsem(), P.dsem()]
    b_s5 = [[Buf("s5_%d_%d" % (a, b)) for b in range(4)] for a in range(2)]
    bank1b = banks[1][:, 0:512].bitcast(BF16)
    last_store = []

    def st5X(ti):
        sl = ti % 2
        xr = xres[:, ti * D:(ti + 1) * D]
        ld("sp", pt[sl], p_d[ti * 128:(ti + 1) * 128, :], b_pt[sl], ds_pt[sl])
        c = 4 + 8 * sl
        rms_stats(xr, b_xr[ti], c, c + 1, h3b[sl], b_h3b[sl], b_s5[sl][0], b_s5[sl][1])
        P.op("dve", lambda g: g.scalar_tensor_tensor(out=h3b[sl], in0=xr, scalar=small[:, c + 1:c + 2], in1=gA[:], op0=ALU.mult, op1=ALU.mult),
             reads=[b_xr[ti], b_s5[sl][1], b_g5], writes=[b_h3b[sl]])
        P.op("dve", lambda g: g.tensor_copy(out=ptb[sl], in_=pt[sl]), reads=[b_pt[sl]], writes=[b_ptb[sl]])

    def st5Y(ti):
        sl = ti % 2
        for k in range(8):
            P.op("pe", lambda g, k=k: g.transpose(out=bank0b[:, k * 128:(k + 1) * 128], in_=h3b[sl][:, k * 128:(k + 1) * 128], identity=identb[:]),
                 reads=[b_h3b[sl], b_const], writes=[b_bank[0]])
        P.op("dve", lambda g: g.tensor_copy(out=h3T[sl], in_=bank0b.rearrange("p (k t) -> p k t", k=8)), reads=[b_bank[0]], writes=[b_h3T[sl]])
        for k in range(2):
            P.op("pe", lambda g, k=k: g.transpose(out=bank1b[:, k * 128:(k + 1) * 128], in_=ptb[sl][:, k * 128:(k + 1) * 128], identity=identb[:]),
                 reads=[b_ptb[sl], b_const], writes=[b_bank[1]])
        P.op("dve", lambda g: g.tensor_copy(out=pT[sl], in_=bank1b[:, 0:256].rearrange("p (k t) -> p k t", k=2)), reads=[b_bank[1]], writes=[b_pT[sl]])

    def st5Z(ti):
        sl = ti % 2
        xr = xres[:, ti * D:(ti + 1) * D]
        for half in range(2):
            for k in range(8):
                P.op("pe", lambda g, k=k, half=half: g.matmul(out=banks[2 + half][:, 0:512], lhsT=h3T[sl][:, k, :],
                                                             rhs=Wpg[:, k, half * 512:(half + 1) * 512], start=(k == 0), stop=(k == 7)),
                     reads=[b_h3T[sl], b_w5], writes=[b_bank[2 + half]])
            for k in range(2):
                P.op("pe", lambda g, k=k, half=half: g.matmul(out=banks[4 + half][:, 0:512], lhsT=pT[sl][:, k, :],
                                                             rhs=Wpp[:, k, half * 512:(half + 1) * 512], start=(k == 0), stop=(k == 1)),
                     reads=[b_pT[sl], b_w5], writes=[b_bank[4 + half]])
            P.op("act", lambda g, half=half: g.activation(out=sig[:, half * 512:(half + 1) * 512], in_=banks[2 + half][:, 0:512], func=AF.Sigmoid),
                 reads=[b_bank[2 + half]], writes=[b_sig])
            P.op("dve", lambda g, half=half: g.tensor_tensor(out=sig[:, half * 512:(half + 1) * 512], in0=banks[4 + half][:, 0:512],
                                                             in1=sig[:, half * 512:(half + 1) * 512], op=ALU.mult),
                 reads=[b_bank[4 + half], b_sig], writes=[b_sig])
        P.op("dve", lambda g: g.tensor_tensor(out=xr, in0=sig, in1=xr, op=ALU.add), reads=[b_sig], writes=[b_xr[ti]])

    def st5W(ti):
        sl = ti % 2
        xr = xres[:, ti * D:(ti + 1) * D]
        c = 6 + 8 * sl
        rms_stats(xr, b_xr[ti], c, c + 1, outst[sl].bitcast(BF16)[:, 0:1024], b_out[sl], b_s5[sl][2], b_s5[sl][3])
        P.op("dve", lambda g: g.scalar_tensor_tensor(out=outst[sl], in0=xr, scalar=small[:, c + 1:c + 2], in1=gB[:], op0=ALU.mult, op1=ALU.mult),
             reads=[b_xr[ti], b_s5[sl][3], b_g5], writes=[b_out[sl]])
        tok = P.op("pool", lambda g: g.dma_start(out=y_d[ti * 128:(ti + 1) * 128, :], in_=outst[sl]),
                   reads=[b_out[sl]], dsem=ds_out[sl])
        last_store.append(tok)

    st5 = [st5X, st5Y, st5Z, st5W]
    for step in range(NT_OWN + len(st5) - 1):
        for k, st in enumerate(st5):
            t = step - k
            if 0 <= t < NT_OWN:
                st(t)
    P.wait_only("pool", last_store[-2:])
    P.wait_only("sp", last_store[-2:])
    P.emit()
    return nc, es


_CACHE = {}


def _consts():
    s = np.arange(128)[:, None]
    t = np.arange(128)[None, :]
    tril = np.where(s <= t, -1.0 / 16.0, 0.0).astype(np.float32)
    triu = np.where(s > t, -1.0 / 16.0, 0.0).astype(np.float32)
    mask = np.tile((s <= t).astype(np.float32), (1, 4))
    return {
        "c_ident": np.eye(128, dtype=np.float32),
        "c_tril": tril,
        "c_triu": triu,
        "c_mask": np.ascontiguousarray(mask),
        "c_ones": np.full((128, 128), 1.0 / 128.0, np.float32),
        "c_neg": np.full((128, 1), -1.0 / 16.0, np.float32),
    }


def kernel(x, p, g_mix, w_in, w_gla_gate, b_gla_gate, g_gla_out, w_conv, w_out,
           g_moe, w_group, b_group, w_router, b_router, w_exp_gate, w_exp_up, w_exp_down,
           g_ple, w_ple_gate, w_ple_proj, g_final):
    f = lambda a: np.ascontiguousarray(np.asarray(a, dtype=np.float32))
    x = f(x); p = f(p)
    if "nc" not in _CACHE:
        _CACHE["nc"] = build_program()
    nc, _es = _CACHE["nc"]
    wg_aug = np.zeros((32, 256), np.float32)
    wg_aug[0:16] = f(w_gla_gate)[0]
    wg_aug[16] = f(b_gla_gate)[0]
    shared = {
        "w_in": f(w_in)[0], "w_out": f(w_out)[0],
        "w_exp_gate": f(w_exp_gate)[0].reshape(NE * D, 512),
        "w_exp_up": f(w_exp_up)[0].reshape(NE * D, 512),
        "w_exp_down": f(w_exp_down)[0].reshape(NE * 512, D),
        "w_ple_gate": f(w_ple_gate)[0], "w_ple_proj": f(w_ple_proj)[0],
        "g_mix": f(g_mix).reshape(1, D), "g_moe": f(g_moe).reshape(1, D), "g_ple": f(g_ple).reshape(1, D),
        "g_final": f(g_final).reshape(1, D),
        "w_rt": np.ascontiguousarray(np.concatenate([f(w_group)[0], f(w_router)[0]], axis=1)),
        "b_rt": np.ascontiguousarray(np.concatenate([f(b_group)[0], f(b_router)[0]], axis=0).reshape(1, 20)),
        "wg_aug": wg_aug,
        "wconv_t": np.ascontiguousarray(f(w_conv)[0].reshape(3, 4, 128).transpose(2, 1, 0).reshape(128, 12)),
        "ggo": np.ascontiguousarray(f(g_gla_out)[0].reshape(128, 1)),
    }
    shared.update(_consts())
    in_maps = []
    for c in range(8):
        b, j = c // 4, c % 4
        xs = np.zeros((NT_ALL * 128, D), np.float32)
        n = 2048 * (j + 1)
        xs[NT_ALL * 128 - n:] = x[b, 0:n]
        m = dict(shared)
        m["xs"] = xs
        m["p_own"] = np.ascontiguousarray(p[0, b, 2048 * j:2048 * (j + 1)])
        in_maps.append(m)
    res = run_bass_kernel_spmd(nc, in_maps, core_ids=list(range(8)))
    out = np.empty((2, 8192, D), np.float32)
    for c in range(8):
        b, j = c // 4, c % 4
        out[b, 2048 * j:2048 * (j + 1)] = res.results[c]["y"]
    return out
```

```python
import numpy as np
from contextlib import ExitStack
import concourse.bass as bass
import concourse.mybir as mybir
from concourse.bass_utils import run_bass_kernel_spmd

F32 = mybir.dt.float32
BF16 = mybir.dt.bfloat16
AF = mybir.ActivationFunctionType
ALU = mybir.AluOpType
AX = mybir.AxisListType

EPS = 1e-6
NT_ALL = 64
NT_OWN = 16
NT_PRE = NT_ALL - NT_OWN
D = 1024
DIN = 3088
NE = 16


class Buf:
    __slots__ = ("name", "w", "r")

    def __init__(self, name):
        self.name = name
        self.w = None
        self.r = []


class DSem:
    def __init__(self, h):
        self.h = h
        self.count = 0
        self.nobar = False


class Prog:
    def __init__(self, nc, es):
        self.nc = nc
        self.es = es
        self.names = ["pe", "act", "dve", "pool", "sp"]
        self.streams = {k: [] for k in self.names}
        self.sem = {k: es.enter_context(nc.semaphore("c_" + k)) for k in ["pe", "act", "dve"]}
        self.cnt = {k: 0 for k in self.sem}
        self.known = {k: {} for k in self.names}
        self.handles = {}
        self.dsems = []
        self.nds = 0

    def dsem(self):
        self.nds += 1
        s = DSem(self.es.enter_context(self.nc.semaphore("d%d" % self.nds)))
        self.dsems.append(s)
        return s

    def _filter(self, e, toks):
        need = {}
        for (s, v) in toks:
            if e == "pe" and s is self.sem["pe"]:
                continue
            k = id(s)
            self.handles[k] = s
            if need.get(k, 0) < v:
                need[k] = v
        out = []
        kn = self.known[e]
        for k, v in need.items():
            if kn.get(k, 0) < v:
                kn[k] = v
                out.append((self.handles[k], v))
        return out

    def op(self, e, fn, reads=(), writes=(), dsem=None):
        toks = []
        for b in reads:
            if b.w is not None:
                toks.append(b.w)
        for b in writes:
            toks.extend(b.r)
            if b.w is not None:
                toks.append(b.w)
        waits = self._filter(e, toks)
        if dsem is None:
            self.cnt[e] += 1
            tok = (self.sem[e], self.cnt[e])
            inc = 1
        else:
            dsem.count += 16
            tok = (dsem.h, dsem.count)
            inc = 16
        self.streams[e].append((waits, fn, tok[0], inc))
        for b in writes:
            b.w = tok
            b.r = []
        for b in reads:
            b.r.append(tok)
        return tok

    def wait_only(self, e, toks):
        waits = self._filter(e, toks)
        if waits:
            self.streams[e].append((waits, None, None, 0))

    def barrier(self):
        toks = [(self.sem[k], self.cnt[k]) for k in self.sem if self.cnt[k] > 0]
        toks += [(d.h, d.count) for d in self.dsems if d.count > 0 and not d.nobar]
        for e in self.names:
            self.wait_only(e, toks)

    def emit(self):
        nc = self.nc
        with nc.Block() as block:
            decos = {"pe": block.tensor, "act": block.scalar, "dve": block.vector,
                     "pool": block.gpsimd, "sp": block.sync}
            for k in self.names:
                stream = self.streams[k]

                def body(engine, stream=stream):
                    for waits, fn, sem, inc in stream:
                        for (s, v) in waits:
                            engine.wait_ge(s, v)
                        if fn is not None:
                            ins = fn(engine)
                            ins.then_inc(sem, inc)

                decos[k](body)


def build_program(stop_after=None):
    nc = bass.Bass("TRN2", target_bir_lowering=False)
    es = ExitStack()

    def din(name, shape):
        return nc.dram_tensor(name, list(shape), F32, kind="ExternalInput").ap()

    xs_d = din("xs", [NT_ALL * 128, D])
    p_d = din("p_own", [NT_OWN * 128, 256])
    w_in_d = din("w_in", [D, DIN])
    w_out_d = din("w_out", [D, D])
    wg_d = din("w_exp_gate", [NE * D, 512])
    wu_d = din("w_exp_up", [NE * D, 512])
    wd_d = din("w_exp_down", [NE * 512, D])
    wpg_d = din("w_ple_gate", [D, D])
    wpp_d = din("w_ple_proj", [256, D])
    gmix_d = din("g_mix", [1, D])
    gmoe_d = din("g_moe", [1, D])
    gple_d = din("g_ple", [1, D])
    gfin_d = din("g_final", [1, D])
    wr_d = din("w_rt", [D, 20])
    br_d = din("b_rt", [1, 20])
    wgate_d = din("wg_aug", [32, 256])
    wconv_d = din("wconv_t", [128, 12])
    ggo_d = din("ggo", [128, 1])
    cid_d = din("c_ident", [128, 128])
    ctl_d = din("c_tril", [128, 128])
    ctu_d = din("c_triu", [128, 128])
    cmask_d = din("c_mask", [128, 512])
    cones_d = din("c_ones", [128, 128])
    cneg_d = din("c_neg", [128, 1])
    y_d = nc.dram_tensor("y", [NT_OWN * 128, D], F32, kind="ExternalOutput").ap()

    def sb(name, shape, dt):
        return es.enter_context(nc.sbuf_tensor("s_" + name, list(shape), dt))

    xres = sb("xres", [128, NT_OWN * D], F32)
    hT_all = sb("hT_all", [128, 8 * 2048], BF16)
    y1_all = sb("y1_all", [128, 4 * 2048], BF16)
    wbig = sb("wbig", [128, 24704], BF16)
    scr2 = sb("scr2", [128, 7200], F32)
    carry_t = sb("carry", [128, 8], F32)
    identb = sb("identb", [128, 128], BF16)
    identf = sb("identf", [128, 128], F32)
    tril = sb("tril", [128, 128], F32)
    triu = sb("triu", [128, 128], F32)
    maskb = sb("maskb", [128, 512], BF16)
    onesb = sb("onesb", [128, 128], BF16)
    negcol = sb("negcol", [128, 1], F32)
    gA = sb("gA", [128, D], F32)
    gB = sb("gB", [128, D], F32)
    wr = sb("wr", [128, 8 * 20], F32)
    brb = sb("brb", [128, 20], F32)
    wgate = sb("wgate", [32, 256], F32)
    wconv = sb("wconv", [128, 12], F32)
    ggo = sb("ggo", [128, 1], F32)
    Lg = sb("Lg", [128, NT_OWN * 20], F32)
    comb = sb("comb", [128, NT_OWN * 16], F32)
    small = sb("small", [128, 16], F32)
    LTs = sb("LTs", [32, 128], F32)

    banks = [es.enter_context(nc.psum_tensor("bank%d" % i, [128, 512], F32)) for i in range(8)]

    P = Prog(nc, es)

    class Carver:
        def __init__(self, base, nwords):
            self.base = base
            self.off = 0
            self.n = nwords

        def f32(self, n):
            a = self.base[:, self.off:self.off + n]
            self.off += n
            assert self.off <= self.n, (self.off, self.n)
            return a

        def bf16(self, n):
            assert n % 2 == 0
            return self.f32(n // 2).bitcast(BF16)

    b_const = Buf("const")
    ds_const = P.dsem()

    def ld(e, out_ap, in_ap, buf, dsem):
        bufs = buf if isinstance(buf, (list, tuple)) else [buf]
        return P.op(e, lambda g: g.dma_start(out=out_ap, in_=in_ap), writes=list(bufs), dsem=dsem)

    ld("sp", identf[:], cid_d, b_const, ds_const)
    ld("sp", tril[:], ctl_d, b_const, ds_const)
    ld("sp", triu[:], ctu_d, b_const, ds_const)
    ld("sp", negcol[:], cneg_d, b_const, ds_const)
    ld("sp", gA[:], gmix_d.broadcast_to([128, D]), b_const, ds_const)
    ld("sp", wr[:].rearrange("p (k n) -> p k n", k=8), wr_d.rearrange("(k p) n -> p k n", p=128), b_const, ds_const)
    ld("sp", brb[:], br_d.broadcast_to([128, 20]), b_const, ds_const)
    ld("sp", wgate[:], wgate_d, b_const, ds_const)
    ld("sp", wconv[:], wconv_d, b_const, ds_const)
    ld("sp", ggo[:], ggo_d, b_const, ds_const)
    ds_const2 = P.dsem()
    ld("pool", identb[:], cid_d, b_const, ds_const2)
    ld("pool", maskb[:], cmask_d, b_const, ds_const2)
    ld("pool", onesb[:], cones_d, b_const, ds_const2)

    c1 = Carver(xres, NT_OWN * D)
    Wqkva = c1.bf16(8 * 1296).rearrange("p (k c) -> p k c", k=8)
    xs_t = [c1.f32(1024) for _ in range(2)]
    hb_t = [c1.bf16(1024) for _ in range(2)]
    hts_off = c1.off
    hTs = [c1.bf16(1024).rearrange("p (k t) -> p k t", k=8) for _ in range(4)]
    aT_t = [c1.f32(128) for _ in range(2)]
    e1 = c1.f32(256)
    sp_t = [c1.f32(256) for _ in range(2)]
    Er = c1.f32(256)
    El_t = [c1.f32(8) for _ in range(2)]
    kd_t = [c1.bf16(256) for _ in range(2)]
    vb_t = [c1.bf16(512) for _ in range(2)]
    Eq = c1.f32(256)
    Ek = c1.f32(256)
    qt_t = [c1.bf16(256) for _ in range(2)]
    kt_t = [c1.bf16(256) for _ in range(2)]
    qkT0 = c1.bf16(1024)
    Pm = c1.bf16(512)
    S = c1.f32(512)
    Sb0 = c1.bf16(512)
    sq0 = c1.bf16(512)
    rstd = c1.f32(512)
    c1o = Carver(xres, hts_off + 1536)
    c1o.off = hts_off
    vb4 = [vb_t[0], vb_t[1], c1o.bf16(512), c1o.bf16(512)]
    qkT_t = [qkT0, c1o.bf16(1024)]
    Sb_t = [Sb0, c1o.bf16(512)]
    sq_t = [sq0, c1o.bf16(512)]

    w_in_v = w_in_d.rearrange("(k p) c -> p k c", p=128)
    b_wqkva = Buf("wqkva")
    ds_w1 = P.dsem()
    b_wa = Buf("wa")
    ds_wa = P.dsem()
    ld("pool", Wqkva[:, :, 1024:1040], w_in_v[:, :, 1536:1552], b_wa, ds_wa)
    ld("pool", Wqkva[:, :, 256:1024], w_in_v[:, :, 256:1024], b_wqkva, ds_w1)
    b_wq = Buf("wq")
    ds_wq = P.dsem()
    ld("pool", Wqkva[:, :, 0:256], w_in_v[:, :, 0:256], b_wq, ds_wq)

    Wgbcu = wbig[:, 0:8 * 2048].rearrange("p (k c) -> p k c", k=8)
    Wout = wbig[:, 8 * 2048:8 * 3072].rearrange("p (k c) -> p k c", k=8)
    b_wg = Buf("wgbcu")
    b_wo = Buf("wout")
    ds_w2 = P.dsem()
    ds_wo = P.dsem()
    ld("pool", Wgbcu[:, :, 0:512], w_in_v[:, :, 1024:1536], b_wg, ds_w2)
    ld("pool", Wgbcu[:, :, 512:2048], w_in_v[:, :, 1552:3088], b_wg, ds_w2)
    ld("pool", Wout, w_out_d.rearrange("(k p) c -> p k c", p=128), b_wo, ds_wo)
    WE = []
    for s_ in range(2):
        base = s_ * 12288
        WE.append((wbig[:, base:base + 4096].rearrange("p (k c) -> p k c", k=8),
                   wbig[:, base + 4096:base + 8192].rearrange("p (k c) -> p k c", k=8),
                   wbig[:, base + 8192:base + 12288].rearrange("p (k c) -> p k c", k=4)))
    b_we = [Buf("we0"), Buf("we1")]
    ds_we = [P.dsem(), P.dsem()]
    ds_we[0].nobar = True
    ds_we[1].nobar = True
    Wpg = wbig[:, 0:8192].rearrange("p (k c) -> p k c", k=8)
    Wpp = wbig[:, 8192:8192 + 2048].rearrange("p (k c) -> p k c", k=2)
    b_w5 = Buf("w5")
    ds_w5 = P.dsem()
    ds_w5.nobar = True

    def load_expert(e, extra=()):
        s_ = e % 2
        Wg_e, Wu_e, Wd_e = WE[s_]
        bufs = [b_we[s_]] + list(extra)
        ld("pool", Wg_e, wg_d[e * D:(e + 1) * D, :].rearrange("(k p) c -> p k c", p=128), bufs, ds_we[s_])
        ld("pool", Wu_e, wu_d[e * D:(e + 1) * D, :].rearrange("(k p) c -> p k c", p=128), bufs, ds_we[s_])
        ld("pool", Wd_e, wd_d[e * 512:(e + 1) * 512, :].rearrange("(k p) c -> p k c", p=128), bufs, ds_we[s_])

    if stop_after == "consts":
        P.barrier(); P.emit(); return nc, es
    import os
    b_xs = [Buf("xs0"), Buf("xs1")]
    ds_xs = [P.dsem(), P.dsem()]
    b_hb = [Buf("hb0"), Buf("hb1")]
    b_hTs = [Buf("hTs%d" % i) for i in range(4)]
    b_hT = [Buf("hT%d" % i) for i in range(NT_OWN)]
    b_y1 = [Buf("y1_%d" % i) for i in range(NT_OWN)]
    b_bank = [Buf("bank%d" % i) for i in range(8)]
    b_aTp = Buf("aTp")
    b_lastp = Buf("lastp")
    b_zp = Buf("zp")
    b_aT = [Buf("aT0"), Buf("aT1")]
    b_e1 = Buf("e1")
    b_sp = [Buf("sp0"), Buf("sp1")]
    b_Eq, b_Ek, b_Er = Buf("Eq"), Buf("Ek"), Buf("Er")
    b_El = [Buf("El0"), Buf("El1")]
    b_kd = [Buf("kd0"), Buf("kd1")]
    b_vb = [Buf("vb0"), Buf("vb1")]
    b_qt, b_kt = [Buf("qt0"), Buf("qt1")], [Buf("kt0"), Buf("kt1")]
    b_Pm, b_S, b_rstd = Buf("Pm"), Buf("S"), Buf("rstd")
    b_qkT, b_Sb, b_sq = [Buf("qkT0"), Buf("qkT1")], [Buf("Sb0"), Buf("Sb1")], [Buf("sq0"), Buf("sq1")]
    b_vb4 = [b_vb[0], b_vb[1], Buf("vb2"), Buf("vb3")]

    def vb_of(i):
        if i >= NT_PRE:
            return vb4[i % 4], b_vb4[i % 4]
        return vb_t[i % 2], b_vb[i % 2]
    b_ss = [Buf("ss0"), Buf("ss1")]
    b_rs = [Buf("rs0"), Buf("rs1")]

    bank0b = banks[0][:, 0:512].bitcast(BF16)
    bank7b = banks[7][:, 0:512].bitcast(BF16)

    hT_all_v = hT_all[:].rearrange("p (k t) -> p k t", k=8)
    y1_all_v = y1_all[:].rearrange("p (h t) -> p h t", h=4)
    S_v = S.rearrange("p (h e) -> p h e", h=4)

    P.op("dve", lambda g: g.memset(S[0:64, :], 0.0), writes=[b_S])
    for a_ in range(2):
        P.op("dve", lambda g, a_=a_: g.memset(aT_t[a_][0:32, :], 1.0), writes=[b_aT[a_]])

    def rms_stats(src_ap, b_src, ss_col, rs_col, junk, b_junk, bss=None, brs=None):
        bss = bss or b_ss[0]
        brs = brs or b_rs[0]
        P.op("act", lambda g: g.activation(out=junk, in_=src_ap, func=AF.Square, accum_out=small[:, ss_col:ss_col + 1]),
             reads=[b_src], writes=[b_junk, bss])
        P.op("act", lambda g: g.activation(out=small[:, rs_col:rs_col + 1], in_=small[:, ss_col:ss_col + 1], func=AF.Ln,
                                           scale=1.0 / D, bias=EPS),
             reads=[bss], writes=[brs])
        P.op("act", lambda g: g.activation(out=small[:, rs_col:rs_col + 1], in_=small[:, rs_col:rs_col + 1], func=AF.Exp, scale=-0.5),
             reads=[brs], writes=[brs])

    def hT_of(i):
        if i >= NT_PRE:
            oi = i - NT_PRE
            return hT_all_v[:, :, oi * 128:(oi + 1) * 128], b_hT[oi]
        return hTs[i % 4], b_hTs[i % 4]

    def stageA(i):
        sl = i % 2
        xt = xs_t[sl]
        ld("sp", xt, xs_d[i * 128:(i + 1) * 128, :], b_xs[sl], ds_xs[sl])
        hb = hb_t[sl]
        rms_stats(xt, b_xs[sl], 8 * sl, 8 * sl + 1, hb, b_hb[sl], b_ss[sl], b_rs[sl])
        P.op("dve", lambda g: g.scalar_tensor_tensor(out=hb, in0=xt, scalar=small[:, 8 * sl + 1:8 * sl + 2], in1=gA[:],
                                                     op0=ALU.mult, op1=ALU.mult),
             reads=[b_xs[sl], b_rs[sl], b_const], writes=[b_hb[sl]])

    def stageB(i):
        sl = i % 2
        hb = hb_t[sl]
        hT, bh = hT_of(i)
        for k in range(8):
            P.op("pe", lambda g, k=k: g.transpose(out=bank0b[:, k * 128:(k + 1) * 128], in_=hb[:, k * 128:(k + 1) * 128],
                                                 identity=identb[:]),
                 reads=[b_hb[sl], b_const], writes=[b_bank[0]])
        P.op("act", lambda g: g.copy(out=hT, in_=bank0b.rearrange("p (k t) -> p k t", k=8)),
             reads=[b_bank[0]], writes=[bh])

    def stageC(i):
        sl = i % 2
        hT, bh = hT_of(i)
        for k in range(8):
            P.op("pe", lambda g, k=k: g.matmul(out=banks[1][0:16, 0:128], lhsT=Wqkva[:, k, 1024:1040], rhs=hT[:, k, :],
                                              start=(k == 0), stop=(k == 7)),
                 reads=[bh, b_wa], writes=[b_bank[1]])
        P.op("act", lambda g: g.copy(out=aT_t[sl][0:16, :], in_=banks[1][0:16, 0:128]), reads=[b_bank[1]], writes=[b_aT[sl]])

    def stageD(i):
        sl = i % 2
        P.op("pe", lambda g: g.matmul(out=banks[2][:, 256:512], lhsT=aT_t[sl][0:17, :], rhs=wgate[0:17, :], start=True, stop=True),
             reads=[b_aT[sl], b_const], writes=[b_bank[2]])
        P.op("act", lambda g: g.activation(out=e1, in_=banks[2][:, 256:512], func=AF.Exp, scale=-1.0), reads=[b_bank[2]], writes=[b_e1])
        P.op("act", lambda g: g.activation(out=sp_t[sl], in_=e1, func=AF.Ln, bias=1.0), reads=[b_e1], writes=[b_sp[sl]])

    def stageE(i):
        own = i >= NT_PRE
        sl = i % 2
        hT, bh = hT_of(i)
        spb = sp_t[sl]
        if own:
            P.op("pe", lambda g: g.matmul(out=banks[3][:, 0:256], lhsT=tril[:], rhs=spb, start=True, stop=True),
                 reads=[b_sp[sl], b_const], writes=[b_bank[3]])
        P.op("pe", lambda g: g.matmul(out=banks[3][:, 256:512], lhsT=triu[:], rhs=spb, start=True, stop=True),
             reads=[b_sp[sl], b_const], writes=[b_bank[3]])
        for h in range(4):
            P.op("pe", lambda g, h=h: g.matmul(out=banks[7][0:64, 128 + h:129 + h], lhsT=spb[:, h * 64:(h + 1) * 64], rhs=negcol[:],
                                              start=True, stop=True),
                 reads=[b_sp[sl], b_const], writes=[b_bank[7]])
        if own:
            ncol, c0 = 512, 0
        else:
            ncol, c0 = 256, 256
        for k in range(8):
            P.op("pe", lambda g, k=k: g.matmul(out=banks[4][:, 0:ncol], lhsT=hT[:, k, :], rhs=Wqkva[:, k, c0:c0 + ncol],
                                              start=(k == 0), stop=(k == 7)),
                 reads=[bh, b_wqkva, b_wq], writes=[b_bank[4]])
        for k in range(8):
            P.op("pe", lambda g, k=k: g.matmul(out=banks[5][:, 0:512], lhsT=hT[:, k, :], rhs=Wqkva[:, k, 512:1024],
                                              start=(k == 0), stop=(k == 7)),
                 reads=[bh, b_wqkva], writes=[b_bank[5]])
        P.op("act", lambda g: g.activation(out=Er, in_=banks[3][:, 256:512], func=AF.Exp), reads=[b_bank[3]], writes=[b_Er])
        if own:
            P.op("act", lambda g: g.activation(out=Eq, in_=banks[3][:, 0:256], func=AF.Exp), reads=[b_bank[3]], writes=[b_Eq])
            P.op("act", lambda g: g.activation(out=Ek, in_=banks[3][:, 0:256], func=AF.Exp, scale=-1.0), reads=[b_bank[3]], writes=[b_Ek])
        P.op("act", lambda g: g.activation(out=El_t[sl][0:64, 0:4], in_=banks[7][0:64, 128:132], func=AF.Exp),
             reads=[b_bank[7]], writes=[b_El[sl]])
        kcol = 256 if own else 0
        P.op("dve", lambda g: g.tensor_tensor(out=kd_t[sl], in0=banks[4][:, kcol:kcol + 256], in1=Er, op=ALU.mult),
             reads=[b_bank[4], b_Er], writes=[b_kd[sl]])
        if own:
            P.op("dve", lambda g: g.scalar_tensor_tensor(out=qt_t[sl], in0=banks[4][:, 0:256], scalar=0.125, in1=Eq, op0=ALU.mult, op1=ALU.mult),
                 reads=[b_bank[4], b_Eq], writes=[b_qt[sl]])
            P.op("dve", lambda g: g.tensor_tensor(out=kt_t[sl], in0=banks[4][:, 256:512], in1=Ek, op=ALU.mult),
                 reads=[b_bank[4], b_Ek], writes=[b_kt[sl]])
        vb, bvb = vb_of(i)
        P.op("dve", lambda g: g.tensor_copy(out=vb, in_=banks[5][:, 0:512]), reads=[b_bank[5]], writes=[bvb])

    def stageF(i):
        sl = i % 2
        kd = kd_t[sl]
        vb, bvb = vb_of(i)
        for h in range(4):
            P.op("pe", lambda g, h=h: g.matmul(out=banks[6][0:64, h * 128:(h + 1) * 128], lhsT=kd[:, h * 64:(h + 1) * 64],
                                              rhs=vb[:, h * 128:(h + 1) * 128], start=True, stop=True),
                 reads=[b_kd[sl], bvb], writes=[b_bank[6]])

    def stageS(i):
        sl = i % 2
        P.op("dve", lambda g: g.tensor_tensor(out=S_v[0:64, :, :], in0=S_v[0:64, :, :],
                                              in1=El_t[sl][0:64, 0:4].unsqueeze(2).broadcast_to([64, 4, 128]), op=ALU.mult),
             reads=[b_S, b_El[sl]], writes=[b_S])
        P.op("dve", lambda g: g.tensor_tensor(out=S[0:64, :], in0=S[0:64, :], in1=banks[6][0:64, 0:512], op=ALU.add),
             reads=[b_S, b_bank[6]], writes=[b_S])

    def stageFo(i):
        sl = i % 2
        stageF(i)
        P.op("act", lambda g: g.copy(out=Sb_t[sl][0:64, :], in_=S[0:64, :]), reads=[b_S], writes=[b_Sb[sl]])
        stageS(i)
        for j in range(8):
            src = qt_t[sl] if j < 4 else kt_t[sl]
            a = j % 4
            P.op("pe", lambda g, j=j, src=src, a=a: g.transpose(out=bank7b[0:64, j * 128:(j + 1) * 128], in_=src[:, a * 64:(a + 1) * 64],
                                                               identity=identb[:]),
                 reads=[b_qt[sl], b_kt[sl], b_const], writes=[b_bank[7]])
        P.op("act", lambda g: g.copy(out=qkT_t[sl][0:64, :], in_=bank7b[0:64, :]), reads=[b_bank[7]], writes=[b_qkT[sl]])

    def stageHI(i):
        sl = i % 2
        vb, bvb = vb_of(i)
        qkT = qkT_t[sl]
        Sb_v = Sb_t[sl].rearrange("p (h e) -> p h e", h=4)
        for h in range(4):
            P.op("pe", lambda g, h=h: g.matmul(out=banks[2][:, h * 128:(h + 1) * 128], lhsT=qkT[0:64, (4 + h) * 128:(5 + h) * 128],
                                              rhs=qkT[0:64, h * 128:(h + 1) * 128], start=True, stop=True),
                 reads=[b_qkT[sl]], writes=[b_bank[2]])
        P.op("dve", lambda g: g.tensor_tensor(out=Pm, in0=banks[2][:, 0:512], in1=maskb[:], op=ALU.mult),
             reads=[b_bank[2], b_const], writes=[b_Pm])
        for h in range(4):
            P.op("pe", lambda g, h=h: g.matmul(out=banks[7][:, h * 128:(h + 1) * 128], lhsT=vb[:, h * 128:(h + 1) * 128],
                                              rhs=Pm[:, h * 128:(h + 1) * 128], start=True, stop=False),
                 reads=[bvb, b_Pm], writes=[b_bank[7]])
            P.op("pe", lambda g, h=h: g.matmul(out=banks[7][:, h * 128:(h + 1) * 128], lhsT=Sb_v[0:64, h, :],
                                              rhs=qkT[0:64, h * 128:(h + 1) * 128], start=False, stop=True),
                 reads=[b_Sb[sl], b_qkT[sl]], writes=[b_bank[7]])
        P.op("act", lambda g: g.activation(out=sq_t[sl], in_=banks[7][:, 0:512], func=AF.Square), reads=[b_bank[7]], writes=[b_sq[sl]])

    def stageJ(i):
        oi = i - NT_PRE
        sl = i % 2
        P.op("pe", lambda g: g.matmul(out=banks[2][:, 0:512], lhsT=onesb[:], rhs=sq_t[sl], start=True, stop=True),
             reads=[b_sq[sl], b_const], writes=[b_bank[2]])
        P.op("act", lambda g: g.activation(out=rstd, in_=banks[2][:, 0:512], func=AF.Ln, bias=EPS), reads=[b_bank[2]], writes=[b_rstd])
        P.op("act", lambda g: g.activation(out=rstd, in_=rstd, func=AF.Exp, scale=-0.5), reads=[b_rstd], writes=[b_rstd])
        P.op("dve", lambda g: g.tensor_tensor(out=y1_all_v[:, :, oi * 128:(oi + 1) * 128],
                                              in0=banks[7][:, 0:512].rearrange("p (h t) -> p h t", h=4),
                                              in1=rstd.rearrange("p (h t) -> p h t", h=4), op=ALU.mult),
             reads=[b_bank[7], b_rstd], writes=[b_y1[oi]])

    T0 = int(os.environ.get('K_T0', 0))
    T1 = int(os.environ.get('K_T1', NT_ALL))
    pre_stages = [stageA, stageB, stageC, stageD, stageE, lambda i: (stageF(i), stageS(i))]
    npre = min(T1, NT_PRE)
    for step in range(T0, npre + len(pre_stages) - 1):
        for k, st in enumerate(pre_stages):
            t = step - k
            if T0 <= t < npre:
                st(t)
    b_carry = Buf("carry")
    carry_v = carry_t[:].rearrange("p (c j) -> p c j", c=4)
    hT47, b_h47 = hT_of(NT_PRE - 1)
    for cc in range(4):
        for k in range(8):
            P.op("pe", lambda g, cc=cc, k=k: g.matmul(out=banks[2][:, 0:2], lhsT=Wgbcu[:, k, 1024 + cc * 128:1024 + (cc + 1) * 128],
                                                     rhs=hT47[:, k, 126:128], start=(k == 0), stop=(k == 7)),
                 reads=[b_h47, b_wg], writes=[b_bank[2]])
        for k in range(8):
            P.op("pe", lambda g, cc=cc, k=k: g.matmul(out=banks[3][:, 0:2], lhsT=Wgbcu[:, k, 1536 + cc * 128:1536 + (cc + 1) * 128],
                                                     rhs=hT47[:, k, 126:128], start=(k == 0), stop=(k == 7)),
                 reads=[b_h47, b_wg], writes=[b_bank[3]])
        P.op("act", lambda g: g.copy(out=small[:, 4:6], in_=banks[2][:, 0:2]), reads=[b_bank[2]], writes=[b_ss[0]])
        P.op("dve", lambda g, cc=cc: g.tensor_tensor(out=carry_v[:, cc, :], in0=banks[3][:, 0:2], in1=small[:, 4:6], op=ALU.mult),
             reads=[b_bank[3], b_ss[0]], writes=[b_carry])
    for dst, src in ((b_vb4[2], b_hTs[0]), (b_vb4[3], b_hTs[0]), (b_qkT[1], b_hTs[1]),
                     (b_Sb[1], b_hTs[2]), (b_sq[1], b_hTs[2])):
        dst.w = src.w
        dst.r = list(src.r)
    own_stages = [(stageJ, 7), (stageA, 0), (stageB, 1), (stageC, 2), (stageD, 3), (stageE, 4), (stageFo, 5), (stageHI, 6)]
    o0 = max(T0, NT_PRE)
    for step in range(o0, T1 + 7):
        for st, k in own_stages:
            t = step - k
            if o0 <= t < T1:
                st(t)

    if stop_after in ("p1", "p1s"):
        P.barrier(); P.emit(); return nc, es
    P.barrier()
    b_xr = [Buf("xr%d" % i) for i in range(NT_OWN)]
    for t in range(NT_OWN):
        ld("sp", xres[:, t * D:(t + 1) * D], xs_d[(NT_PRE + t) * 128:(NT_PRE + t + 1) * 128, :], b_xr[t], P.dsem())
    b_gB = Buf("gB")
    ds_gB = P.dsem()
    ld("sp", gB[:], gmoe_d.broadcast_to([128, D]), b_gB, ds_gB)

    c2 = Carver(scr2, 7200)
    gs = c2.f32(512)
    Ct = c2.f32(512)
    cu = c2.f32(520)
    acc = c2.f32(512)
    yg = c2.bf16(2048).rearrange("p (c t) -> p c t", c=4)
    ycv = c2.bf16(2048).rearrange("p (c t) -> p c t", c=4)
    h2_t = [c2.f32(1024) for _ in range(2)]
    h2Tf = c2.f32(1024).rearrange("p (k t) -> p k t", k=8)
    b_gs, b_Ct, b_cu, b_acc = Buf("gs"), Buf("Ct"), Buf("cu"), Buf("acc")
    b_yg, b_ycv, b_h2Tf, b_Lg = Buf("yg"), Buf("ycv"), Buf("h2Tf"), Buf("Lg")
    b_h2 = [Buf("h2a"), Buf("h2b")]
    b_LTs = Buf("LTs")

    Lg_v = Lg[:].rearrange("p (t n) -> p t n", t=NT_OWN)
    L2 = int(os.environ.get('K_L2', 99))
    b_s2 = [[Buf("s2_%d_%d" % (a, b)) for b in range(2)] for a in range(2)]
    for blk in range(4 if L2 >= 1 else 0):
        tcols = slice(blk * 512, (blk + 1) * 512)
        bts = [b_hT[blk * 4 + t] for t in range(4)]
        for cc in range(4):
            bb = 4 * (cc % 2)
            for k in range(8):
                P.op("pe", lambda g, cc=cc, k=k, bb=bb, tcols=tcols: g.matmul(out=banks[bb][:, 0:512], lhsT=Wgbcu[:, k, cc * 128:(cc + 1) * 128],
                                                                rhs=hT_all_v[:, k, tcols], start=(k == 0), stop=(k == 7)),
                     reads=bts + [b_wg], writes=[b_bank[bb]])
            P.op("act", lambda g, bb=bb: g.activation(out=gs, in_=banks[bb][:, 0:512], func=AF.Silu), reads=[b_bank[bb]], writes=[b_gs])
            P.op("dve", lambda g, cc=cc, tcols=tcols: g.scalar_tensor_tensor(out=yg[:, cc, :], in0=y1_all_v[:, cc, tcols], scalar=ggo[:, 0:1],
                                                                in1=gs, op0=ALU.mult, op1=ALU.mult),
                 reads=[b_gs, b_const] + [b_y1[blk * 4 + t] for t in range(4)], writes=[b_yg])
            for (bk, off) in ((bb + 1, 512), (bb + 2, 1024), (bb + 3, 1536)):
                for k in range(8):
                    P.op("pe", lambda g, cc=cc, k=k, bk=bk, off=off, tcols=tcols: g.matmul(
                        out=banks[bk][:, 0:512], lhsT=Wgbcu[:, k, off + cc * 128:off + (cc + 1) * 128],
                        rhs=hT_all_v[:, k, tcols], start=(k == 0), stop=(k == 7)),
                        reads=bts + [b_wg], writes=[b_bank[bk]])
            P.op("act", lambda g, bb=bb: g.copy(out=Ct, in_=banks[bb + 2][:, 0:512]), reads=[b_bank[bb + 2]], writes=[b_Ct])
            P.op("act", lambda g, cc=cc: g.copy(out=cu[:, 0:2], in_=carry_v[:, cc, :]), reads=[b_carry], writes=[b_cu])
            P.op("dve", lambda g, bb=bb: g.tensor_tensor(out=cu[:, 2:514], in0=banks[bb + 3][:, 0:512], in1=Ct, op=ALU.mult),
                 reads=[b_bank[bb + 3], b_Ct], writes=[b_cu])
            P.op("act", lambda g, cc=cc: g.copy(out=carry_v[:, cc, :], in_=cu[:, 512:514]), reads=[b_cu], writes=[b_carry])
            P.op("dve", lambda g, cc=cc: g.tensor_scalar(out=acc, in0=cu[:, 2:514], scalar1=wconv[:, cc * 3 + 2:cc * 3 + 3], scalar2=None,
                                                         op0=ALU.mult),
                 reads=[b_cu, b_const], writes=[b_acc])
            P.op("dve", lambda g, cc=cc: g.scalar_tensor_tensor(out=acc, in0=cu[:, 1:513], scalar=wconv[:, cc * 3 + 1:cc * 3 + 2], in1=acc,
                                                                op0=ALU.mult, op1=ALU.add),
                 reads=[b_cu, b_const], writes=[b_acc])
            P.op("dve", lambda g, cc=cc: g.scalar_tensor_tensor(out=acc, in0=cu[:, 0:512], scalar=wconv[:, cc * 3:cc * 3 + 1], in1=acc,
                                                                op0=ALU.mult, op1=ALU.add),
                 reads=[b_cu, b_const], writes=[b_acc])
            P.op("dve", lambda g, cc=cc, bb=bb: g.tensor_tensor(out=ycv[:, cc, :], in0=banks[bb + 1][:, 0:512], in1=acc, op=ALU.mult),
                 reads=[b_bank[bb + 1], b_acc], writes=[b_ycv])

        if blk == 3:
            load_expert(0, extra=[b_wg])

        def st2P(t):
            ti = blk * 4 + t
            sl = t % 2
            xr = xres[:, ti * D:(ti + 1) * D]
            for half in range(2):
                for kc in range(8):
                    src = yg if kc < 4 else ycv
                    bsrc = b_yg if kc < 4 else b_ycv
                    P.op("pe", lambda g, half=half, kc=kc, src=src: g.matmul(
                        out=banks[half][:, 0:512], lhsT=src[:, kc % 4, t * 128:(t + 1) * 128],
                        rhs=Wout[:, kc, half * 512:(half + 1) * 512], start=(kc == 0), stop=(kc == 7)),
                        reads=[bsrc, b_wo], writes=[b_bank[half]])
                P.op("dve", lambda g, half=half: g.tensor_tensor(out=xr[:, half * 512:(half + 1) * 512], in0=banks[half][:, 0:512],
                                                                 in1=xr[:, half * 512:(half + 1) * 512], op=ALU.add),
                     reads=[b_bank[half]], writes=[b_xr[ti]])
            c = 2 + 8 * sl
            rms_stats(xr, b_xr[ti], c, c + 1, h2_t[sl].bitcast(BF16)[:, 0:1024], b_h2[sl], b_s2[sl][0], b_s2[sl][1])
            P.op("dve", lambda g: g.scalar_tensor_tensor(out=h2_t[sl], in0=xr, scalar=small[:, c + 1:c + 2], in1=gB[:], op0=ALU.mult, op1=ALU.mult),
                 reads=[b_xr[ti], b_s2[sl][1], b_gB], writes=[b_h2[sl]])

        def st2Q(t):
            ti = blk * 4 + t
            sl = t % 2
            h2 = h2_t[sl]
            for k in range(8):
                bk = 2 + k // 4
                P.op("pe", lambda g, k=k, bk=bk: g.transpose(out=banks[bk][:, (k % 4) * 128:(k % 4 + 1) * 128], in_=h2[:, k * 128:(k + 1) * 128],
                                                            identity=identf[:]),
                     reads=[b_h2[sl], b_const], writes=[b_bank[bk]])
            for hh in range(2):
                P.op("dve", lambda g, hh=hh: g.tensor_copy(out=h2Tf[:, hh * 4:(hh + 1) * 4, :],
                                                           in_=banks[2 + hh][:, 0:512].rearrange("p (k t) -> p k t", k=4)),
                     reads=[b_bank[2 + hh]], writes=[b_h2Tf])
            P.op("act", lambda g: g.copy(out=hT_all_v[:, :, ti * 128:(ti + 1) * 128], in_=h2Tf),
                 reads=[b_h2Tf], writes=[b_hT[ti]])

        def st2R(t):
            for k in range(8):
                P.op("pe", lambda g, k=k: g.matmul(out=banks[4][0:20, 0:128], lhsT=wr[:, k * 20:(k + 1) * 20], rhs=h2Tf[:, k, :],
                                                  start=(k == 0), stop=(k == 7)),
                     reads=[b_h2Tf, b_const], writes=[b_bank[4]])
            P.op("act", lambda g: g.copy(out=LTs[0:20, :], in_=banks[4][0:20, 0:128]), reads=[b_bank[4]], writes=[b_LTs])

        def st2S(t):
            ti = blk * 4 + t
            P.op("pe", lambda g: g.matmul(out=banks[5][:, 0:20], lhsT=LTs[0:20, :], rhs=identf[0:20, 0:20], start=True, stop=True),
                 reads=[b_LTs, b_const], writes=[b_bank[5]])
            P.op("dve", lambda g: g.tensor_tensor(out=Lg_v[:, ti, :], in0=banks[5][:, 0:20], in1=brb[:], op=ALU.add),
                 reads=[b_bank[5], b_const], writes=[b_Lg])

        for step in range(7):
            if step < 4:
                st2P(step)
            if 3 <= step:
                st2S(step - 3)
            if 2 <= step < 6:
                st2R(step - 2)
            if 1 <= step < 5:
                st2Q(step - 1)

    if stop_after == "p2":
        P.barrier(); P.emit(); return nc, es
    P.barrier()
    b_g5 = Buf("g5")
    ds_g5 = P.dsem()
    ld("sp", gA[:], gple_d.broadcast_to([128, D]), b_g5, ds_g5)
    ld("sp", gB[:], gfin_d.broadcast_to([128, D]), b_g5, ds_g5)
    c3 = Carver(scr2, 7200)
    T16 = NT_OWN
    gmax = c3.f32(16)
    ohg = c3.f32(64)
    ge = c3.f32(64)
    gsum = c3.f32(16)
    pg = c3.f32(16)
    gw = c3.f32(64)
    m1 = c3.f32(64)
    eq1 = c3.f32(256)
    el2 = c3.f32(256)
    m2 = c3.f32(64)
    sel = c3.f32(256)
    dd = c3.f32(256)
    ex = c3.f32(256)
    den = c3.f32(64)
    rden = c3.f32(64)
    wq = c3.f32(256)
    b_r = Buf("route")
    gl = Lg_v[:, :, 0:4]
    el4 = Lg_v[:, :, 4:20].rearrange("p t (g j) -> p t g j", g=4)

    def v3(ap, n=4):
        return ap.rearrange("p (t g) -> p t g", g=n)

    def v4(ap):
        return ap.rearrange("p (t g j) -> p t g j", g=4, j=4)

    def bc3(ap16):
        return ap16.unsqueeze(2).broadcast_to([128, T16, 4])

    def bc4(ap64):
        return ap64.rearrange("p (t g) -> p t g", g=4).unsqueeze(3).broadcast_to([128, T16, 4, 4])

    R = dict(reads=[b_r, b_Lg], writes=[b_r])
    P.op("dve", lambda g: g.tensor_reduce(out=gmax, in_=gl, axis=AX.X, op=ALU.max), **R)
    P.op("dve", lambda g: g.tensor_tensor(out=v3(ohg), in0=gl, in1=bc3(gmax), op=ALU.is_equal), **R)
    P.op("dve", lambda g: g.tensor_tensor(out=v3(ge), in0=gl, in1=bc3(gmax), op=ALU.subtract), **R)
    P.op("act", lambda g: g.activation(out=ge, in_=ge, func=AF.Exp), **R)
    P.op("dve", lambda g: g.tensor_reduce(out=gsum, in_=v3(ge), axis=AX.X, op=ALU.add), **R)
    P.op("dve", lambda g: g.reciprocal(out=pg, in_=gsum), **R)
    P.op("dve", lambda g: g.tensor_tensor(out=v3(gw), in0=v3(ohg), in1=bc3(pg), op=ALU.mult), **R)
    P.op("dve", lambda g: g.tensor_reduce(out=v3(m1), in_=el4, axis=AX.X, op=ALU.max), **R)
    P.op("dve", lambda g: g.tensor_tensor(out=v4(eq1), in0=el4, in1=bc4(m1), op=ALU.is_equal), **R)
    P.op("dve", lambda g: g.scalar_tensor_tensor(out=v4(el2), in0=v4(eq1), scalar=-1e30, in1=el4, op0=ALU.mult, op1=ALU.add), **R)
    P.op("dve", lambda g: g.tensor_reduce(out=v3(m2), in_=v4(el2), axis=AX.X, op=ALU.max), **R)
    P.op("dve", lambda g: g.tensor_tensor(out=v4(sel), in0=el4, in1=bc4(m2), op=ALU.is_ge), **R)
    P.op("dve", lambda g: g.tensor_tensor(out=v4(dd), in0=el4, in1=bc4(m1), op=ALU.subtract), **R)
    P.op("act", lambda g: g.activation(out=ex, in_=dd, func=AF.Exp), **R)
    P.op("dve", lambda g: g.tensor_tensor(out=ex, in0=ex, in1=sel, op=ALU.mult), **R)
    P.op("dve", lambda g: g.tensor_reduce(out=v3(den), in_=v4(ex), axis=AX.X, op=ALU.add), **R)
    P.op("dve", lambda g: g.reciprocal(out=rden, in_=den), **R)
    P.op("dve", lambda g: g.tensor_tensor(out=v4(wq), in0=v4(ex), in1=bc4(rden), op=ALU.mult), **R)
    b_comb = Buf("comb")
    P.op("dve", lambda g: g.tensor_tensor(out=v4(comb[:]), in0=v4(wq), in1=bc4(gw), op=ALU.mult), reads=[b_r], writes=[b_comb])
    comb_v = comb[:].rearrange("p (t e) -> p t e", e=16)

    if stop_after == "p3":
        P.barrier(); P.emit(); return nc, es
    assert c3.off <= 2048
    c4 = Carver(scr2, 7200)
    c4.off = 2048
    hid = [c4.bf16(2048).rearrange("p (c t) -> p c t", c=4) for _ in range(2)]
    sg = [c4.f32(512) for _ in range(2)]
    b_hid = [Buf("hid0"), Buf("hid1")]
    b_sg = [Buf("sg0"), Buf("sg1")]
    cnt4 = {"it": 0, "dn": 0}

    def moe_gu(e, blk):
        s = e % 2
        Wg_e, Wu_e, Wd_e = WE[s]
        tcols = slice(blk * 512, (blk + 1) * 512)
        bts = [b_hT[blk * 4 + t] for t in range(4)]
        hs = (e * 4 + blk) % 2
        for hc in range(4):
            pb = (cnt4["it"] % 2) * 2
            cnt4["it"] += 1
            ss_ = cnt4["it"] % 2
            for k in range(8):
                P.op("pe", lambda g, k=k, hc=hc, pb=pb: g.matmul(
                    out=banks[pb][:, 0:512], lhsT=Wg_e[:, k, hc * 128:(hc + 1) * 128], rhs=hT_all_v[:, k, tcols],
                    start=(k == 0), stop=(k == 7)), reads=bts + [b_we[s]], writes=[b_bank[pb]])
            for k in range(8):
                P.op("pe", lambda g, k=k, hc=hc, pb=pb: g.matmul(
                    out=banks[pb + 1][:, 0:512], lhsT=Wu_e[:, k, hc * 128:(hc + 1) * 128], rhs=hT_all_v[:, k, tcols],
                    start=(k == 0), stop=(k == 7)), reads=bts + [b_we[s]], writes=[b_bank[pb + 1]])
            P.op("act", lambda g, pb=pb, ss_=ss_: g.activation(out=sg[ss_], in_=banks[pb][:, 0:512], func=AF.Silu),
                 reads=[b_bank[pb]], writes=[b_sg[ss_]])
            P.op("dve", lambda g, pb=pb, ss_=ss_, hc=hc: g.tensor_tensor(out=hid[hs][:, hc, :], in0=banks[pb + 1][:, 0:512],
                                                                         in1=sg[ss_], op=ALU.mult),
                 reads=[b_bank[pb + 1], b_sg[ss_]], writes=[b_hid[hs]])

    def moe_dn(e, blk):
        s = e % 2
        Wg_e, Wu_e, Wd_e = WE[s]
        hs = (e * 4 + blk) % 2
        for t in range(4):
            ti = blk * 4 + t
            xr = xres[:, ti * D:(ti + 1) * D]
            db = 4 + (cnt4["dn"] % 2) * 2
            cnt4["dn"] += 1
            for half in range(2):
                for hc in range(4):
                    P.op("pe", lambda g, t=t, half=half, hc=hc, db=db: g.matmul(
                        out=banks[db + half][:, 0:512], lhsT=hid[hs][:, hc, t * 128:(t + 1) * 128],
                        rhs=Wd_e[:, hc, half * 512:(half + 1) * 512], start=(hc == 0), stop=(hc == 3)),
                        reads=[b_hid[hs], b_we[s]], writes=[b_bank[db + half]])
                P.op("dve", lambda g, half=half, db=db, xr=xr, ti=ti: g.scalar_tensor_tensor(
                    out=xr[:, half * 512:(half + 1) * 512], in0=banks[db + half][:, 0:512], scalar=comb_v[:, ti, e:e + 1],
                    in1=xr[:, half * 512:(half + 1) * 512], op0=ALU.mult, op1=ALU.add),
                    reads=[b_bank[db + half], b_comb], writes=[b_xr[ti]])

    prev = None
    for e in range(NE):
        if e > 0:
            load_expert(e)
        for blk in range(4):
            moe_gu(e, blk)
            if prev is not None:
                moe_dn(*prev)
            prev = (e, blk)
            if e == NE - 1 and blk == 0:
                ld("pool", Wpg, wpg_d.rearrange("(k p) c -> p k c", p=128), [b_w5, b_we[0]], ds_w5)
                ld("pool", Wpp, wpp_d.rearrange("(k p) c -> p k c", p=128), [b_w5, b_we[0]], ds_w5)
    moe_dn(*prev)

    if stop_after == "p4":
        P.barrier(); P.emit(); return nc, es
    P.barrier()
    c5 = Carver(scr2, 7200)
    pt = [c5.f32(256) for _ in range(2)]
    ptb = [c5.bf16(256) for _ in range(2)]
    pT = [c5.bf16(256).rearrange("p (k t) -> p k t", k=2) for _ in range(2)]
    h3b = [c5.bf16(1024) for _ in range(2)]
    h3T = [c5.bf16(1024).rearrange("p (k t) -> p k t", k=8) for _ in range(2)]
    sig = c5.f32(1024)
    outst = [c5.f32(1024) for _ in range(2)]
    b_pt = [Buf("pt0"), Buf("pt1")]
    ds_pt = [P.dsem(), P.dsem()]
    b_ptb, b_pT = [Buf("ptb0"), Buf("ptb1")], [Buf("pT0"), Buf("pT1")]
    b_h3b, b_h3T = [Buf("h3b0"), Buf("h3b1")], [Buf("h3T0"), Buf("h3T1")]
    b_sig = Buf("sig")
    b_out = [Buf("o0"), Buf("o1")]
    ds_out = [P.dsem(), P.dsem()]
    b_s5 = [[Buf("s5_%d_%d" % (a, b)) for b in range(4)] for a in range(2)]
    bank1b = banks[1][:, 0:512].bitcast(BF16)
    last_store = []

    def st5X(ti):
        sl = ti % 2
        xr = xres[:, ti * D:(ti + 1) * D]
        ld("sp", pt[sl], p_d[ti * 128:(ti + 1) * 128, :], b_pt[sl], ds_pt[sl])
        c = 4 + 8 * sl
        rms_stats(xr, b_xr[ti], c, c + 1, h3b[sl], b_h3b[sl], b_s5[sl][0], b_s5[sl][1])
        P.op("dve", lambda g: g.scalar_tensor_tensor(out=h3b[sl], in0=xr, scalar=small[:, c + 1:c + 2], in1=gA[:], op0=ALU.mult, op1=ALU.mult),
             reads=[b_xr[ti], b_s5[sl][1], b_g5], writes=[b_h3b[sl]])
        P.op("dve", lambda g: g.tensor_copy(out=ptb[sl], in_=pt[sl]), reads=[b_pt[sl]], writes=[b_ptb[sl]])

    def st5Y(ti):
        sl = ti % 2
        for k in range(8):
            P.op("pe", lambda g, k=k: g.transpose(out=bank0b[:, k * 128:(k + 1) * 128], in_=h3b[sl][:, k * 128:(k + 1) * 128], identity=identb[:]),
                 reads=[b_h3b[sl], b_const], writes=[b_bank[0]])
        P.op("dve", lambda g: g.tensor_copy(out=h3T[sl], in_=bank0b.rearrange("p (k t) -> p k t", k=8)), reads=[b_bank[0]], writes=[b_h3T[sl]])
        for k in range(2):
            P.op("pe", lambda g, k=k: g.transpose(out=bank1b[:, k * 128:(k + 1) * 128], in_=ptb[sl][:, k * 128:(k + 1) * 128], identity=identb[:]),
                 reads=[b_ptb[sl], b_const], writes=[b_bank[1]])
        P.op("dve", lambda g: g.tensor_copy(out=pT[sl], in_=bank1b[:, 0:256].rearrange("p (k t) -> p k t", k=2)), reads=[b_bank[1]], writes=[b_pT[sl]])

    def st5Z(ti):
        sl = ti % 2
        xr = xres[:, ti * D:(ti + 1) * D]
        for half in range(2):
            for k in range(8):
                P.op("pe", lambda g, k=k, half=half: g.matmul(out=banks[2 + half][:, 0:512], lhsT=h3T[sl][:, k, :],
                                                             rhs=Wpg[:, k, half * 512:(half + 1) * 512], start=(k == 0), stop=(k == 7)),
                     reads=[b_h3T[sl], b_w5], writes=[b_bank[2 + half]])
            for k in range(2):
                P.op("pe", lambda g, k=k, half=half: g.matmul(out=banks[4 + half][:, 0:512], lhsT=pT[sl][:, k, :],
                                                             rhs=Wpp[:, k, half * 512:(half + 1) * 512], start=(k == 0), stop=(k == 1)),
                     reads=[b_pT[sl], b_w5], writes=[b_bank[4 + half]])
            P.op("act", lambda g, half=half: g.activation(out=sig[:, half * 512:(half + 1) * 512], in_=banks[2 + half][:, 0:512], func=AF.Sigmoid),
                 reads=[b_bank[2 + half]], writes=[b_sig])
            P.op("dve", lambda g, half=half: g.tensor_tensor(out=sig[:, half * 512:(half + 1) * 512], in0=banks[4 + half][:, 0:512],
                                                             in1=sig[:, half * 512:(half + 1) * 512], op=ALU.mult),
                 reads=[b_bank[4 + half], b_sig], writes=[b_sig])
        P.op("dve", lambda g: g.tensor_tensor(out=xr, in0=sig, in1=xr, op=ALU.add), reads=[b_sig], writes=[b_xr[ti]])

    def st5W(ti):
        sl = ti % 2
        xr = xres[:, ti * D:(ti + 1) * D]
        c = 6 + 8 * sl
        rms_stats(xr, b_xr[ti], c, c + 1, outst[sl].bitcast(BF16)[:, 0:1024], b_out[sl], b_s5[sl][2], b_s5[sl][3])
        P.op("dve", lambda g: g.scalar_tensor_tensor(out=outst[sl], in0=xr, scalar=small[:, c + 1:c + 2], in1=gB[:], op0=ALU.mult, op1=ALU.mult),
             reads=[b_xr[ti], b_s5[sl][3], b_g5], writes=[b_out[sl]])
        tok = P.op("pool", lambda g: g.dma_start(out=y_d[ti * 128:(ti + 1) * 128, :], in_=outst[sl]),
                   reads=[b_out[sl]], dsem=ds_out[sl])
        last_store.append(tok)

    st5 = [st5X, st5Y, st5Z, st5W]
    for step in range(NT_OWN + len(st5) - 1):
        for k, st in enumerate(st5):
            t = step - k
            if 0 <= t < NT_OWN:
                st(t)
    P.wait_only("pool", last_store[-2:])
    P.wait_only("sp", last_store[-2:])
    P.emit()
    return nc, es


_CACHE = {}


def _consts():
    s = np.arange(128)[:, None]
    t = np.arange(128)[None, :]
    tril = np.where(s <= t, -1.0 / 16.0, 0.0).astype(np.float32)
    triu = np.where(s > t, -1.0 / 16.0, 0.0).astype(np.float32)
    mask = np.tile((s <= t).astype(np.float32), (1, 4))
    return {
        "c_ident": np.eye(128, dtype=np.float32),
        "c_tril": tril,
        "c_triu": triu,
        "c_mask": np.ascontiguousarray(mask),
        "c_ones": np.full((128, 128), 1.0 / 128.0, np.float32),
        "c_neg": np.full((128, 1), -1.0 / 16.0, np.float32),
    }


def kernel(x, p, g_mix, w_in, w_gla_gate, b_gla_gate, g_gla_out, w_conv, w_out,
           g_moe, w_group, b_group, w_router, b_router, w_exp_gate, w_exp_up, w_exp_down,
           g_ple, w_ple_gate, w_ple_proj, g_final):
    f = lambda a: np.ascontiguousarray(np.asarray(a, dtype=np.float32))
    x = f(x); p = f(p)
    if "nc" not in _CACHE:
        _CACHE["nc"] = build_program()
    nc, _es = _CACHE["nc"]
    wg_aug = np.zeros((32, 256), np.float32)
    wg_aug[0:16] = f(w_gla_gate)[0]
    wg_aug[16] = f(b_gla_gate)[0]
    shared = {
        "w_in": f(w_in)[0], "w_out": f(w_out)[0],
        "w_exp_gate": f(w_exp_gate)[0].reshape(NE * D, 512),
        "w_exp_up": f(w_exp_up)[0].reshape(NE * D, 512),
        "w_exp_down": f(w_exp_down)[0].reshape(NE * 512, D),
        "w_ple_gate": f(w_ple_gate)[0], "w_ple_proj": f(w_ple_proj)[0],
        "g_mix": f(g_mix).reshape(1, D), "g_moe": f(g_moe).reshape(1, D), "g_ple": f(g_ple).reshape(1, D),
        "g_final": f(g_final).reshape(1, D),
        "w_rt": np.ascontiguousarray(np.concatenate([f(w_group)[0], f(w_router)[0]], axis=1)),
        "b_rt": np.ascontiguousarray(np.concatenate([f(b_group)[0], f(b_router)[0]], axis=0).reshape(1, 20)),
        "wg_aug": wg_aug,
        "wconv_t": np.ascontiguousarray(f(w_conv)[0].reshape(3, 4, 128).transpose(2, 1, 0).reshape(128, 12)),
        "ggo": np.ascontiguousarray(f(g_gla_out)[0].reshape(128, 1)),
    }
    shared.update(_consts())
    in_maps = []
    for c in range(8):
        b, j = c // 4, c % 4
        xs = np.zeros((NT_ALL * 128, D), np.float32)
        n = 2048 * (j + 1)
        xs[NT_ALL * 128 - n:] = x[b, 0:n]
        m = dict(shared)
        m["xs"] = xs
        m["p_own"] = np.ascontiguousarray(p[0, b, 2048 * j:2048 * (j + 1)])
        in_maps.append(m)
    res = run_bass_kernel_spmd(nc, in_maps, core_ids=list(range(8)))
    out = np.empty((2, 8192, D), np.float32)
    for c in range(8):
        b, j = c // 4, c % 4
        out[b, 2048 * j:2048 * (j + 1)] = res.results[c]["y"]
    return out
```
